# Optimizing a Trainium2 kernel written in Bass

```python
import jax, jax.numpy as jnp
from jax import lax
import numpy as np

D_MODEL = 1024
BATCH = 8
SEQ = 2048
DEPTH = 4

ROPE_THETA = 10000.0
ROPE_DIM = 64
EPS = 1e-6
NEG_INF = -1e30
ADA_CHUNKS = 6

MLA_HEADS = 8
MLA_Q_RANK = 768
MLA_KV_RANK = 512
MLA_NOPE = 128
MLA_ROPE = ROPE_DIM
MLA_V = 128
MLA_Q_BLOCK = 128

DIL_PAIRS = ((128, 1), (512, 4), (2048, 16))
DIL_GROUPS = len(DIL_PAIRS)
DIL_HEADS = 8
DIL_HEAD_DIM = ROPE_DIM
DIL_BLOCK = 64
DIL_W = DIL_GROUPS * DIL_HEADS * DIL_HEAD_DIM

RET_HEADS = 8
RET_QK_DIM = ROPE_DIM
RET_V_DIM = 2 * RET_QK_DIM
RET_CHUNK = 128
RET_QK_W = RET_HEADS * RET_QK_DIM
RET_V_W = RET_HEADS * RET_V_DIM

IN_SPLITS = (MLA_Q_RANK, MLA_KV_RANK, MLA_ROPE,
             DIL_W, DIL_W, DIL_W,
             RET_QK_W, RET_QK_W, RET_V_W, RET_V_W,
             D_MODEL, D_MODEL, D_MODEL)
IN_COLS = sum(IN_SPLITS)

D_FF = 2816
N_EXPERTS = 8
TOP_K = 2
D_FF_EXPERT = 3584
MOE_BLOCK = 256
N_DENSE = (DEPTH + 1) // 2
N_MOE = DEPTH // 2

kernel_name = 'hybrid_mla_dilated_retention_moe_encoder'


def rmsnorm(x, g):
    xf = x.astype(jnp.float32)
    y = xf * lax.rsqrt(jnp.mean(xf * xf, axis=-1, keepdims=True) + EPS)
    return (y * g.astype(jnp.float32)).astype(x.dtype)


def rope_tables(positions):
    inv_freq = ROPE_THETA ** (-jnp.arange(0, ROPE_DIM, 2, dtype=jnp.float32) / ROPE_DIM)
    ang = positions.astype(jnp.float32)[..., None] * inv_freq
    return jnp.cos(ang), jnp.sin(ang)


def apply_rope(t, cos, sin):
    c = cos[:, :, None, :].astype(t.dtype)
    s = sin[:, :, None, :].astype(t.dtype)
    t1, t2 = jnp.split(t, 2, axis=-1)
    return jnp.concatenate([t1 * c - t2 * s, t2 * c + t1 * s], axis=-1)


def split_columns(t, sizes):
    out, start = [], 0
    for n in sizes:
        out.append(t[..., start:start + n])
        start += n
    return out


def mla_attention(q_nope, q_rope, k_nope, k_rope, v):
    B_, S_, H, _ = q_nope.shape
    nqb = S_ // MLA_Q_BLOCK
    scale = (MLA_NOPE + MLA_ROPE) ** -0.5

    def blocks(t):
        return jnp.moveaxis(t.reshape((B_, nqb, MLA_Q_BLOCK) + t.shape[2:]), 1, 0)

    def attend(qs):
        qn, qr = qs
        s = (jnp.einsum('bqhd,bkhd->bhqk', qn, k_nope)
             + jnp.einsum('bqhr,bkr->bhqk', qr, k_rope))
        p = jax.nn.softmax(s.astype(jnp.float32) * scale, axis=-1).astype(v.dtype)
        return jnp.einsum('bhqk,bkhd->bqhd', p, v)

    o = lax.map(attend, (blocks(q_nope), blocks(q_rope)))
    return jnp.moveaxis(o, 0, 1).reshape(B_, S_, H * MLA_V)


def dilated_attention(q, k, v, dilation, band):
    B_, S_, H, dh = q.shape
    L = S_ // dilation
    nb = -(-L // DIL_BLOCK)
    Lp = nb * DIL_BLOCK

    def sub(t):
        t = t.reshape(B_, L, dilation, H, dh).transpose(0, 2, 1, 3, 4)
        return jnp.pad(t, ((0, 0), (0, 0), (0, Lp - L), (0, 0), (0, 0)))

    def neighbours(t):
        tp = jnp.pad(t, ((0, 0), (0, 0), (DIL_BLOCK, DIL_BLOCK), (0, 0), (0, 0)))
        tp = tp.reshape(B_, dilation, nb + 2, DIL_BLOCK, H, dh)
        return jnp.concatenate([tp[:, :, :-2], tp[:, :, 1:-1], tp[:, :, 2:]], axis=3)

    qb = sub(q).reshape(B_, dilation, nb, DIL_BLOCK, H, dh)
    kw = neighbours(sub(k))
    vw = neighbours(sub(v))
    jq = jnp.arange(nb)[:, None] * DIL_BLOCK + jnp.arange(DIL_BLOCK)[None, :]
    jk = (jnp.arange(nb)[:, None] - 1) * DIL_BLOCK + jnp.arange(3 * DIL_BLOCK)[None, :]
    valid = ((jnp.abs(jq[:, :, None] - jk[:, None, :]) <= band)
             & (jk[:, None, :] >= 0) & (jk[:, None, :] < L))
    s = jnp.einsum('brnqhd,brnkhd->brnhqk', qb, kw).astype(jnp.float32) * (dh ** -0.5)
    s = jnp.where(valid[None, None, :, None], s, NEG_INF)
    lse = jax.nn.logsumexp(s, axis=-1, keepdims=True)
    p = jnp.exp(s - lse).astype(v.dtype)
    o = jnp.einsum('brnhqk,brnkhd->brnqhd', p, vw)
    lse = lse[..., 0].transpose(0, 1, 2, 4, 3)

    def unsub(t):
        t = t.reshape((B_, dilation, Lp) + t.shape[4:])[:, :, :L]
        t = jnp.moveaxis(t, 1, 2)
        return t.reshape((B_, S_) + t.shape[3:])

    return unsub(o), unsub(lse)


def retention_scan(q, k, v, log_gamma, inclusive):
    B_, S_, H, dk = q.shape
    dv = v.shape[-1]
    C = RET_CHUNK
    nc = S_ // C

    def chunks(t):
        return t.reshape(B_, nc, C, H, t.shape[-1]).transpose(1, 0, 3, 2, 4)

    idx = jnp.arange(C, dtype=jnp.float32)
    diff = idx[:, None] - idx[None, :]
    mask = (diff >= 0) if inclusive else (diff > 0)
    intra = jnp.where(mask, jnp.exp(log_gamma[:, None, None] * jnp.where(mask, diff, 0.0)), 0.0)
    q_dec = jnp.exp(log_gamma[:, None] * (idx + 1.0))[:, :, None]
    k_dec = jnp.exp(log_gamma[:, None] * (C - 1.0 - idx))[:, :, None]
    c_dec = jnp.exp(log_gamma * C)[:, None, None]

    def step(state, inp):
        qc, kc, vc = inp
        s = jnp.einsum('bhqd,bhkd->bhqk', qc, kc) * intra
        y = (jnp.einsum('bhqk,bhkv->bhqv', s, vc)
             + jnp.einsum('bhqd,bhdv->bhqv', qc, state) * q_dec)
        state = c_dec * state + jnp.einsum('bhkd,bhkv->bhdv', kc * k_dec, vc)
        return state, y

    init = jnp.zeros((B_, H, dk, dv), jnp.float32)
    _, ys = lax.scan(step, init, (chunks(q), chunks(k), chunks(v)))
    return ys.transpose(1, 0, 3, 2, 4).reshape(B_, S_, H, dv)


def token_mixer(h, cos, sin, w_in, q_norm, w_uq, kv_norm, w_ukv, ret_log_decay, ret_norm,
                w_br_mla, w_br_dil, w_br_ret, w_out):
    B_, S_, _ = h.shape
    proj = jnp.einsum('bsd,dn->bsn', h, w_in)
    (cq, ckv, kr, dq, dk, dv, rq, rk, rv, rg, ga, gb, gc) = split_columns(proj, IN_SPLITS)

    cq = rmsnorm(cq, q_norm)
    ckv = rmsnorm(ckv, kv_norm)
    q = jnp.einsum('bsr,rn->bsn', cq, w_uq).reshape(B_, S_, MLA_HEADS, MLA_NOPE + MLA_ROPE)
    q_nope = q[..., :MLA_NOPE]
    q_rope = apply_rope(q[..., MLA_NOPE:], cos, sin)
    kv = jnp.einsum('bsr,rn->bsn', ckv, w_ukv).reshape(B_, S_, MLA_HEADS, MLA_NOPE + MLA_V)
    k_nope, v_mla = kv[..., :MLA_NOPE], kv[..., MLA_NOPE:]
    k_rope = apply_rope(kr[:, :, None, :], cos, sin)[:, :, 0]
    y_mla = mla_attention(q_nope, q_rope, k_nope, k_rope, v_mla)

    n_dh = DIL_GROUPS * DIL_HEADS
    qd = apply_rope(dq.reshape(B_, S_, n_dh, DIL_HEAD_DIM), cos, sin)
    kd = apply_rope(dk.reshape(B_, S_, n_dh, DIL_HEAD_DIM), cos, sin)
    qd = qd.reshape(B_, S_, DIL_GROUPS, DIL_HEADS, DIL_HEAD_DIM)
    kd = kd.reshape(B_, S_, DIL_GROUPS, DIL_HEADS, DIL_HEAD_DIM)
    vd = dv.reshape(B_, S_, DIL_GROUPS, DIL_HEADS, DIL_HEAD_DIM)
    outs, lses = [], []
    for g, (window, dilation) in enumerate(DIL_PAIRS):
        o, lse = dilated_attention(qd[:, :, g], kd[:, :, g], vd[:, :, g], dilation, window // (2 * dilation))
        outs.append(o)
        lses.append(lse)
    outs = jnp.stack(outs)
    wts = jax.nn.softmax(jnp.stack(lses), axis=0)
    y_dil = jnp.sum(wts[..., None].astype(outs.dtype) * outs, axis=0).reshape(B_, S_, DIL_HEADS * DIL_HEAD_DIM)

    rq = apply_rope(rq.reshape(B_, S_, RET_HEADS, RET_QK_DIM), cos, sin).astype(jnp.float32)
    rk = (apply_rope(rk.reshape(B_, S_, RET_HEADS, RET_QK_DIM), cos, sin).astype(jnp.float32)
          * (RET_QK_DIM ** -0.5))
    rv = rv.reshape(B_, S_, RET_HEADS, RET_V_DIM).astype(jnp.float32)
    ld = ret_log_decay.astype(jnp.float32)
    fwd = retention_scan(rq, rk, rv, ld[0], True)
    bwd = jnp.flip(retention_scan(jnp.flip(rq, 1), jnp.flip(rk, 1), jnp.flip(rv, 1), ld[1], False), 1)
    yr = fwd + bwd
    mu = jnp.mean(yr, axis=-1, keepdims=True)
    var = jnp.mean(jnp.square(yr - mu), axis=-1, keepdims=True)
    yr = ((yr - mu) * lax.rsqrt(var + EPS)).reshape(B_, S_, RET_V_W) * ret_norm.astype(jnp.float32)
    y_ret = (jax.nn.silu(rg.astype(jnp.float32)) * yr).astype(h.dtype)

    merged = (jax.nn.sigmoid(ga) * jnp.dot(y_mla, w_br_mla)
              + jax.nn.sigmoid(gb) * jnp.dot(y_dil, w_br_dil)
              + jax.nn.sigmoid(gc) * jnp.dot(y_ret, w_br_ret))
    return jnp.dot(merged, w_out)


def swiglu(h, w1, w3, w2):
    return jnp.dot(jax.nn.silu(jnp.dot(h, w1)) * jnp.dot(h, w3), w2)


def moe_swiglu(h, w_router, w1, w3, w2):
    B_, S_, D = h.shape
    t = h.reshape(-1, D)
    T = t.shape[0]
    logits = jnp.dot(t, w_router).astype(jnp.float32)
    top_val, top_idx = lax.top_k(logits, TOP_K)
    top_w = jax.nn.softmax(top_val, axis=-1)
    A = T * TOP_K
    e_flat = top_idx.reshape(-1)
    tok_flat = jnp.arange(A, dtype=jnp.int32) // TOP_K
    w_flat = top_w.reshape(-1)
    order = jnp.argsort(e_flat)
    e_sorted = e_flat[order]
    counts = jnp.bincount(e_flat, length=N_EXPERTS)
    starts = jnp.cumsum(counts) - counts
    padded = (counts + MOE_BLOCK - 1) // MOE_BLOCK * MOE_BLOCK
    pends = jnp.cumsum(padded)
    pstarts = pends - padded
    dest = pstarts[e_sorted] + (jnp.arange(A) - starts[e_sorted])
    NB = -(-A // MOE_BLOCK) + N_EXPERTS
    R = NB * MOE_BLOCK
    row_tok = jnp.zeros((R,), jnp.int32).at[dest].set(tok_flat[order])
    row_w = jnp.zeros((R,), jnp.float32).at[dest].set(w_flat[order])
    block_expert = jnp.minimum(jnp.searchsorted(pends, jnp.arange(NB) * MOE_BLOCK, side='right'), N_EXPERTS - 1)

    def expert_block(args):
        tok, e = args
        xb = t[tok]
        hb = jax.nn.silu(jnp.dot(xb, w1[e])) * jnp.dot(xb, w3[e])
        return jnp.dot(hb, w2[e])

    yb = lax.map(expert_block, (row_tok.reshape(NB, MOE_BLOCK), block_expert)).reshape(R, D)
    y = jax.ops.segment_sum(yb * row_w[:, None].astype(yb.dtype), row_tok, num_segments=T)
    return y.reshape(B_, S_, D)


def setup_inputs(seed: int = 0) -> dict:
    key = jax.random.key(seed)
    keys = iter(jax.random.split(key, 32))
    f32 = jnp.float32

    def w(shape, fan_in, scale=1.0):
        return jax.random.normal(next(keys), shape, f32) * (scale * fan_in ** -0.5)

    def gain(shape):
        return 1.0 + 0.05 * jax.random.normal(next(keys), shape, f32)

    x = jax.random.normal(next(keys), (BATCH, SEQ, D_MODEL), f32)
    c = jax.random.normal(next(keys), (BATCH, D_MODEL), f32)
    positions = (jnp.arange(SEQ, dtype=jnp.int32)[None, :]
                 + jax.random.randint(next(keys), (BATCH, 1), 0, 4096, dtype=jnp.int32))
    ada_w = w((DEPTH, D_MODEL, ADA_CHUNKS * D_MODEL), D_MODEL, 0.5)
    ada_b = 0.01 * jax.random.normal(next(keys), (DEPTH, ADA_CHUNKS * D_MODEL), f32)
    norm_mix = gain((DEPTH, D_MODEL))
    norm_ffn = gain((DEPTH, D_MODEL))
    w_in = w((DEPTH, D_MODEL, IN_COLS), D_MODEL)
    mla_q_norm = gain((DEPTH, MLA_Q_RANK))
    mla_w_uq = w((DEPTH, MLA_Q_RANK, MLA_HEADS * (MLA_NOPE + MLA_ROPE)), MLA_Q_RANK)
    mla_kv_norm = gain((DEPTH, MLA_KV_RANK))
    mla_w_ukv = w((DEPTH, MLA_KV_RANK, MLA_HEADS * (MLA_NOPE + MLA_V)), MLA_KV_RANK)
    expo = -(5.0 + jnp.arange(RET_HEADS, dtype=f32)) + 0.1 * jax.random.normal(next(keys), (DEPTH, 2, RET_HEADS), f32)
    ret_log_decay = jnp.log1p(-jnp.exp2(expo))
    ret_norm = gain((DEPTH, RET_V_W))
    w_br_mla = w((DEPTH, MLA_HEADS * MLA_V, D_MODEL), MLA_HEADS * MLA_V)
    w_br_dil = w((DEPTH, DIL_HEADS * DIL_HEAD_DIM, D_MODEL), DIL_HEADS * DIL_HEAD_DIM)
    w_br_ret = w((DEPTH, RET_V_W, D_MODEL), RET_V_W)
    w_out = w((DEPTH, D_MODEL, D_MODEL), D_MODEL)
    ffn_w1 = w((N_DENSE, D_MODEL, D_FF), D_MODEL)
    ffn_w3 = w((N_DENSE, D_MODEL, D_FF), D_MODEL)
    ffn_w2 = w((N_DENSE, D_FF, D_MODEL), D_FF)
    moe_router = w((N_MOE, D_MODEL, N_EXPERTS), D_MODEL)
    moe_w1 = w((N_MOE, N_EXPERTS, D_MODEL, D_FF_EXPERT), D_MODEL)
    moe_w3 = w((N_MOE, N_EXPERTS, D_MODEL, D_FF_EXPERT), D_MODEL)
    moe_w2 = w((N_MOE, N_EXPERTS, D_FF_EXPERT, D_MODEL), D_FF_EXPERT)
    final_norm = gain((D_MODEL,))
    return {'x': x, 'c': c, 'positions': positions, 'ada_w': ada_w, 'ada_b': ada_b,
            'norm_mix': norm_mix, 'norm_ffn': norm_ffn, 'w_in': w_in,
            'mla_q_norm': mla_q_norm, 'mla_w_uq': mla_w_uq, 'mla_kv_norm': mla_kv_norm, 'mla_w_ukv': mla_w_ukv,
            'ret_log_decay': ret_log_decay, 'ret_norm': ret_norm,
            'w_br_mla': w_br_mla, 'w_br_dil': w_br_dil, 'w_br_ret': w_br_ret, 'w_out': w_out,
            'ffn_w1': ffn_w1, 'ffn_w3': ffn_w3, 'ffn_w2': ffn_w2,
            'moe_router': moe_router, 'moe_w1': moe_w1, 'moe_w3': moe_w3, 'moe_w2': moe_w2,
            'final_norm': final_norm}


def reference(x, c, positions, ada_w, ada_b, norm_mix, norm_ffn, w_in,
              mla_q_norm, mla_w_uq, mla_kv_norm, mla_w_ukv, ret_log_decay, ret_norm,
              w_br_mla, w_br_dil, w_br_ret, w_out, ffn_w1, ffn_w3, ffn_w2,
              moe_router, moe_w1, moe_w3, moe_w2, final_norm):
    cos, sin = rope_tables(positions)
    c_act = jax.nn.silu(c)
    for layer in range(DEPTH):
        mod = jnp.dot(c_act, ada_w[layer]) + ada_b[layer]
        shift1, scale1, gate1, shift2, scale2, gate2 = jnp.split(mod[:, None, :], ADA_CHUNKS, axis=-1)
        h = rmsnorm(x, norm_mix[layer]) * (1.0 + scale1) + shift1
        x = x + gate1 * token_mixer(h, cos, sin, w_in[layer], mla_q_norm[layer], mla_w_uq[layer],
                                    mla_kv_norm[layer], mla_w_ukv[layer], ret_log_decay[layer],
                                    ret_norm[layer], w_br_mla[layer], w_br_dil[layer],
                                    w_br_ret[layer], w_out[layer])
        h = rmsnorm(x, norm_ffn[layer]) * (1.0 + scale2) + shift2
        if layer % 2 == 0:
            i = layer // 2
            f = swiglu(h, ffn_w1[i], ffn_w3[i], ffn_w2[i])
        else:
            i = layer // 2
            f = moe_swiglu(h, moe_router[i], moe_w1[i], moe_w3[i], moe_w2[i])
        x = x + gate2 * f
    return rmsnorm(x, final_norm)
```

```python
import numpy as np
from contextlib import ExitStack
import concourse.bass as bass
import concourse.mybir as mybir
from concourse.bass_utils import run_bass_kernel_spmd

F32 = mybir.dt.float32
BF16 = mybir.dt.bfloat16
I32 = mybir.dt.int32
AF = mybir.ActivationFunctionType
ALU = mybir.AluOpType

D = 1024
SEQ = 2048
DEPTH = 4
NCORES = 8
KC = 8
NTB = 4
NTT = 16
EPS = 1e-6
IN_COLS = 12096
C_CQ, C_CKV, C_KR, C_DQ, C_DK, C_DV, C_RQ, C_RK, C_RV, C_RG, C_GA, C_GB, C_GC = (
    0, 768, 1280, 1344, 2880, 4416, 5952, 6464, 6976, 8000, 9024, 10048, 11072)
DIL_D = (1, 4, 16)
D_FF = 2816
D_FFE = 3584
NEXP = 8
STRIPW = 3968
NVEC = 82

SEM_LIMIT = 30000
DMA_SEMS_PER_QUEUE = 20


class Buf:
    __slots__ = ("name", "w", "r")

    def __init__(self, name=""):
        self.name = name
        self.w = {}
        self.r = {}


class Sched:
    ENGS = ("pe", "act", "dve", "pool", "sp")

    def __init__(self, nc, stack, nsem=96):
        self.nc = nc
        self.sems = [stack.enter_context(nc.semaphore(f"s{i}")) for i in range(nsem)]
        self.sem_next = 0
        self.streams = {e: [] for e in self.ENGS}
        self.cur = {}
        for e in self.ENGS:
            self.cur[e] = [self._new_sem(), 0]
        self.seen = {e: {} for e in self.ENGS}
        self.dq = {}
        for q in ("sp", "pool"):
            self.dq[q] = {"sems": [[self._new_sem(), 0] for _ in range(DMA_SEMS_PER_QUEUE)], "next": 0}
        self.n_ins = 0

    def _new_sem(self):
        i = self.sem_next
        self.sem_next += 1
        assert i < len(self.sems), "out of semaphores"
        return i

    def _wait(self, eng, sidx, val):
        if self.seen[eng].get(sidx, 0) >= val:
            return
        self.seen[eng][sidx] = val
        sem = self.sems[sidx]
        self.streams[eng].append(lambda e, sem=sem, val=val: e.wait_ge(sem, val))

    def _collect(self, eng, reads, writes, same_engine_ok=False):
        deps = {}
        for b in reads:
            for s, v in b.w.items():
                if deps.get(s, 0) < v:
                    deps[s] = v
        for b in writes:
            for s, v in b.w.items():
                if deps.get(s, 0) < v:
                    deps[s] = v
            for s, v in b.r.items():
                if deps.get(s, 0) < v:
                    deps[s] = v
        own = self.cur[eng][0]
        for s, v in deps.items():
            if same_engine_ok and s == own:
                continue
            self._wait(eng, s, v)

    def _mark(self, ticket, reads, writes):
        s, v = ticket
        for b in reads:
            if b.r.get(s, 0) < v:
                b.r[s] = v
        for b in writes:
            b.w = {s: v}
            b.r = {}

    def op(self, eng, fn, reads=(), writes=(), signal=True):
        self._collect(eng, reads, writes, same_engine_ok=(eng == "pe"))
        cur = self.cur[eng]
        if signal:
            if cur[1] >= SEM_LIMIT:
                cur[0] = self._new_sem()
                cur[1] = 0
            cur[1] += 1
            sem = self.sems[cur[0]]
            self.streams[eng].append(lambda e, fn=fn, sem=sem: fn(e).then_inc(sem, 1))
            ticket = (cur[0], cur[1])
        else:
            assert eng == "pe"
            if cur[1] + 1 > SEM_LIMIT:
                cur[0] = self._new_sem()
                cur[1] = 0
            self.streams[eng].append(lambda e, fn=fn: fn(e))
            ticket = (cur[0], cur[1] + 1)
        self._mark(ticket, reads, writes)
        self.n_ins += 1
        return ticket

    def dma(self, q, fns, reads=(), writes=()):
        if not isinstance(fns, (list, tuple)):
            fns = [fns]
        self._collect(q, reads, writes)
        pool = self.dq[q]
        slot = pool["sems"][pool["next"]]
        pool["next"] = (pool["next"] + 1) % len(pool["sems"])
        if slot[1] > 0:
            self._wait(q, slot[0], slot[1])
        if slot[1] + 16 * len(fns) > SEM_LIMIT:
            slot[0] = self._new_sem()
            slot[1] = 0
        sem = self.sems[slot[0]]
        for fn in fns:
            slot[1] += 16
            self.streams[q].append(lambda e, fn=fn, sem=sem: fn(e).then_inc(sem, 16))
        ticket = (slot[0], slot[1])
        self._mark(ticket, reads, writes)
        self.n_ins += len(fns)
        return ticket

    def barrier(self):
        tickets = []
        for e in self.ENGS:
            c = self.cur[e]
            if c[1] > 0:
                tickets.append((c[0], c[1]))
        for q in self.dq.values():
            for s in q["sems"]:
                if s[1] > 0:
                    tickets.append((s[0], s[1]))
        for e in self.ENGS:
            for s, v in tickets:
                if s == self.cur[e][0] and e == "pe":
                    continue
                self._wait(e, s, v)

    def wait_all(self, eng, bufs):
        for b in bufs:
            for s, v in b.w.items():
                self._wait(eng, s, v)

    def emit(self):
        nc = self.nc
        streams = self.streams
        with nc.Block() as block:
            @block.tensor
            def _(e):
                for f in streams["pe"]:
                    f(e)

            @block.scalar
            def _(e):
                for f in streams["act"]:
                    f(e)

            @block.vector
            def _(e):
                for f in streams["dve"]:
                    f(e)

            @block.gpsimd
            def _(e):
                for f in streams["pool"]:
                    f(e)

            @block.sync
            def _(e):
                for f in streams["sp"]:
                    f(e)


class WStream:
    def __init__(self, bld, jobs):
        self.b = bld
        self.jobs = jobs
        self.views = {}
        self.nxt = 0
        self.done = -1
        self.slot_ctr = 0
        self.slot_job = [-1, -1, -1, -1]

    def _try_issue(self):
        while self.nxt < len(self.jobs):
            need = len(self.jobs[self.nxt])
            slots = [(self.slot_ctr + i) % 4 for i in range(need)]
            if any(self.slot_job[s_] > self.done for s_ in slots):
                return
            vs = []
            for s_, (src, nk, ncols) in zip(slots, self.jobs[self.nxt]):
                vs.append(self.b.wload([s_], src, 128, nk, ncols))
                self.slot_job[s_] = self.nxt
            self.slot_ctr = (self.slot_ctr + need) % 4
            self.views[self.nxt] = vs
            self.nxt += 1

    def get(self, j):
        self._try_issue()
        assert j in self.views, (j, self.nxt, self.done, self.slot_job)
        return self.views.pop(j)

    def finish(self, j):
        self.done = j
        self._try_issue()


class Ring:
    def __init__(self, items):
        self.items = items
        self.i = 0

    def next(self):
        it = self.items[self.i]
        self.i = (self.i + 1) % len(self.items)
        return it


def make_consts():
    c = np.zeros((128, 648), np.float32)
    c[:, 0:128] = np.eye(128, dtype=np.float32)
    for m in range(128):
        if m % 64 < 32:
            c[m + 32, 128 + m] = -1.0
        else:
            c[m - 32, 128 + m] = 1.0
    a = np.arange(128)[:, None]
    b = np.arange(128)[None, :]
    c[:, 256:384] = (a >= b + 64)
    c[:, 384:512] = (np.abs(a - b) <= 64)
    c[:, 512:640] = (a <= b - 64)
    inv_freq = (10000.0 ** (-np.arange(0, 64, 2, dtype=np.float32) / 64.0)).astype(np.float32)
    c[:, 640] = inv_freq[np.arange(128) % 32]
    c[:, 641] = EPS
    dm = (np.arange(STRIPW)[None, :] - 1920 - np.arange(128)[:, None]).astype(np.float32)
    dpn = np.stack([np.maximum(dm, 0.0), np.minimum(dm, 0.0)]).astype(np.float32)
    return c, dpn


class Builder:
    def __init__(self, n_layers=DEPTH, dbg=None):
        self.n_layers = n_layers
        self.dbg = dbg or {}
        self.nc = bass.Bass("TRN2", target_bir_lowering=False)

    def mm(self, out, lhsT, rhs, start, stop, reads, writes, signal=True):
        self.S.op("pe", lambda e: e.matmul(out, lhsT=lhsT, rhs=rhs, start=start, stop=stop), reads, writes, signal)

    def tr(self, out, in_, ident, reads, writes):
        self.S.op("pe", lambda e: e.transpose(out=out, in_=in_, identity=ident), reads, writes)

    def act(self, out, in_, func, reads, writes, scale=None, bias=None):
        kw = {}
        if scale is not None:
            kw["scale"] = scale
            if func == AF.Copy:
                func = AF.Identity
        if bias is not None:
            kw["bias"] = bias
        self.S.op("act", lambda e: e.activation(out=out, in_=in_, func=func, **kw), reads, writes)

    def tt(self, eng, out, in0, in1, op, reads, writes):
        self.S.op(eng, lambda e: e.tensor_tensor(out=out, in0=in0, in1=in1, op=op), reads, writes)

    def ts(self, eng, out, in0, s1, s2, op0, op1, reads, writes):
        if op1 is None:
            self.S.op(eng, lambda e: e.tensor_scalar(out=out, in0=in0, scalar1=s1, scalar2=None, op0=op0), reads, writes)
        else:
            self.S.op(eng, lambda e: e.tensor_scalar(out=out, in0=in0, scalar1=s1, scalar2=s2, op0=op0, op1=op1), reads, writes)

    def stt(self, eng, out, in0, scalar, in1, op0, op1, reads, writes):
        self.S.op(eng, lambda e: e.scalar_tensor_tensor(out=out, in0=in0, scalar=scalar, in1=in1, op0=op0, op1=op1), reads, writes)

    def cp(self, eng, out, in_, reads, writes):
        if eng == "act":
            self.act(out, in_, AF.Copy, reads, writes)
        else:
            self.S.op(eng, lambda e: e.tensor_copy(out=out, in_=in_), reads, writes)

    def recip(self, out, in_, reads, writes):
        self.S.op("dve", lambda e: e.reciprocal(out=out, in_=in_), reads, writes)

    def memset(self, eng, ap, val, writes):
        self.S.op(eng, lambda e: e.memset(ap, val), (), writes)

    def dma(self, q, out, in_, reads, writes, slow=False):
        if slow:
            self.S.dma(q, lambda e: e.dma_start(out=out, in_=in_, allow_slow_non_contiguous=True), reads, writes)
        else:
            self.S.dma(q, lambda e: e.dma_start(out=out, in_=in_), reads, writes)

    def bank(self, ring):
        return ring.next()

    def build(self):
        nc = self.nc
        L = self.n_layers
        din = lambda name, shape, dt=F32: nc.dram_tensor(name, list(shape), dt, kind="ExternalInput").ap()
        self.x_d = din("x", [SEQ, D])
        self.c_d = din("c", [1, D])
        self.pos_d = din("positions", [1, SEQ], I32)
        self.adaw_d = din("ada_w", [DEPTH, D, 6 * D])
        self.vec_d = din("vecpack", [DEPTH, NVEC, 128])
        self.fin_d = din("final_norm", [8, 128])
        self.win_d = din("w_in", [DEPTH, D, IN_COLS])
        self.wuq_d = din("mla_w_uq", [DEPTH, 768, 1536])
        self.wukv_d = din("mla_w_ukv", [DEPTH, 512, 2048])
        self.rld_d = din("ret_log_decay", [1, DEPTH * 16])
        self.wbm_d = din("w_br_mla", [DEPTH, 1024, 1024])
        self.wbd_d = din("w_br_dil", [DEPTH, 512, 1024])
        self.wbr_d = din("w_br_ret", [DEPTH, 1024, 1024])
        self.wout_d = din("w_out", [DEPTH, 1024, 1024])
        self.f1_d = din("ffn_w1", [2, D, D_FF])
        self.f3_d = din("ffn_w3", [2, D, D_FF])
        self.f2_d = din("ffn_w2", [2, D_FF, D])
        self.mr_d = din("moe_router", [2, D, NEXP])
        self.m1_d = din("moe_w1", [2, NEXP, D, D_FFE])
        self.m3_d = din("moe_w3", [2, NEXP, D, D_FFE])
        self.m2_d = din("moe_w2", [2, NEXP, D_FFE, D])
        self.cst_d = din("consts", [128, 648])
        self.dpn_d = din("dposneg", [2, 128, STRIPW])
        self.out_d = nc.dram_tensor("out", [SEQ, D], F32, kind="ExternalOutput").ap()

        def scratch(name, shape, dt=BF16):
            kind = "ExternalOutput" if name in self.dbg else "Internal"
            return nc.dram_tensor(name, list(shape), dt, kind=kind).ap()
        self.xs = scratch("xs", [KC, 128, SEQ], F32)
        self.mg = scratch("mg", [KC, 128, SEQ], F32)
        self.qT = scratch("qT", [8, 192, SEQ])
        self.knT = scratch("knT", [8, 128, SEQ])
        self.vm = scratch("vm", [SEQ, 1024])
        self.dqT = scratch("dqT", [3, 512, SEQ])
        self.dkT = scratch("dkT", [3, 512, SEQ])
        self.dv = scratch("dv", [3, SEQ, 520])
        self.rqT = scratch("rqT", [512, SEQ])
        self.rkT = scratch("rkT", [512, SEQ])
        self.rv = scratch("rv", [SEQ, 1024])
        self.rgT = scratch("rgT", [1024, SEQ])
        self.gT = scratch("gT", [3, 1024, SEQ])
        self.ydbg = scratch("ydbg", [3, 1024, SEQ]) if "ydbg" in self.dbg else None
        self.xsB = [Buf("xs%d" % t) for t in range(NTB)]
        self.mgB = [Buf("mg%d" % t) for t in range(NTB)]
        self.qTB = [Buf() for _ in range(8)]
        self.knTB = [Buf() for _ in range(8)]
        self.vmB = [Buf() for _ in range(2)]
        self.dqTB = [[Buf() for _ in range(4)] for _ in range(3)]
        self.dkTB = [[Buf() for _ in range(4)] for _ in range(3)]
        self.dvB = [Buf() for _ in range(3)]
        self.rqTB = [Buf() for _ in range(4)]
        self.rkTB = [Buf() for _ in range(4)]
        self.rvB = [Buf() for _ in range(2)]
        self.rgTB = [Buf() for _ in range(8)]
        self.gTB = [[Buf() for _ in range(8)] for _ in range(3)]
        self.outB = [Buf() for _ in range(NTB)]
        self.dbgB = Buf()

        with ExitStack() as st:
            self.st = st
            self.S = Sched(nc, st)
            sb = lambda n, sh, dt: st.enter_context(nc.sbuf_tensor(n, sh, dt))
            self.pb = [st.enter_context(nc.psum_tensor("pb%d" % i, [128, 512], F32)) for i in range(8)]
            self.PB = [Buf("pb%d" % i) for i in range(8)]
            self.allbanks = Ring([(self.pb[i], self.PB[i]) for i in range(8)])
            self.RA = sb("RA", [128, KC, SEQ], BF16)
            self.RAb = [[Buf("RA%d_%d" % (k, t)) for t in range(NTB)] for k in range(KC)]
            self.RW = sb("RW", [128, 4, 4096], BF16)
            self.WB = [Buf("W%d" % i) for i in range(4)]
            self.cst = sb("cst", [128, 648], F32); self.cstB = Buf("cst")
            self.cbf = sb("cbf", [128, 1152], BF16); self.cbfB = Buf("cbf")
            self.onesf = sb("onesf", [128, 128], F32); self.onesfB = Buf("onesf")
            self.avgf = sb("avgf", [128, 128], F32)
            self.cosT = sb("cosT", [128, SEQ], BF16); self.sinT = sb("sinT", [128, SEQ], BF16); self.csB = Buf("cs")
            self.krT = sb("krT", [128, SEQ], BF16); self.krB = [Buf() for _ in range(NTB)]
            self.modT = sb("modT", [128, DEPTH, 48], F32); self.modB = Buf("mod")
            self.vecT = sb("vecT", [128, DEPTH, NVEC], F32); self.vecB = Buf("vec")
            self.finT = sb("finT", [128, 8], F32)
            self.gsT = sb("gsT", [128, DEPTH, 16], F32); self.gsB = Buf("gs")
            self.rldT = sb("rldT", [128, DEPTH * 16], F32); self.rldB = Buf("rld")
            self.cact = sb("cact", [128, 8], BF16); self.cactB = Buf("cact")
            self.OVN = 58368
            self.OV = sb("OV", [128, self.OVN], BF16)
            self.ident_f = self.cst[:, 0:128]
            self.ident_b = self.cbf[:, 0:128]
            self.perm_b = self.cbf[:, 128:256]
            self.ones_b = self.cbf[:, 256:384]
            self.mask_b = self.cbf[:, 384:768]
            self.negm_b = self.cbf[:, 768:1152]
            self.eps_ap = self.cst[:, 641:642]

            self.setup()
            for l in range(L):
                self.layer(l)
            self.final()
            if self.dbg:
                self.S.wait_all("sp", [self.dbgB])
            self.S.wait_all("sp", self.outB)
            self.S.emit()
        return nc

    def ovv(self, off, n, dt=BF16):
        nb = n * (2 if dt == F32 else 1)
        assert off + nb <= self.OVN, (off, nb, self.OVN)
        v = self.OV[:, off:off + nb]
        if dt == F32:
            v = v.bitcast(F32)
        return v, off + nb

    def wload(self, slots, src_ap, kp, nk, ncols):
        n = nk * ncols
        assert n <= 4096 * len(slots)
        s0 = slots[0]
        if len(slots) == 1:
            flat = self.RW[0:kp, s0, 0:n]
        else:
            flat = self.RW[0:kp, s0:s0 + len(slots), :].rearrange("p a b -> p (a b)")[:, 0:n]
        view = flat.rearrange("p (k n) -> p k n", n=ncols)
        bufs = [self.WB[s] for s in slots]
        self.dma("pool", view, src_ap, [], bufs)
        return view, bufs

    def setup(self):
        S = self.S
        o = 0
        self.dma("sp", self.cst[:], self.cst_d, [], [self.cstB])
        self.cp("act", self.cbf[:, 0:256], self.cst[:, 0:256], [self.cstB], [self.cbfB])
        self.memset("dve", self.cbf[:, 256:384], 1.0, [self.cbfB])
        self.cp("act", self.cbf[:, 384:768], self.cst[:, 256:640], [self.cstB], [self.cbfB])
        self.memset("dve", self.onesf[:], 1.0, [self.onesfB])
        self.memset("dve", self.avgf[:], 1.0 / 128, [self.onesfB])
        self.ts("dve", self.cbf[:, 768:1152], self.cst[:, 256:640], 1.0, 30000.0, ALU.subtract, ALU.mult, [self.cstB], [self.cbfB])
        self.dma("sp", self.rldT[:], self.rld_d.partition_broadcast(128), [], [self.rldB])
        for l in range(DEPTH):
            self.ts("dve", self.rldT[:, l * 16 + 8:l * 16 + 16], self.rldT[:, l * 16 + 8:l * 16 + 16], -1.0, None, ALU.mult, None,
                    [self.rldB], [self.rldB])
        posi, o1 = self.ovv(0, SEQ, F32)
        posi = self.OV[:, 0:2 * SEQ].bitcast(I32)
        ang, o2 = self.ovv(o1, SEQ, F32)
        kf, o3 = self.ovv(o2, SEQ, F32)
        ki = self.OV[:, o3:o3 + 2 * SEQ].bitcast(I32)
        o4 = o3 + 2 * SEQ
        a2, o5 = self.ovv(o4, SEQ, F32)
        Bp, Ba, Bk, Bki, Ba2 = Buf(), Buf(), Buf(), Buf(), Buf()
        self.dma("sp", posi, self.pos_d.partition_broadcast(128), [], [Bp])
        self.cp("dve", ang, posi, [Bp], [Ba])
        self.ts("dve", ang, ang, self.cst[:, 640:641], None, ALU.mult, None, [Ba, self.cstB], [Ba])
        TWO_PI = float(2 * np.pi)

        def reduce_sin(src, dst_bf, shift):
            self.ts("dve", a2, src, float(shift), None, ALU.add, None, [Ba], [Ba2])
            self.ts("dve", kf, a2, float(1.0 / TWO_PI), None, ALU.mult, None, [Ba2], [Bk])
            self.cp("dve", ki, kf, [Bk], [Bki])
            self.cp("dve", kf, ki, [Bki], [Bk])
            self.stt("dve", a2, kf, -TWO_PI, a2, ALU.mult, ALU.add, [Bk, Ba2], [Ba2])
            self.ts("dve", kf, a2, float(np.pi), -TWO_PI, ALU.is_gt, ALU.mult, [Ba2], [Bk])
            self.tt("dve", a2, a2, kf, ALU.add, [Bk, Ba2], [Ba2])
            self.ts("dve", kf, a2, float(-np.pi), TWO_PI, ALU.is_lt, ALU.mult, [Ba2], [Bk])
            self.tt("dve", a2, a2, kf, ALU.add, [Bk, Ba2], [Ba2])
            self.act(dst_bf, a2, AF.Sin, [Ba2], [self.csB])
        reduce_sin(ang, self.sinT[:], 0.0)
        reduce_sin(ang, self.cosT[:], np.pi / 2)
        vst, o6 = self.ovv(o5, DEPTH * 128 + 128 + 128, F32)
        Bv = Buf()
        for l in range(DEPTH):
            self.dma("sp", vst[0:NVEC, l * 128:(l + 1) * 128], self.vec_d[l], [], [Bv])
        self.dma("sp", vst[0:8, 512:640], self.fin_d, [], [Bv])
        self.dma("sp", vst[0:8, 640:768], self.c_d.rearrange("o (k p) -> (o k) p", p=128), [], [Bv])
        for l in range(DEPTH):
            bk, bb = self.allbanks.next()
            self.tr(bk[:, 0:NVEC], vst[0:NVEC, l * 128:(l + 1) * 128], self.ident_f[0:NVEC, 0:NVEC], [Bv, self.cstB], [bb])
            self.cp("dve", self.vecT[:, l, :], bk[:, 0:NVEC], [bb], [self.vecB])
        bk, bb = self.allbanks.next()
        self.tr(bk[:, 0:8], vst[0:8, 512:640], self.ident_f[0:8, 0:8], [Bv, self.cstB], [bb])
        self.tr(bk[:, 8:16], vst[0:8, 640:768], self.ident_f[0:8, 0:8], [Bv, self.cstB], [bb])
        self.cp("act", self.finT[:], bk[:, 0:8], [bb], [self.vecB])
        self.act(self.cact[:], bk[:, 8:16], AF.Silu, [bb], [self.cactB])
        nblk = 0
        pending = []
        jobs = [(l, cb) for l in range(DEPTH) for cb in range(12)]

        def issue(i):
            l, cb = jobs[i]
            src = self.adaw_d[l].rearrange("(k p) n -> p k n", p=128)[:, :, cb * 512:(cb + 1) * 512]
            return self.wload([i % 4], src, 128, KC, 512)
        for i in range(min(3, len(jobs))):
            pending.append(issue(i))
        for i, (l, cb) in enumerate(jobs):
            W, wb = pending.pop(0)
            if i + 3 < len(jobs):
                pending.append(issue(i + 3))
            if cb % 12 == 0:
                mbk, mbb = self.allbanks.next()
            for j in range(4):
                col = cb * 4 + j
                for kc in range(KC):
                    self.mm(mbk[:, col:col + 1], W[:, kc, j * 128:(j + 1) * 128], self.cact[:, kc:kc + 1], kc == 0, kc == KC - 1,
                            wb + [self.cactB], [mbb], signal=(kc == KC - 1))
            if cb == 11:
                self.tt("dve", self.modT[:, l, :], mbk[:, 0:48], self.vecT[:, l, 0:48], ALU.add, [mbb, self.vecB], [self.modB])
                self.stt("dve", self.gsT[:, l, 0:8], self.modT[:, l, 8:16], 1.0, self.vecT[:, l, 48:56], ALU.add, ALU.mult,
                         [self.modB, self.vecB], [self.gsB])
                self.stt("dve", self.gsT[:, l, 8:16], self.modT[:, l, 32:40], 1.0, self.vecT[:, l, 56:64], ALU.add, ALU.mult,
                         [self.modB, self.vecB], [self.gsB])
        xt0, p = self.ovv(o6, 4 * D, F32)
        xt1, p = self.ovv(p, 4 * D, F32)
        xs0, p = self.ovv(p, KC * 512, F32)
        xs1, p = self.ovv(p, KC * 512, F32)
        xtr = Ring([(xt0.rearrange("p (a d) -> p a d", a=4), Buf()), (xt1.rearrange("p (a d) -> p a d", a=4), Buf())])
        xsr = Ring([(xs0.rearrange("p (k t) -> p k t", k=KC), Buf()), (xs1.rearrange("p (k t) -> p k t", k=KC), Buf())])
        xv = self.x_d.rearrange("(a p) d -> p a d", p=128)
        for tb in range(NTB):
            xt, xtB = xtr.next()
            self.dma("sp", xt, xv[:, 4 * tb:4 * tb + 4, :], [], [xtB])
            xo, xoB = xsr.next()
            for kc in range(KC):
                bk, bb = self.allbanks.next()
                for a in range(4):
                    self.tr(bk[:, a * 128:(a + 1) * 128], xt[:, a, kc * 128:(kc + 1) * 128], self.ident_f, [xtB, self.cstB], [bb])
                self.cp("act" if kc % 2 else "dve", xo[:, kc, :], bk[:, :], [bb], [xoB])
            self.dma("sp", self.xs.rearrange("k p t -> p k t")[:, :, tb * 512:(tb + 1) * 512], xo, [xoB], [self.xsB[tb]])
        S.barrier()

    def norm_phase(self, l, which, tbs, ov0, router=None):
        gs = self.gsT[:, l, 8 * which:8 * which + 8]
        sh = self.modT[:, l, (0 if which == 0 else 24):(0 if which == 0 else 24) + 8]
        p = ov0
        xi = []
        for i in range(2):
            v, p = self.ovv(p, KC * 512, F32)
            xi.append((v.rearrange("p (k t) -> p k t", k=KC), Buf()))
        xir = Ring(xi)
        sq = []
        for i in range(2):
            v, p = self.ovv(p, 512)
            sq.append((v, Buf()))
        sqr = Ring(sq)
        rt = []
        for i in range(2):
            v, p = self.ovv(p, 512, F32)
            rt.append((v, Buf()))
        rtr = Ring(rt)
        tm = []
        for i in range(3):
            v, p = self.ovv(p, 512, F32)
            tm.append((v, Buf()))
        tmr = Ring(tm)
        h32 = []
        if router is not None:
            for i in range(2):
                v, p = self.ovv(p, 512, F32)
                h32.append((v, Buf()))
            h32r = Ring(h32)
        xsv = self.xs.rearrange("k p t -> p k t")
        loaded = {}

        def load(tb):
            x, xB = xir.next()
            self.dma("sp", x, xsv[:, :, tb * 512:(tb + 1) * 512], [self.xsB[tb]], [xB])
            loaded[tb] = (x, xB)
        load(tbs[0])
        for i, tb in enumerate(tbs):
            if i + 1 < len(tbs):
                load(tbs[i + 1])
            x, xB = loaded.pop(tb)
            bk, bb = self.allbanks.next()
            for kc in range(KC):
                s, sB = sqr.next()
                self.act(s, x[:, kc, :], AF.Square, [xB], [sB])
                self.mm(bk[:, :], self.ones_b, s, kc == 0, kc == KC - 1, [sB, self.cbfB], [bb], signal=True)
            r, rB = rtr.next()
            self.act(r, bk[:, :], AF.Sqrt, [bb, self.cstB], [rB], scale=1.0 / D, bias=self.eps_ap)
            self.recip(r, r, [rB], [rB])
            if router is not None:
                lgbk, lgbb = self.allbanks.next()
            for kc in range(KC):
                t, tB = tmr.next()
                self.tt("dve", t, x[:, kc, :], r, ALU.mult, [xB, rB], [tB])
                self.act(self.RA[:, kc, tb * 512:(tb + 1) * 512], t, AF.Identity, [tB, self.gsB, self.modB], [self.RAb[kc][tb]],
                         scale=gs[:, kc:kc + 1], bias=sh[:, kc:kc + 1])
                if router is not None:
                    h, hB = h32r.next()
                    self.act(h, t, AF.Identity, [tB, self.gsB, self.modB], [hB], scale=gs[:, kc:kc + 1], bias=sh[:, kc:kc + 1])
                    wr, wrB = router["w"]
                    for a in range(4):
                        self.mm(lgbk[:, a * 8:(a + 1) * 8], h[:, a * 128:(a + 1) * 128], wr[:, kc, :], (kc == 0 and a == 0), kc == KC - 1,
                                [hB, wrB], [lgbb], signal=(a == 3))
            if router is not None:
                lg, lgB = router["lg"]
                tl = (tb % 2) * 4
                self.cp("dve", lg[:, tl:tl + 4, :], lgbk[:, 0:32].rearrange("p (a e) -> p a e", e=8), [lgbb], [lgB])
        return p

    def proj_fm(self, src, srcB, nk, kp, W, wb, wcol, ncols, handler):
        for tb in range(NTB):
            bk, bb = self.allbanks.next()
            for kc in range(nk):
                self.mm(bk[0:ncols, :], W[0:kp, kc, wcol:wcol + ncols], src[0:kp, kc, tb * 512:(tb + 1) * 512], kc == 0, kc == nk - 1,
                        wb + [srcB[kc][tb]], [bb], signal=(kc == nk - 1))
            self.flush_pe_deferred()
            self._pe_deferred = handler(tb, bk, bb)

    def flush_pe_deferred(self):
        d = getattr(self, "_pe_deferred", None)
        self._pe_deferred = None
        if d is not None:
            d()

    def rope_evac(self, tb, bk, bb, n, tmps, then):
        (cbt, cbB), (ubt, ubB) = tmps.next(), tmps.next()
        cs = slice(tb * 512, (tb + 1) * 512)
        self.tt("dve", cbt[0:n, :], bk[0:n, :], self.cosT[0:n, cs], ALU.mult, [bb, self.csB], [cbB])
        self.tt("dve", ubt[0:n, :], bk[0:n, :], self.sinT[0:n, cs], ALU.mult, [bb, self.csB], [ubB])

        def stage2():
            b2, b2B = self.allbanks.next()
            self.mm(b2[0:n, :], self.ident_b[0:n, 0:n], cbt[0:n, :], True, False, [cbB, self.cbfB], [b2B], signal=False)
            self.mm(b2[0:n, :], self.perm_b[0:n, 0:n], ubt[0:n, :], False, True, [ubB, self.cbfB], [b2B])
            then(b2, b2B)
        return stage2

    def layer(self, l):
        S = self.S
        self._pj_pre = [self.pj_issue(l, i) for i in range(3)]
        self.norm_phase(l, 0, list(range(NTB)), 0)
        S.barrier()
        self.proj_phase(l)
        S.barrier()
        self._br_pre = {0: self.br_issue(l, 0, [0, 1])}
        self.mla_attn(l)
        S.barrier()
        self._br_pre[1] = self.br_issue(l, 1, [2, 3])
        self.branch_out(l, 0)
        S.barrier()
        self.dil_attn(l)
        S.barrier()
        self._br_pre[2] = self.br_issue(l, 2, [0, 1])
        self.branch_out(l, 1)
        S.barrier()
        self._wout_pre = self.wload([2, 3], self.wout_d[l].rearrange("(k p) n -> p k n", p=128), 128, KC, 1024)
        self.ret_attn(l)
        S.barrier()
        self.branch_out(l, 2)
        S.barrier()
        self.ffn(l)
        S.barrier()

    def pj_blocks(self):
        blocks = []
        blocks.append((C_CQ, 512, "cq", 0)); blocks.append((C_CQ + 512, 256, "cq", 4))
        blocks.append((C_CKV, 512, "ckv", 0))
        blocks.append((C_KR, 64, "kr", 0))
        for g in range(3):
            blocks.append((C_DQ + g * 512, 512, "dq", g))
        for g in range(3):
            blocks.append((C_DK + g * 512, 512, "dk", g))
        for g in range(3):
            blocks.append((C_DV + g * 512, 512, "dv", g))
        blocks.append((C_RQ, 512, "rq", 0)); blocks.append((C_RK, 512, "rk", 0))
        blocks.append((C_RV, 512, "rv", 0)); blocks.append((C_RV + 512, 512, "rv", 1))
        blocks.append((C_RG, 512, "rg", 0)); blocks.append((C_RG + 512, 512, "rg", 1))
        for b3 in range(3):
            blocks.append((C_GA + b3 * 1024, 512, "gate", (b3, 0))); blocks.append((C_GA + b3 * 1024 + 512, 512, "gate", (b3, 1)))
        return blocks

    def pj_issue(self, l, i):
        c0, n, _, _ = self.pj_blocks()[i]
        winv = self.win_d[l].rearrange("(k p) n -> p k n", p=128)
        return self.wload([i % 4], winv[:, :, c0:c0 + n], 128, KC, n)

    def br_issue(self, l, b, slots):
        kp = 64 if b == 1 else 128
        if b == 0:
            src = self.wbm_d[l].rearrange("(k p) n -> p k n", p=128)
        elif b == 1:
            src = self.wbd_d[l].rearrange("(k p) n -> p k n", p=64)
        else:
            src = self.wbr_d[l].rearrange("(k p) n -> p k n", p=128)
        return self.wload(slots, src, kp, KC, 1024)

    def proj_phase(self, l):
        p = 0
        cq, p = self.ovv(p, 6 * SEQ)
        cq = cq.rearrange("p (k t) -> p k t", k=6)
        ckv, p = self.ovv(p, 4 * SEQ)
        ckv = ckv.rearrange("p (k t) -> p k t", k=4)
        cqB = [[Buf() for _ in range(NTB)] for _ in range(6)]
        ckvB = [[Buf() for _ in range(NTB)] for _ in range(4)]
        stg = []
        for i in range(3):
            v, p = self.ovv(p, SEQ)
            stg.append((v, Buf()))
        stgr = Ring(stg)
        rtm = []
        for i in range(6):
            v, p = self.ovv(p, 512)
            rtm.append((v, Buf()))
        rtmr = Ring(rtm)
        stv = []
        for i in range(3):
            v, p = self.ovv(p, 520)
            stv.append((v, Buf()))
            self.memset("dve", v, 1.0, [stv[-1][1]])
        stvr = Ring(stv)
        stw = []
        for i in range(3):
            v, p = self.ovv(p, 512)
            stw.append((v, Buf()))
        stwr = Ring(stw)
        sq = []
        for i in range(2):
            v, p = self.ovv(p, 512)
            sq.append((v, Buf()))
        sqr = Ring(sq)
        rt = []
        for i in range(2):
            v, p = self.ovv(p, 512, F32)
            rt.append((v, Buf()))
        rtr = Ring(rt)
        RA, RAb = self.RA, self.RAb
        winv = self.win_d[l].rearrange("(k p) n -> p k n", p=128)

        blocks = []
        blocks.append((C_CQ, 512, "cq", 0)); blocks.append((C_CQ + 512, 256, "cq", 4))
        blocks.append((C_CKV, 512, "ckv", 0))
        blocks.append((C_KR, 64, "kr", 0))
        for g in range(3):
            blocks.append((C_DQ + g * 512, 512, "dq", g))
        for g in range(3):
            blocks.append((C_DK + g * 512, 512, "dk", g))
        for g in range(3):
            blocks.append((C_DV + g * 512, 512, "dv", g))
        blocks.append((C_RQ, 512, "rq", 0)); blocks.append((C_RK, 512, "rk", 0))
        blocks.append((C_RV, 512, "rv", 0)); blocks.append((C_RV + 512, 512, "rv", 1))
        blocks.append((C_RG, 512, "rg", 0)); blocks.append((C_RG + 512, 512, "rg", 1))
        for b3 in range(3):
            blocks.append((C_GA + b3 * 1024, 512, "gate", (b3, 0))); blocks.append((C_GA + b3 * 1024 + 512, 512, "gate", (b3, 1)))
        nb = len(blocks)
        pend = []

        def issue(i):
            c0, n, _, _ = blocks[i]
            return self.wload([i % 4], winv[:, :, c0:c0 + n], 128, KC, n)
        pend.extend(self._pj_pre)

        def fm_store_handler(dram_rows_ap, dramB, n, func=AF.Copy, scale=None):
            st_, stB = stgr.next()

            def h(tb, bk, bb):
                self.act(st_[0:n, tb * 512:(tb + 1) * 512], bk[0:n, :], func, [bb], [stB], scale=scale)
                if tb == NTB - 1:
                    self.dma("sp", dram_rows_ap, st_[0:n, :], [stB], [dramB])
            return h

        def rope_store_handler(dram_rows_ap, dramB, n, d, scale=None, sbuf_dest=None, sbufB=None):
            if sbuf_dest is None:
                st_, stB = stgr.next()
            else:
                st_, stB = sbuf_dest, None

            def h(tb, bk, bb):
                def then(b2, b2B):
                    if d == 1:
                        dst = st_[0:n, tb * 512:(tb + 1) * 512]
                        src = b2[0:n, :]
                    else:
                        w = 512 // d
                        dst = st_[0:n, :].rearrange("p (r l) -> p r l", r=d)[:, :, tb * w:(tb + 1) * w]
                        src = b2[0:n, :].rearrange("p (j r) -> p r j", r=d)
                    wB = [stB] if sbuf_dest is None else [sbufB[tb]]
                    self.act(dst, src, AF.Copy, [b2B], wB, scale=scale)
                    if sbuf_dest is None and tb == NTB - 1:
                        self.dma("sp", dram_rows_ap, st_[0:n, :], [stB], [dramB])
                return self.rope_evac(tb, bk, bb, n, rtmr, then)
            return h

        for bi_, (c0, n, kind, info) in enumerate(blocks):
            W, wb = pend.pop(0)
            if bi_ + 3 < nb:
                pend.append(issue(bi_ + 3))
            if kind in ("cq", "ckv"):
                dstt, dB = (cq, cqB) if kind == "cq" else (ckv, ckvB)
                for j in range(n // 128):
                    c = info + j

                    def h(tb, bk, bb, c=c, dstt=dstt, dB=dB):
                        self.cp("act", dstt[:, c, tb * 512:(tb + 1) * 512], bk[:, :], [bb], [dB[c][tb]])
                    self.proj_fm(RA, RAb, KC, 128, W, wb, j * 128, 128, h)
            elif kind == "kr":
                self.proj_fm(RA, RAb, KC, 128, W, wb, 0, 64, rope_store_handler(None, None, 64, 1, sbuf_dest=self.krT, sbufB=self.krB))
            elif kind in ("dq", "dk"):
                g = info
                dr, dB = (self.dqT, self.dqTB) if kind == "dq" else (self.dkT, self.dkTB)
                for j in range(4):
                    self.proj_fm(RA, RAb, KC, 128, W, wb, j * 128, 128,
                                 rope_store_handler(dr[g, j * 128:(j + 1) * 128, :], dB[g][j], 128, DIL_D[g]))
            elif kind in ("rq", "rk"):
                dr, dB = (self.rqT, self.rqTB) if kind == "rq" else (self.rkT, self.rkTB)
                for j in range(4):
                    self.proj_fm(RA, RAb, KC, 128, W, wb, j * 128, 128,
                                 rope_store_handler(dr[j * 128:(j + 1) * 128, :], dB[j], 128, 1, scale=(0.125 if kind == "rk" else None)))
            elif kind == "rg":
                for j in range(4):
                    o = info * 4 + j
                    self.proj_fm(RA, RAb, KC, 128, W, wb, j * 128, 128, fm_store_handler(self.rgT[o * 128:(o + 1) * 128, :], self.rgTB[o], 128, AF.Silu))
            elif kind == "gate":
                b3, hf = info
                for j in range(4):
                    o = hf * 4 + j
                    self.proj_fm(RA, RAb, KC, 128, W, wb, j * 128, 128,
                                 fm_store_handler(self.gT[b3, o * 128:(o + 1) * 128, :], self.gTB[b3][o], 128, AF.Sigmoid))
            elif kind == "dv":
                self.flush_pe_deferred()
                g = info
                d = DIL_D[g]
                Lr = SEQ // d
                for tt_ in range(NTT):
                    r = (128 * tt_) // Lr
                    j0 = (128 * tt_) % Lr
                    t0 = r + d * j0
                    tsl = slice(t0, t0 + d * 127 + 1, d)
                    tbs = sorted(set([t0 // 512, (t0 + d * 127) // 512])) if d < 16 else list(range(NTB))
                    bk, bb = self.allbanks.next()
                    for kc in range(KC):
                        self.mm(bk[:, :], RA[:, kc, tsl], W[:, kc, :], kc == 0, kc == KC - 1, wb + [RAb[kc][t] for t in tbs], [bb],
                                signal=(kc == KC - 1))
                    sv, svB = stvr.next()
                    self.cp("act" if tt_ % 2 else "dve", sv.rearrange("p (h c) -> p h c", c=65)[:, :, 0:64],
                            bk[:, :].rearrange("p (h c) -> p h c", c=64), [bb], [svB])
                    self.dma("sp", self.dv[g, tt_ * 128:(tt_ + 1) * 128, :], sv, [svB], [self.dvB[g]])
            elif kind == "rv":
                self.flush_pe_deferred()
                hf = info
                for tt_ in range(NTT):
                    tb = tt_ // 4
                    bk, bb = self.allbanks.next()
                    for kc in range(KC):
                        self.mm(bk[:, :], RA[:, kc, tt_ * 128:(tt_ + 1) * 128], W[:, kc, :], kc == 0, kc == KC - 1, wb + [RAb[kc][tb]], [bb],
                                signal=(kc == KC - 1))
                    sw, swB = stwr.next()
                    self.cp("act" if tt_ % 2 else "dve", sw, bk[:, :], [bb], [swB])
                    self.dma("sp", self.rv[tt_ * 128:(tt_ + 1) * 128, hf * 512:(hf + 1) * 512], sw, [swB], [self.rvB[hf]])

        self.flush_pe_deferred()

        def rmsn(src, srcB, nk, gcol0, inv_n):
            for tb in range(NTB):
                bk, bb = self.allbanks.next()
                for c in range(nk):
                    s, sB = sqr.next()
                    self.act(s, src[:, c, tb * 512:(tb + 1) * 512], AF.Square, [srcB[c][tb]], [sB])
                    self.mm(bk[:, :], self.ones_b, s, c == 0, c == nk - 1, [sB, self.cbfB], [bb], signal=True)
                r, rB = rtr.next()
                self.act(r, bk[:, :], AF.Sqrt, [bb, self.cstB], [rB], scale=inv_n, bias=self.eps_ap)
                self.recip(r, r, [rB], [rB])
                for c in range(nk):
                    v = src[:, c, tb * 512:(tb + 1) * 512]
                    self.stt("dve", v, v, self.vecT[:, l, gcol0 + c:gcol0 + c + 1], r, ALU.mult, ALU.mult, [srcB[c][tb], rB, self.vecB],
                             [srcB[c][tb]])
        rmsn(cq, cqB, 6, 64, 1.0 / 768)
        rmsn(ckv, ckvB, 4, 70, 1.0 / 512)

        wuqv = self.wuq_d[l].rearrange("(k p) n -> p k n", p=128)
        wukvv = self.wukv_d[l].rearrange("(k p) n -> p k n", p=128)
        jobs = []
        for hp in range(4):
            jobs.append(("q", hp))
            jobs.append(("kv", hp))
        pend = []

        def issue2(i):
            kind, hp = jobs[i]
            if kind == "q":
                return self.wload([i % 4], wuqv[:, :, hp * 384:(hp + 1) * 384], 128, 6, 384)
            return self.wload([i % 4], wukvv[:, :, hp * 512:(hp + 1) * 512], 128, 4, 512)
        for i in range(3):
            pend.append(issue2(i))
        for i, (kind, hp) in enumerate(jobs):
            W, wb = pend.pop(0)
            if i + 3 < len(jobs):
                pend.append(issue2(i + 3))
            for hh in range(2):
                h_ = 2 * hp + hh
                if kind == "q":
                    self.proj_fm(cq, cqB, 6, 128, W, wb, hh * 192, 128, fm_store_handler(self.qT[h_, 0:128, :], self.qTB[h_], 128))
                    self.proj_fm(cq, cqB, 6, 128, W, wb, hh * 192 + 128, 64, rope_store_handler(self.qT[h_, 128:192, :], self.qTB[h_], 64, 1))
                else:
                    self.proj_fm(ckv, ckvB, 4, 128, W, wb, hh * 256, 128, fm_store_handler(self.knT[h_], self.knTB[h_], 128))
            self.flush_pe_deferred()
            if kind == "kv":
                Wv = W.rearrange("p k (h c) -> p k h c", c=256)[:, :, :, 128:256]
                for tt_ in range(NTT):
                    tb = tt_ // 4
                    bk, bb = self.allbanks.next()
                    for c in range(4):
                        self.mm(bk[:, 0:256].rearrange("p (h c) -> p h c", c=128), ckv[:, c, tt_ * 128:(tt_ + 1) * 128], Wv[:, c, :, :],
                                c == 0, c == 3, wb + [ckvB[c][tb]], [bb], signal=(c == 3))
                    sw, swB = stwr.next()
                    self.cp("act" if tt_ % 2 else "dve", sw[:, 0:256], bk[:, 0:256], [bb], [swB])
                    self.dma("sp", self.vm[tt_ * 128:(tt_ + 1) * 128, hp * 256:(hp + 1) * 256], sw[:, 0:256], [swB], [self.vmB[hp // 2]])

    def mla_attn(self, l):
        p = 0
        Ld = []
        for i in range(2):
            d_ = {}
            for nm, n in (("qn", SEQ), ("qr", SEQ), ("kn", SEQ), ("vh", NTT * 128)):
                v, p = self.ovv(p, n)
                d_[nm] = (v, Buf())
            Ld.append(d_)
        Et = []
        for i in range(6):
            v, p = self.ovv(p, 512)
            Et.append((v, Buf()))
        Er = Ring(Et)
        rcs = []
        for i in range(2):
            v, p = self.ovv(p, 512, F32)
            rcs.append((v, Buf()))
        rcr = Ring(rcs)
        ess = []
        for i in range(4):
            v, p = self.ovv(p, 512, F32)
            ess.append((v, Buf()))
        esr = Ring(ess)
        Sr = Ring([(self.pb[i], self.PB[i]) for i in (0, 1, 2)])
        Or = Ring([(self.pb[i], self.PB[i]) for i in (3, 4)])
        Dr = Ring([(self.pb[i], self.PB[i]) for i in (5, 6)])
        vmv = self.vm.rearrange("(t p) f -> p t f", p=128)
        scale = float(192 ** -0.5)

        def loads(h):
            d_ = Ld[h % 2]
            self.dma("sp", d_["qn"][0], self.qT[h, 0:128, :], [self.qTB[h]], [d_["qn"][1]])
            self.dma("sp", d_["qr"][0][0:64, :], self.qT[h, 128:192, :], [self.qTB[h]], [d_["qr"][1]])
            self.dma("sp", d_["kn"][0], self.knT[h], [self.knTB[h]], [d_["kn"][1]])
            self.dma("sp", d_["vh"][0].rearrange("p (t f) -> p t f", f=128), vmv[:, :, h * 128:(h + 1) * 128], [self.vmB[h // 4]], [d_["vh"][1]])
        loads(0)
        for h in range(8):
            if h + 1 < 8:
                loads(h + 1)
            d_ = Ld[h % 2]
            qn, qnB = d_["qn"]; qr, qrB = d_["qr"]; kn, knB = d_["kn"]; vh, vhB = d_["vh"]
            vh3 = vh.rearrange("p (t f) -> p t f", f=128)
            for qb in range(NTB):
                qs = slice(qb * 512, (qb + 1) * 512)
                ob, obB = Or.next()
                db, dbB = Dr.next()

                def smm(kt):
                    sb_, sbB = Sr.next()
                    ks = slice(kt * 128, (kt + 1) * 128)
                    self.mm(sb_[:, :], kn[:, ks], qn[:, qs], True, False, [knB, qnB], [sbB], signal=False)
                    self.mm(sb_[:, :], self.krT[0:64, ks], qr[0:64, qs], False, True, [self.krB[kt // 4], qrB], [sbB])
                    e, eB = Er.next()
                    self.act(e, sb_[:, :], AF.Exp, [sbB], [eB], scale=scale)
                    return e, eB
                esA, esAB = esr.next()
                esBt, esBB = esr.next()
                cur, nxt = smm(0), smm(1)
                for kt in range(NTT):
                    nn = smm(kt + 2) if kt + 2 < NTT else None
                    e, eB = cur
                    self.mm(ob[:, :], vh3[:, kt, :], e, kt == 0, kt == NTT - 1, [vhB, eB], [obB], signal=True)
                    if kt == 0:
                        self.cp("dve", esA, e, [eB], [esAB])
                    elif kt == 1:
                        self.cp("pool", esBt, e, [eB], [esBB])
                    elif kt % 2 == 0:
                        self.tt("dve", esA, esA, e, ALU.add, [eB, esAB], [esAB])
                    else:
                        self.tt("pool", esBt, esBt, e, ALU.add, [eB, esBB], [esBB])
                    cur, nxt = nxt, nn
                self.mm(db[:, :], self.onesf[:], esA, True, False, [esAB, self.onesfB], [dbB], signal=False)
                self.mm(db[:, :], self.onesf[:], esBt, False, True, [esBB, self.onesfB], [dbB])
                r, rB = rcr.next()
                self.recip(r, db[:, :], [dbB], [rB])
                self.tt("dve", self.RA[:, h, qs], ob[:, :], r, ALU.mult, [obB, rB], [self.RAb[h][qb]])
        self.dbg_dump_y(0)

    def dbg_dump_y(self, b, kp=128):
        if self.ydbg is None:
            return
        for k in range(KC):
            self.dma("sp", self.ydbg[b, k * 128:k * 128 + kp, :], self.RA[0:kp, k, :], [self.RAb[k][t] for t in range(NTB)], [self.dbgB])

    def branch_out(self, l, b):
        kp = 64 if b == 1 else 128
        if b == 0:
            src = self.wbm_d[l].rearrange("(k p) n -> p k n", p=128)
        elif b == 1:
            src = self.wbd_d[l].rearrange("(k p) n -> p k n", p=64)
        else:
            src = self.wbr_d[l].rearrange("(k p) n -> p k n", p=128)
        Wb, wbb = self._br_pre[b]
        last = (b == 2)
        if last:
            Wo, wob = self._wout_pre
        p = 0
        xi = []
        for i in range(2):
            v, p = self.ovv(p, KC * 512, F32)
            xi.append((v.rearrange("p (k t) -> p k t", k=KC), Buf()))
        xir = Ring(xi)
        mi = []
        for i in range(2):
            v, p = self.ovv(p, KC * 512, F32)
            mi.append((v.rearrange("p (k t) -> p k t", k=KC), Buf()))
        mir = Ring(mi)
        gts = []
        for i in range(2):
            v, p = self.ovv(p, KC * 512)
            gts.append((v.rearrange("p (k t) -> p k t", k=KC), Buf()))
        gtr = Ring(gts)
        ms = []
        for i in range(2):
            v, p = self.ovv(p, KC * 512)
            ms.append((v.rearrange("p (k t) -> p k t", k=KC), Buf()))
        msr = Ring(ms)
        tps = []
        for i in range(2):
            v, p = self.ovv(p, 512, F32)
            tps.append((v, Buf()))
        tpr = Ring(tps)
        xsv = self.xs.rearrange("k p t -> p k t")
        mgv = self.mg.rearrange("k p t -> p k t")
        gv = self.gT[b].rearrange("(o p) t -> p o t", p=128)
        gate1 = self.modT[:, l, 16:24]
        pre = {}

        def load(tb):
            ts_ = slice(tb * 512, (tb + 1) * 512)
            g, gB = gtr.next()
            self.dma("sp", g, gv[:, :, ts_], self.gTB[b], [gB])
            mgt, mgB = mir.next()
            if b > 0:
                self.dma("sp", mgt, mgv[:, :, ts_], [self.mgB[tb]], [mgB])
            x, xB = (None, None)
            if last:
                x, xB = xir.next()
                self.dma("sp", x, xsv[:, :, ts_], [self.xsB[tb]], [xB])
            pre[tb] = (x, xB, g, gB, mgt, mgB)
        load(0)
        for tb in range(NTB):
            if tb + 1 < NTB:
                load(tb + 1)
            x, xB, g, gB, mgt, mgB = pre.pop(tb)
            ts_ = slice(tb * 512, (tb + 1) * 512)
            if last:
                m, mB = msr.next()
            for o in range(KC):
                bk, bb = self.allbanks.next()
                for kc in range(KC):
                    self.mm(bk[:, :], Wb[0:kp, kc, o * 128:(o + 1) * 128], self.RA[0:kp, kc, ts_], kc == 0, kc == KC - 1,
                            wbb + [self.RAb[kc][tb]], [bb], signal=(kc == KC - 1))
                if b == 0:
                    self.tt("dve", mgt[:, o, :], bk[:, :], g[:, o, :], ALU.mult, [bb, gB], [mgB])
                else:
                    t_, tB = tpr.next()
                    self.tt("dve", t_, bk[:, :], g[:, o, :], ALU.mult, [bb, gB], [tB])
                    if last:
                        self.tt("pool", m[:, o, :], t_, mgt[:, o, :], ALU.add, [tB, mgB], [mB])
                    else:
                        self.tt("pool", mgt[:, o, :], t_, mgt[:, o, :], ALU.add, [tB, mgB], [mgB])
            if not last:
                self.dma("sp", mgv[:, :, ts_], mgt, [mgB], [self.mgB[tb]])
                continue
            for o2 in range(KC):
                bk, bb = self.allbanks.next()
                for o in range(KC):
                    self.mm(bk[:, :], Wo[:, o, o2 * 128:(o2 + 1) * 128], m[:, o, :], o == 0, o == KC - 1, wob + [mB], [bb], signal=(o == KC - 1))
                self.stt("dve", x[:, o2, :], bk[:, :], gate1[:, o2:o2 + 1], x[:, o2, :], ALU.mult, ALU.add, [bb, xB, self.modB], [xB])
            self.dma("sp", xsv[:, :, ts_], x, [xB], [self.xsB[tb]])

    def dil_attn(self, l):
        p = 0
        va = []
        for g in range(3):
            v, p = self.ovv(p, NTT * 520)
            va.append((v.rearrange("p (t f) -> p t f", f=520), Buf()))
            self.dma("sp", va[g][0], self.dv[g].rearrange("(t p) f -> p t f", p=128), [self.dvB[g]], [va[g][1]])
        qk = []
        for g in range(3):
            q_, p = self.ovv(p, SEQ)
            k_, p = self.ovv(p, SEQ)
            qk.append((q_, Buf(), k_, Buf()))
        accs = []
        for i in range(2):
            v, p = self.ovv(p, SEQ, F32)
            accs.append((v, Buf()))
        Et = []
        for i in range(4):
            v, p = self.ovv(p, 384)
            Et.append((v, Buf()))
        Er = Ring(Et)
        rrv, p = self.ovv(p, SEQ, F32)
        rrB = Buf()
        bcs = []
        for i in range(2):
            v, p = self.ovv(p, 512, F32)
            bcs.append((v, Buf()))
        bcr = Ring(bcs)
        Sr = Ring([(self.pb[i], self.PB[i]) for i in (0, 1, 2)])
        Or = Ring([(self.pb[i], self.PB[i]) for i in (3, 4, 5)])
        Br = Ring([(self.pb[i], self.PB[i]) for i in (6, 7)])

        def loads(hp):
            for g in range(3):
                q_, qB, k_, kB = qk[g]
                self.dma("sp", q_, self.dqT[g, hp * 128:(hp + 1) * 128, :], [self.dqTB[g][hp]], [qB])
                self.dma("sp", k_, self.dkT[g, hp * 128:(hp + 1) * 128, :], [self.dkTB[g][hp]], [kB])
        for hp in range(4):
            loads(hp)
            for hh in range(2):
                h = 2 * hp + hh
                rows = slice(64 * hh, 64 * hh + 64)
                acc, accB = accs[hh]
                for g in range(3):
                    d = DIL_D[g]
                    TPR = (SEQ // d) // 128
                    q_, qB, k_, kB = qk[g]
                    vg, vgB = va[g]

                    def s_stage(i):
                        nbrs = [n for n in (i - 1, i, i + 1) if 0 <= n < NTT and n // TPR == i // TPR]
                        sb_, sbB = Sr.next()
                        for idx, n in enumerate(nbrs):
                            c0 = (n - i + 1) * 128
                            self.mm(sb_[:, c0:c0 + 128], k_[rows, n * 128:(n + 1) * 128], q_[rows, i * 128:(i + 1) * 128], idx == 0, False,
                                    [kB, qB], [sbB], signal=False)
                        lo = (nbrs[0] - i + 1) * 128
                        hi = (nbrs[-1] - i + 2) * 128
                        self.mm(sb_[:, lo:hi], self.ident_b, self.negm_b[:, lo:hi], False, True, [self.cbfB], [sbB], signal=True)
                        e, eB = Er.next()
                        self.act(e[:, lo:hi], sb_[:, lo:hi], AF.Exp, [sbB], [eB], scale=0.125)
                        return (e, eB, nbrs)
                    stages = {0: s_stage(0), 1: s_stage(1)}
                    for b4 in range(4):
                        ob, obB = Or.next()
                        for ib in range(4):
                            i = 4 * b4 + ib
                            if i + 2 < NTT:
                                stages[i + 2] = s_stage(i + 2)
                            e, eB, nbrs = stages.pop(i)
                            for n in nbrs:
                                c0 = (n - i + 1) * 128
                                self.mm(ob[0:65, ib * 128:(ib + 1) * 128], vg[:, n, h * 65:(h + 1) * 65], e[:, c0:c0 + 128], n == nbrs[0], n == nbrs[-1],
                                        [vgB, eB], [obB], signal=(n == nbrs[-1]))
                        if g == 0:
                            self.cp("dve", acc[0:65, b4 * 512:(b4 + 1) * 512], ob[0:65, :], [obB], [accB])
                        elif g == 1:
                            dst = acc[0:65, :].rearrange("p (j r) -> p r j", r=4)[:, b4, :]
                            self.tt("dve", dst, dst, ob[0:65, :], ALU.add, [obB, accB], [accB])
                        else:
                            dst = acc[0:65, :].rearrange("p (j r) -> p r j", r=16)[:, 4 * b4:4 * b4 + 4, :]
                            self.tt("dve", dst, dst, ob[0:65, :].rearrange("p (b j) -> p b j", b=4), ALU.add, [obB, accB], [accB])
                self.act(rrv[64:65, :], acc[64:65, :], AF.Ln, [accB], [rrB])
                self.act(rrv[64:65, :], rrv[64:65, :], AF.Exp, [rrB], [rrB], scale=-1.0)
                for tb in range(NTB):
                    ts_ = slice(tb * 512, (tb + 1) * 512)
                    bk, bb = Br.next()
                    self.mm(bk[0:64, :], self.onesf[64:65, 0:64], rrv[64:65, ts_], True, True, [rrB, self.onesfB], [bb])
                    bc, bcB = bcr.next()
                    self.cp("act", bc[0:64, :], bk[0:64, :], [bb], [bcB])
                    self.tt("dve", self.RA[0:64, h, ts_], acc[0:64, ts_], bc[0:64, :], ALU.mult, [accB, bcB], [self.RAb[h][tb]])
        self.dbg_dump_y(1, 64)

    def ret_attn(self, l):
        p = 0
        dpos, p = self.ovv(p, STRIPW, F32)
        dneg, p = self.ovv(p, STRIPW, F32)
        dB = Buf()
        self.dma("sp", dpos, self.dpn_d[0], [], [dB])
        self.dma("sp", dneg, self.dpn_d[1], [], [dB])
        HW_ = STRIPW // 2
        ef, p = self.ovv(p, HW_)
        eb, p = self.ovv(p, HW_)
        efB, ebB = Buf(), Buf()
        strips = []
        for i in range(2):
            v, p = self.ovv(p, STRIPW)
            strips.append((v, Buf()))
        qk = []
        for i in range(2):
            q_, p = self.ovv(p, SEQ)
            k_, p = self.ovv(p, SEQ)
            qk.append((q_, Buf(), k_, Buf()))
        hv = []
        for i in range(2):
            v_, p = self.ovv(p, NTT * 128)
            g_, p = self.ovv(p, SEQ)
            hv.append((v_.rearrange("p (t f) -> p t f", f=128), Buf(), g_, Buf()))
        SD = []
        for i in range(6):
            v, p = self.ovv(p, 512)
            SD.append((v, Buf()))
        SDr = Ring(SD)
        SC = []
        for i in range(3):
            v, p = self.ovv(p, 512)
            SC.append((v, Buf()))
        SCr = Ring(SC)
        ysbs, sqs = [], []
        for i in range(2):
            v, p = self.ovv(p, 512, F32)
            ysbs.append((v, Buf()))
            v, p = self.ovv(p, 512, F32)
            sqs.append((v, Buf()))
        ysbr, sqr_ = Ring(ysbs), Ring(sqs)
        tmp = {}
        for nm in ("msq", "var", "rstd"):
            v, p = self.ovv(p, 512, F32)
            tmp[nm] = (v, Buf())
        Sr = Ring([(self.pb[i], self.PB[i]) for i in (0, 1, 2)])
        Yr = Ring([(self.pb[i], self.PB[i]) for i in (3, 4)])
        Mr = Ring([(self.pb[i], self.PB[i]) for i in (5,)])
        Vb, VbB = self.pb[6], self.PB[6]
        dmy, dmyB = self.pb[7], self.PB[7]
        rvv = self.rv.rearrange("(t p) f -> p t f", p=128)

        def loads_pair(hp):
            q_, qB, k_, kB = qk[hp % 2]
            self.dma("sp", q_, self.rqT[hp * 128:(hp + 1) * 128, :], [self.rqTB[hp]], [qB])
            self.dma("sp", k_, self.rkT[hp * 128:(hp + 1) * 128, :], [self.rkTB[hp]], [kB])

        def loads_head(h):
            v_, vB, g_, gB = hv[h % 2]
            self.dma("sp", v_, rvv[:, :, h * 128:(h + 1) * 128], [self.rvB[h // 4]], [vB])
            self.dma("sp", g_, self.rgT[h * 128:(h + 1) * 128, :], [self.rgTB[h]], [gB])

        def strip_steps(h):
            strip, stripB = strips[h % 2]
            lgf = self.rldT[:, l * 16 + h:l * 16 + h + 1]
            nlgb = self.rldT[:, l * 16 + 8 + h:l * 16 + 8 + h + 1]
            steps = []
            for half in range(2):
                cs = slice(half * HW_, (half + 1) * HW_)
                steps.append(lambda cs=cs: self.act(ef, dpos[:, cs], AF.Exp, [dB, self.rldB], [efB], scale=lgf))
                steps.append(lambda cs=cs: self.act(eb, dneg[:, cs], AF.Exp, [dB, self.rldB], [ebB], scale=nlgb))
                steps.append(lambda cs=cs: self.tt("dve", strip[:, cs], ef, eb, ALU.mult, [efB, ebB], [stripB]))
            return steps

        def stats_steps(h, qb, yb, ybB, g_, gB):
            qs = slice(qb * 512, (qb + 1) * 512)
            ysb, ysbB = ysbr.next()
            sq, sqB = sqr_.next()
            msq, msqB = tmp["msq"]; var, varB = tmp["var"]; rstd, rstdB = tmp["rstd"]
            st = {}

            def s3():
                st["mb"], st["mbB"] = Mr.next()
                self.mm(st["mb"][:, :], self.avgf[:], ysb, True, True, [ysbB, self.onesfB], [st["mbB"]])
                self.mm(Vb[:, :], self.avgf[:], sq, True, True, [sqB, self.onesfB], [VbB])
            return [
                lambda: self.cp("act", ysb, yb[:, :], [ybB], [ysbB]),
                lambda: self.act(sq, yb[:, :], AF.Square, [ybB], [sqB]),
                s3,
                lambda: self.act(msq, st["mb"][:, :], AF.Square, [st["mbB"]], [msqB]),
                lambda: self.tt("dve", var, Vb[:, :], msq, ALU.subtract, [VbB, msqB], [varB]),
                lambda: self.ts("dve", var, var, 0.0, EPS, ALU.max, ALU.add, [varB], [varB]),
                lambda: self.act(rstd, var, AF.Ln, [varB], [rstdB]),
                lambda: self.act(rstd, rstd, AF.Exp, [rstdB], [rstdB], scale=-0.5),
                lambda: self.tt("dve", ysb, ysb, st["mb"][:, :], ALU.subtract, [ysbB, st["mbB"]], [ysbB]),
                lambda: self.tt("pool", ysb, ysb, rstd, ALU.mult, [ysbB, rstdB], [ysbB]),
                lambda: self.stt("dve", self.RA[:, h, qs], ysb, self.vecT[:, l, 74 + h:75 + h], g_[:, qs], ALU.mult, ALU.mult,
                                 [ysbB, self.vecB, gB], [self.RAb[h][qb]]),
            ]
        loads_pair(0)
        loads_head(0)
        for s in strip_steps(0):
            s()
        pending = []
        for h in range(8):
            hp, hh = h // 2, h % 2
            if hh == 0 and hp + 1 < 4:
                loads_pair(hp + 1)
            rows = slice(64 * hh, 64 * hh + 64)
            q_, qB, k_, kB = qk[hp % 2]
            v_, vB, g_, gB = hv[h % 2]
            strip, stripB = strips[h % 2]
            for qb in range(NTB):
                qs = slice(qb * 512, (qb + 1) * 512)
                yb, ybB = Yr.next()
                if qb == 1 and h + 1 < 8:
                    loads_head(h + 1)
                    pending.extend(strip_steps(h + 1))

                def smm(kt):
                    sb_, sbB = Sr.next()
                    self.mm(sb_[:, :], k_[rows, kt * 128:(kt + 1) * 128], q_[rows, qs], True, True, [kB, qB], [sbB])
                    sd, sdB = SDr.next()
                    off = 512 * qb - 128 * kt + 1920
                    if kt % 2 == 0:
                        self.tt("dve", sd, sb_[:, :], strip[:, off:off + 512], ALU.mult, [sbB, stripB], [sdB])
                    else:
                        sc, scB = SCr.next()
                        self.cp("act", sc, sb_[:, :], [sbB], [scB])
                        self.tt("dve", sd, sc, strip[:, off:off + 512], ALU.mult, [scB, stripB], [sdB])
                    return sd, sdB
                cur, nxt = smm(0), smm(1)
                for kt in range(NTT):
                    nn = smm(kt + 2) if kt + 2 < NTT else None
                    sd, sdB = cur
                    self.mm(yb[:, :], v_[:, kt, :], sd, kt == 0, kt == NTT - 1, [vB, sdB], [ybB], signal=True)
                    cur, nxt = nxt, nn
                    if pending:
                        pending.pop(0)()
                while pending:
                    pending.pop(0)()
                pending.extend(stats_steps(h, qb, yb, ybB, g_, gB))
        while pending:
            pending.pop(0)()
        self.dbg_dump_y(2)

    def ffn(self, l):
        moe = (l % 2 == 1)
        li = l // 2
        NF = (D_FFE if moe else D_FF) // 128
        nexp = NEXP if moe else 1
        p0 = 0
        gTt, p0 = self.ovv(p0, NF * 1024)
        gTt = gTt.rearrange("p (f t) -> p f t", t=1024)
        gB = [[Buf() for _ in range(2)] for _ in range(NF)]
        facc, p0 = self.ovv(p0, KC * 1024, F32)
        facc = facc.rearrange("p (k t) -> p k t", t=1024)
        faccB = [[Buf() for _ in range(2)] for _ in range(KC)]
        gate2 = self.modT[:, l, 40:48]
        router = None
        if moe:
            wr, p0 = self.ovv(p0, KC * 8, F32)
            wr = wr.rearrange("p (k e) -> p k e", e=8)
            wrB = Buf()
            self.dma("sp", wr, self.mr_d[li].rearrange("(k p) e -> p k e", p=128), [], [wrB])
            lg, p0 = self.ovv(p0, 64, F32)
            lg = lg.rearrange("p (a e) -> p a e", e=8)
            lgB = Buf()
            router = {"w": (wr, wrB), "lg": (lg, lgB)}
            small = {}
            for nm, n in (("m1", 8), ("m2", 8), ("eq1", 64), ("eq2", 64), ("lg2", 64), ("dm", 8), ("w1", 8), ("w2", 8), ("wts", 64)):
                v, p0 = self.ovv(p0, n, F32)
                small[nm] = v
            smB = Buf()
            dg, p0 = self.ovv(p0, 128, F32)
            dgs = [(dg, Buf())]
            v, p0 = self.ovv(p0, 128, F32)
            dgs.append((v, Buf()))
            dgr = Ring(dgs)
            wbt, p0 = self.ovv(p0, NEXP * 1024)
            wbt = wbt.rearrange("p (e t) -> p e t", t=1024)
            wbB = [Buf() for _ in range(NEXP)]
        tms = []
        for i in range(2):
            s_, p0 = self.ovv(p0, 512, F32)
            g0, p0 = self.ovv(p0, 512)
            tms.append((s_, Buf(), g0, Buf()))
        tmr = Ring(tms)
        pn = 0

        steps = [(f0, min(4, NF - f0)) for f0 in range(0, NF, 4)]

        def wviews(ex):
            if moe:
                return (self.m1_d[li, ex].rearrange("(k p) n -> p k n", p=128), self.m3_d[li, ex].rearrange("(k p) n -> p k n", p=128),
                        self.m2_d[li, ex].rearrange("(f p) n -> p f n", p=128))
            return (self.f1_d[li].rearrange("(k p) n -> p k n", p=128), self.f3_d[li].rearrange("(k p) n -> p k n", p=128),
                    self.f2_d[li].rearrange("(f p) n -> p f n", p=128))
        jobs = []
        jidx = {}
        for ex in range(nexp):
            w1v, w3v, w2v = wviews(ex)
            for i, (f0, nf) in enumerate(steps):
                jidx[("f1", ex, i)] = len(jobs)
                jobs.append([(w1v[:, :, f0 * 128:(f0 + nf) * 128], KC, nf * 128), (w3v[:, :, f0 * 128:(f0 + nf) * 128], KC, nf * 128)])
            for o in range(KC):
                jidx[("f2", ex, o)] = len(jobs)
                jobs.append([(w2v[:, :, o * 128:(o + 1) * 128], NF, 128)])

        for hf in range(2):
            tbs = [2 * hf, 2 * hf + 1]
            ws = WStream(self, jobs)
            ws._try_issue()
            self.norm_phase(l, 1, tbs, pn, router=router)
            if moe:
                lg3 = lg
                m1, m2, eq1, eq2, lg2, dm, w1, w2, wts = (small[k] for k in ("m1", "m2", "eq1", "eq2", "lg2", "dm", "w1", "w2", "wts"))
                e3 = lambda v: v.rearrange("p (a e) -> p a e", e=8)
                b3 = lambda v: v.unsqueeze(2).to_broadcast([128, 8, 8])
                self.S.op("dve", lambda e: e.tensor_reduce(out=m1, in_=lg3, axis=mybir.AxisListType.X, op=ALU.max), [lgB], [smB])
                self.tt("dve", e3(eq1), lg3, b3(m1), ALU.is_ge, [lgB, smB], [smB])
                self.stt("dve", e3(lg2), e3(eq1), -1e30, lg3, ALU.mult, ALU.add, [smB, lgB], [smB])
                self.S.op("dve", lambda e: e.tensor_reduce(out=m2, in_=e3(lg2), axis=mybir.AxisListType.X, op=ALU.max), [smB], [smB])
                self.tt("dve", e3(eq2), e3(lg2), b3(m2), ALU.is_ge, [smB], [smB])
                self.tt("dve", dm, m1, m2, ALU.subtract, [smB], [smB])
                self.act(w1, dm, AF.Sigmoid, [smB], [smB])
                self.act(w2, dm, AF.Sigmoid, [smB], [smB], scale=-1.0)
                self.tt("dve", e3(eq1), e3(eq1), b3(w1), ALU.mult, [smB], [smB])
                self.tt("dve", e3(eq2), e3(eq2), b3(w2), ALU.mult, [smB], [smB])
                self.tt("dve", e3(wts), e3(eq1), e3(eq2), ALU.add, [smB], [smB])
                for ex in range(NEXP):
                    for a4 in range(2):
                        bk, bb = self.allbanks.next()
                        for a in range(4):
                            ta = a4 * 4 + a
                            dgt, dgB = dgr.next()
                            self.ts("dve", dgt, self.ident_f, e3(wts)[:, ta, ex:ex + 1], None, ALU.mult, None, [smB, self.cstB], [dgB])
                            self.mm(bk[:, a * 128:(a + 1) * 128], self.onesf[:], dgt, True, True, [dgB, self.onesfB], [bb])
                        self.cp("act", wbt[:, ex, a4 * 512:(a4 + 1) * 512], bk[:, :], [bb], [wbB[ex]])
            self.S.barrier()
            for ex in range(nexp):
                if moe:
                    w1v = self.m1_d[li, ex].rearrange("(k p) n -> p k n", p=128)
                    w3v = self.m3_d[li, ex].rearrange("(k p) n -> p k n", p=128)
                    w2v = self.m2_d[li, ex].rearrange("(f p) n -> p f n", p=128)
                else:
                    w1v = self.f1_d[li].rearrange("(k p) n -> p k n", p=128)
                    w3v = self.f3_d[li].rearrange("(k p) n -> p k n", p=128)
                    w2v = self.f2_d[li].rearrange("(f p) n -> p f n", p=128)
                for i, (f0, nf) in enumerate(steps):
                    jn = jidx[("f1", ex, i)]
                    (W1, w1b), (W3, w3b) = ws.get(jn)
                    for j in range(nf):
                        f = f0 + j
                        for t2 in range(2):
                            tb = tbs[t2]
                            ts_ = slice(tb * 512, (tb + 1) * 512)
                            b1, b1B = self.allbanks.next()
                            b3_, b3B = self.allbanks.next()
                            for kc in range(KC):
                                self.mm(b1[:, :], W1[:, kc, j * 128:(j + 1) * 128], self.RA[:, kc, ts_], kc == 0, kc == KC - 1,
                                        w1b + [self.RAb[kc][tb]], [b1B], signal=(kc == KC - 1))
                            for kc in range(KC):
                                self.mm(b3_[:, :], W3[:, kc, j * 128:(j + 1) * 128], self.RA[:, kc, ts_], kc == 0, kc == KC - 1,
                                        w3b + [self.RAb[kc][tb]], [b3B], signal=(kc == KC - 1))
                            s_, sB, g0, g0B = tmr.next()
                            self.act(s_, b1[:, :], AF.Silu, [b1B], [sB])
                            gd = gTt[:, f, t2 * 512:(t2 + 1) * 512]
                            if moe:
                                self.tt("dve", g0, b3_[:, :], s_, ALU.mult, [b3B, sB], [g0B])
                                self.tt("pool", gd, g0, wbt[:, ex, t2 * 512:(t2 + 1) * 512], ALU.mult, [g0B, wbB[ex]], [gB[f][t2]])
                            else:
                                self.tt("dve", gd, b3_[:, :], s_, ALU.mult, [b3B, sB], [gB[f][t2]])
                    ws.finish(jn)
                for o in range(KC):
                    jn = jidx[("f2", ex, o)]
                    ((W2, w2b),) = ws.get(jn)
                    for t2 in range(2):
                        bk, bb = self.allbanks.next()
                        for f in range(NF):
                            self.mm(bk[:, :], W2[:, f, :], gTt[:, f, t2 * 512:(t2 + 1) * 512], f == 0, f == NF - 1, w2b + [gB[f][t2]], [bb],
                                    signal=(f == NF - 1))
                        fa = facc[:, o, t2 * 512:(t2 + 1) * 512]
                        if ex == 0:
                            self.cp("act", fa, bk[:, :], [bb], [faccB[o][t2]])
                        else:
                            self.tt("dve", fa, fa, bk[:, :], ALU.add, [bb, faccB[o][t2]], [faccB[o][t2]])
                    ws.finish(jn)
            xsv = self.xs.rearrange("k p t -> p k t")
            pp = pn
            xi = []
            for i in range(2):
                v, pp = self.ovv(pp, KC * 512, F32)
                xi.append((v.rearrange("p (k t) -> p k t", k=KC), Buf()))
            self.S.barrier()
            for t2 in range(2):
                tb = tbs[t2]
                x, xB = xi[t2]
                self.dma("sp", x, xsv[:, :, tb * 512:(tb + 1) * 512], [self.xsB[tb]], [xB])
                for o in range(KC):
                    self.stt("dve", x[:, o, :], facc[:, o, t2 * 512:(t2 + 1) * 512], gate2[:, o:o + 1], x[:, o, :], ALU.mult, ALU.add,
                             [faccB[o][t2], xB, self.modB], [xB])
                self.dma("sp", xsv[:, :, tb * 512:(tb + 1) * 512], x, [xB], [self.xsB[tb]])
            self.S.barrier()

    def final(self):
        p = 0
        xi = []
        for i in range(2):
            v, p = self.ovv(p, KC * 512, F32)
            xi.append((v.rearrange("p (k t) -> p k t", k=KC), Buf()))
        xir = Ring(xi)
        sq = []
        for i in range(2):
            v, p = self.ovv(p, 512)
            sq.append((v, Buf()))
        sqr = Ring(sq)
        rt = []
        for i in range(2):
            v, p = self.ovv(p, 512, F32)
            rt.append((v, Buf()))
        rtr = Ring(rt)
        tm = []
        for i in range(3):
            v, p = self.ovv(p, 512, F32)
            tm.append((v, Buf()))
        tmr = Ring(tm)
        ob = []
        for i in range(2):
            v, p = self.ovv(p, 4 * D, F32)
            ob.append((v.rearrange("p (a k m) -> p a k m", a=4, k=KC), Buf()))
        obr = Ring(ob)
        xsv = self.xs.rearrange("k p t -> p k t")
        ov = self.out_d.rearrange("(a p) d -> p a d", p=128)
        for tb in range(NTB):
            x, xB = xir.next()
            self.dma("sp", x, xsv[:, :, tb * 512:(tb + 1) * 512], [self.xsB[tb]], [xB])
            bk, bb = self.allbanks.next()
            for kc in range(KC):
                s, sB = sqr.next()
                self.act(s, x[:, kc, :], AF.Square, [xB], [sB])
                self.mm(bk[:, :], self.ones_b, s, kc == 0, kc == KC - 1, [sB, self.cbfB], [bb], signal=True)
            r, rB = rtr.next()
            self.act(r, bk[:, :], AF.Sqrt, [bb, self.cstB], [rB], scale=1.0 / D, bias=self.eps_ap)
            self.recip(r, r, [rB], [rB])
            o_, oB = obr.next()
            for kc in range(KC):
                t, tB = tmr.next()
                self.stt("dve", t, x[:, kc, :], self.finT[:, kc:kc + 1], r, ALU.mult, ALU.mult, [xB, rB, self.vecB], [tB])
                b2, b2B = self.allbanks.next()
                for a in range(4):
                    self.tr(b2[:, a * 128:(a + 1) * 128], t[:, a * 128:(a + 1) * 128], self.ident_f, [tB, self.cstB], [b2B])
                self.cp("act", o_[:, :, kc, :], b2[:, :].rearrange("p (a m) -> p a m", a=4), [b2B], [oB])
            self.dma("sp", ov[:, 4 * tb:4 * tb + 4, :], o_.rearrange("p a k m -> p a (k m)"), [oB], [self.outB[tb]])


_CACHE = {}


def _vecpack(inputs):
    vp = np.zeros((DEPTH, NVEC, 128), np.float32)
    for l in range(DEPTH):
        vp[l, 0:48] = np.asarray(inputs["ada_b"][l], np.float32).reshape(48, 128)
        vp[l, 48:56] = np.asarray(inputs["norm_mix"][l], np.float32).reshape(8, 128)
        vp[l, 56:64] = np.asarray(inputs["norm_ffn"][l], np.float32).reshape(8, 128)
        vp[l, 64:70] = np.asarray(inputs["mla_q_norm"][l], np.float32).reshape(6, 128)
        vp[l, 70:74] = np.asarray(inputs["mla_kv_norm"][l], np.float32).reshape(4, 128)
        vp[l, 74:82] = np.asarray(inputs["ret_norm"][l], np.float32).reshape(8, 128)
    return vp


def make_in_maps(inputs, ncores=NCORES):
    cst, dpn = make_consts()
    f = lambda k: np.ascontiguousarray(np.asarray(inputs[k], np.float32))
    shared = {
        "ada_w": f("ada_w"), "vecpack": _vecpack(inputs), "final_norm": f("final_norm").reshape(8, 128),
        "w_in": f("w_in"), "mla_w_uq": f("mla_w_uq"), "mla_w_ukv": f("mla_w_ukv"),
        "ret_log_decay": f("ret_log_decay").reshape(1, DEPTH * 16),
        "w_br_mla": f("w_br_mla"), "w_br_dil": f("w_br_dil"), "w_br_ret": f("w_br_ret"), "w_out": f("w_out"),
        "ffn_w1": f("ffn_w1"), "ffn_w3": f("ffn_w3"), "ffn_w2": f("ffn_w2"),
        "moe_router": f("moe_router"), "moe_w1": f("moe_w1"), "moe_w3": f("moe_w3"), "moe_w2": f("moe_w2"),
        "consts": cst, "dposneg": dpn,
    }
    x = f("x")
    c = f("c")
    pos = np.ascontiguousarray(np.asarray(inputs["positions"], np.int32))
    maps = []
    for b in range(ncores):
        m = dict(shared)
        m["x"] = x[b]
        m["c"] = c[b:b + 1]
        m["positions"] = pos[b:b + 1]
        maps.append(m)
    return maps


def kernel(**inputs):
    if "nc" not in _CACHE:
        _CACHE["nc"] = Builder().build()
    nc = _CACHE["nc"]
    maps = make_in_maps(inputs)
    res = run_bass_kernel_spmd(nc, maps, core_ids=list(range(NCORES)))
    return np.stack([np.asarray(r["out"], np.float32) for r in res.results], axis=0)
```

```python
import numpy as np
from contextlib import ExitStack
import concourse.bass as bass
import concourse.mybir as mybir
from concourse.bass_utils import run_bass_kernel_spmd

F32 = mybir.dt.float32
BF16 = mybir.dt.bfloat16
I32 = mybir.dt.int32
AF = mybir.ActivationFunctionType
ALU = mybir.AluOpType

D = 1024
SEQ = 2048
DEPTH = 4
NCORES = 8
KC = 8
NTB = 4
NTT = 16
EPS = 1e-6
IN_COLS = 12096
C_CQ, C_CKV, C_KR, C_DQ, C_DK, C_DV, C_RQ, C_RK, C_RV, C_RG, C_GA, C_GB, C_GC = (
    0, 768, 1280, 1344, 2880, 4416, 5952, 6464, 6976, 8000, 9024, 10048, 11072)
DIL_D = (1, 4, 16)
D_FF = 2816
D_FFE = 3584
NEXP = 8
STRIPW = 3968
NVEC = 82

SEM_LIMIT = 30000
DMA_SEMS_PER_QUEUE = 20


class Buf:
    __slots__ = ("name", "w", "r")

    def __init__(self, name=""):
        self.name = name
        self.w = {}
        self.r = {}


class Sched:
    ENGS = ("pe", "act", "dve", "pool", "sp")

    def __init__(self, nc, stack, nsem=96):
        self.nc = nc
        self.sems = [stack.enter_context(nc.semaphore(f"s{i}")) for i in range(nsem)]
        self.sem_next = 0
        self.streams = {e: [] for e in self.ENGS}
        self.cur = {}
        for e in self.ENGS:
            self.cur[e] = [self._new_sem(), 0]
        self.seen = {e: {} for e in self.ENGS}
        self.dq = {}
        for q in ("sp", "pool"):
            self.dq[q] = {"sems": [[self._new_sem(), 0] for _ in range(DMA_SEMS_PER_QUEUE)], "next": 0}
        self.n_ins = 0

    def _new_sem(self):
        i = self.sem_next
        self.sem_next += 1
        assert i < len(self.sems), "out of semaphores"
        return i

    def _wait(self, eng, sidx, val):
        if self.seen[eng].get(sidx, 0) >= val:
            return
        self.seen[eng][sidx] = val
        sem = self.sems[sidx]
        self.streams[eng].append(lambda e, sem=sem, val=val: e.wait_ge(sem, val))

    def _collect(self, eng, reads, writes, same_engine_ok=False):
        deps = {}
        for b in reads:
            for s, v in b.w.items():
                if deps.get(s, 0) < v:
                    deps[s] = v
        for b in writes:
            for s, v in b.w.items():
                if deps.get(s, 0) < v:
                    deps[s] = v
            for s, v in b.r.items():
                if deps.get(s, 0) < v:
                    deps[s] = v
        own = self.cur[eng][0]
        for s, v in deps.items():
            if same_engine_ok and s == own:
                continue
            self._wait(eng, s, v)

    def _mark(self, ticket, reads, writes):
        s, v = ticket
        for b in reads:
            if b.r.get(s, 0) < v:
                b.r[s] = v
        for b in writes:
            b.w = {s: v}
            b.r = {}

    def op(self, eng, fn, reads=(), writes=(), signal=True):
        self._collect(eng, reads, writes, same_engine_ok=(eng == "pe"))
        cur = self.cur[eng]
        if signal:
            if cur[1] >= SEM_LIMIT:
                cur[0] = self._new_sem()
                cur[1] = 0
            cur[1] += 1
            sem = self.sems[cur[0]]
            self.streams[eng].append(lambda e, fn=fn, sem=sem: fn(e).then_inc(sem, 1))
            ticket = (cur[0], cur[1])
        else:
            assert eng == "pe"
            if cur[1] + 1 > SEM_LIMIT:
                cur[0] = self._new_sem()
                cur[1] = 0
            self.streams[eng].append(lambda e, fn=fn: fn(e))
            ticket = (cur[0], cur[1] + 1)
        self._mark(ticket, reads, writes)
        self.n_ins += 1
        return ticket

    def dma(self, q, fns, reads=(), writes=()):
        if not isinstance(fns, (list, tuple)):
            fns = [fns]
        self._collect(q, reads, writes)
        pool = self.dq[q]
        slot = pool["sems"][pool["next"]]
        pool["next"] = (pool["next"] + 1) % len(pool["sems"])
        if slot[1] > 0:
            self._wait(q, slot[0], slot[1])
        if slot[1] + 16 * len(fns) > SEM_LIMIT:
            slot[0] = self._new_sem()
            slot[1] = 0
        sem = self.sems[slot[0]]
        for fn in fns:
            slot[1] += 16
            self.streams[q].append(lambda e, fn=fn, sem=sem: fn(e).then_inc(sem, 16))
        ticket = (slot[0], slot[1])
        self._mark(ticket, reads, writes)
        self.n_ins += len(fns)
        return ticket

    def barrier(self):
        tickets = []
        for e in self.ENGS:
            c = self.cur[e]
            if c[1] > 0:
                tickets.append((c[0], c[1]))
        for q in self.dq.values():
            for s in q["sems"]:
                if s[1] > 0:
                    tickets.append((s[0], s[1]))
        for e in self.ENGS:
            for s, v in tickets:
                if s == self.cur[e][0] and e == "pe":
                    continue
                self._wait(e, s, v)

    def wait_all(self, eng, bufs):
        for b in bufs:
            for s, v in b.w.items():
                self._wait(eng, s, v)

    def emit(self):
        nc = self.nc
        streams = self.streams
        with nc.Block() as block:
            @block.tensor
            def _(e):
                for f in streams["pe"]:
                    f(e)

            @block.scalar
            def _(e):
                for f in streams["act"]:
                    f(e)

            @block.vector
            def _(e):
                for f in streams["dve"]:
                    f(e)

            @block.gpsimd
            def _(e):
                for f in streams["pool"]:
                    f(e)

            @block.sync
            def _(e):
                for f in streams["sp"]:
                    f(e)


class WStream:
    def __init__(self, bld, jobs):
        self.b = bld
        self.jobs = jobs
        self.views = {}
        self.nxt = 0
        self.done = -1
        self.slot_ctr = 0
        self.slot_job = [-1, -1, -1, -1]

    def _try_issue(self):
        while self.nxt < len(self.jobs):
            need = len(self.jobs[self.nxt])
            slots = [(self.slot_ctr + i) % 4 for i in range(need)]
            if any(self.slot_job[s_] > self.done for s_ in slots):
                return
            vs = []
            for s_, (src, nk, ncols) in zip(slots, self.jobs[self.nxt]):
                vs.append(self.b.wload([s_], src, 128, nk, ncols))
                self.slot_job[s_] = self.nxt
            self.slot_ctr = (self.slot_ctr + need) % 4
            self.views[self.nxt] = vs
            self.nxt += 1

    def get(self, j):
        self._try_issue()
        assert j in self.views, (j, self.nxt, self.done, self.slot_job)
        return self.views.pop(j)

    def finish(self, j):
        self.done = j
        self._try_issue()


class Ring:
    def __init__(self, items):
        self.items = items
        self.i = 0

    def next(self):
        it = self.items[self.i]
        self.i = (self.i + 1) % len(self.items)
        return it


def make_consts():
    c = np.zeros((128, 648), np.float32)
    c[:, 0:128] = np.eye(128, dtype=np.float32)
    for m in range(128):
        if m % 64 < 32:
            c[m + 32, 128 + m] = -1.0
        else:
            c[m - 32, 128 + m] = 1.0
    a = np.arange(128)[:, None]
    b = np.arange(128)[None, :]
    c[:, 256:384] = (a >= b + 64)
    c[:, 384:512] = (np.abs(a - b) <= 64)
    c[:, 512:640] = (a <= b - 64)
    inv_freq = (10000.0 ** (-np.arange(0, 64, 2, dtype=np.float32) / 64.0)).astype(np.float32)
    c[:, 640] = inv_freq[np.arange(128) % 32]
    c[:, 641] = EPS
    dm = (np.arange(STRIPW)[None, :] - 1920 - np.arange(128)[:, None]).astype(np.float32)
    dpn = np.stack([np.maximum(dm, 0.0), np.minimum(dm, 0.0)]).astype(np.float32)
    return c, dpn


class Builder:
    def __init__(self, n_layers=DEPTH, dbg=None):
        self.n_layers = n_layers
        self.dbg = dbg or {}
        self.nc = bass.Bass("TRN2", target_bir_lowering=False)

    def mm(self, out, lhsT, rhs, start, stop, reads, writes, signal=True):
        self.S.op("pe", lambda e: e.matmul(out, lhsT=lhsT, rhs=rhs, start=start, stop=stop), reads, writes, signal)

    def tr(self, out, in_, ident, reads, writes):
        self.S.op("pe", lambda e: e.transpose(out=out, in_=in_, identity=ident), reads, writes)

    def act(self, out, in_, func, reads, writes, scale=None, bias=None):
        kw = {}
        if scale is not None:
            kw["scale"] = scale
            if func == AF.Copy:
                func = AF.Identity
        if bias is not None:
            kw["bias"] = bias
        self.S.op("act", lambda e: e.activation(out=out, in_=in_, func=func, **kw), reads, writes)

    def tt(self, eng, out, in0, in1, op, reads, writes):
        self.S.op(eng, lambda e: e.tensor_tensor(out=out, in0=in0, in1=in1, op=op), reads, writes)

    def ts(self, eng, out, in0, s1, s2, op0, op1, reads, writes):
        if op1 is None:
            self.S.op(eng, lambda e: e.tensor_scalar(out=out, in0=in0, scalar1=s1, scalar2=None, op0=op0), reads, writes)
        else:
            self.S.op(eng, lambda e: e.tensor_scalar(out=out, in0=in0, scalar1=s1, scalar2=s2, op0=op0, op1=op1), reads, writes)

    def stt(self, eng, out, in0, scalar, in1, op0, op1, reads, writes):
        self.S.op(eng, lambda e: e.scalar_tensor_tensor(out=out, in0=in0, scalar=scalar, in1=in1, op0=op0, op1=op1), reads, writes)

    def cp(self, eng, out, in_, reads, writes):
        if eng == "act":
            self.act(out, in_, AF.Copy, reads, writes)
        else:
            self.S.op(eng, lambda e: e.tensor_copy(out=out, in_=in_), reads, writes)

    def recip(self, out, in_, reads, writes):
        self.S.op("dve", lambda e: e.reciprocal(out=out, in_=in_), reads, writes)

    def memset(self, eng, ap, val, writes):
        self.S.op(eng, lambda e: e.memset(ap, val), (), writes)

    def dma(self, q, out, in_, reads, writes, slow=False):
        if slow:
            self.S.dma(q, lambda e: e.dma_start(out=out, in_=in_, allow_slow_non_contiguous=True), reads, writes)
        else:
            self.S.dma(q, lambda e: e.dma_start(out=out, in_=in_), reads, writes)

    def bank(self, ring):
        return ring.next()

    def build(self):
        nc = self.nc
        L = self.n_layers
        din = lambda name, shape, dt=F32: nc.dram_tensor(name, list(shape), dt, kind="ExternalInput").ap()
        self.x_d = din("x", [SEQ, D])
        self.c_d = din("c", [1, D])
        self.pos_d = din("positions", [1, SEQ], I32)
        self.adaw_d = din("ada_w", [DEPTH, D, 6 * D])
        self.vec_d = din("vecpack", [DEPTH, NVEC, 128])
        self.fin_d = din("final_norm", [8, 128])
        self.win_d = din("w_in", [DEPTH, D, IN_COLS])
        self.wuq_d = din("mla_w_uq", [DEPTH, 768, 1536])
        self.wukv_d = din("mla_w_ukv", [DEPTH, 512, 2048])
        self.rld_d = din("ret_log_decay", [1, DEPTH * 16])
        self.wbm_d = din("w_br_mla", [DEPTH, 1024, 1024])
        self.wbd_d = din("w_br_dil", [DEPTH, 512, 1024])
        self.wbr_d = din("w_br_ret", [DEPTH, 1024, 1024])
        self.wout_d = din("w_out", [DEPTH, 1024, 1024])
        self.f1_d = din("ffn_w1", [2, D, D_FF])
        self.f3_d = din("ffn_w3", [2, D, D_FF])
        self.f2_d = din("ffn_w2", [2, D_FF, D])
        self.mr_d = din("moe_router", [2, D, NEXP])
        self.m1_d = din("moe_w1", [2, NEXP, D, D_FFE])
        self.m3_d = din("moe_w3", [2, NEXP, D, D_FFE])
        self.m2_d = din("moe_w2", [2, NEXP, D_FFE, D])
        self.cst_d = din("consts", [128, 648])
        self.dpn_d = din("dposneg", [2, 128, STRIPW])
        self.out_d = nc.dram_tensor("out", [SEQ, D], F32, kind="ExternalOutput").ap()

        def scratch(name, shape, dt=BF16):
            kind = "ExternalOutput" if name in self.dbg else "Internal"
            return nc.dram_tensor(name, list(shape), dt, kind=kind).ap()
        self.xs = scratch("xs", [KC, 128, SEQ], F32)
        self.mg = scratch("mg", [KC, 128, SEQ], F32)
        self.qT = scratch("qT", [8, 192, SEQ])
        self.knT = scratch("knT", [8, 128, SEQ])
        self.vm = scratch("vm", [SEQ, 1024])
        self.dqT = scratch("dqT", [3, 512, SEQ])
        self.dkT = scratch("dkT", [3, 512, SEQ])
        self.dv = scratch("dv", [3, SEQ, 520])
        self.rqT = scratch("rqT", [512, SEQ])
        self.rkT = scratch("rkT", [512, SEQ])
        self.rv = scratch("rv", [SEQ, 1024])
        self.rgT = scratch("rgT", [1024, SEQ])
        self.gT = scratch("gT", [3, 1024, SEQ])
        self.ydbg = scratch("ydbg", [3, 1024, SEQ]) if "ydbg" in self.dbg else None
        self.xsB = [Buf("xs%d" % t) for t in range(NTB)]
        self.mgB = [Buf("mg%d" % t) for t in range(NTB)]
        self.qTB = [Buf() for _ in range(8)]
        self.knTB = [Buf() for _ in range(8)]
        self.vmB = [Buf() for _ in range(2)]
        self.dqTB = [[Buf() for _ in range(4)] for _ in range(3)]
        self.dkTB = [[Buf() for _ in range(4)] for _ in range(3)]
        self.dvB = [Buf() for _ in range(3)]
        self.rqTB = [Buf() for _ in range(4)]
        self.rkTB = [Buf() for _ in range(4)]
        self.rvB = [Buf() for _ in range(2)]
        self.rgTB = [Buf() for _ in range(8)]
        self.gTB = [[Buf() for _ in range(8)] for _ in range(3)]
        self.outB = [Buf() for _ in range(NTB)]
        self.dbgB = Buf()

        with ExitStack() as st:
            self.st = st
            self.S = Sched(nc, st)
            sb = lambda n, sh, dt: st.enter_context(nc.sbuf_tensor(n, sh, dt))
            self.pb = [st.enter_context(nc.psum_tensor("pb%d" % i, [128, 512], F32)) for i in range(8)]
            self.PB = [Buf("pb%d" % i) for i in range(8)]
            self.allbanks = Ring([(self.pb[i], self.PB[i]) for i in range(8)])
            self.RA = sb("RA", [128, KC, SEQ], BF16)
            self.RAb = [[Buf("RA%d_%d" % (k, t)) for t in range(NTB)] for k in range(KC)]
            self.RW = sb("RW", [128, 4, 4096], BF16)
            self.WB = [Buf("W%d" % i) for i in range(4)]
            self.cst = sb("cst", [128, 648], F32); self.cstB = Buf("cst")
            self.cbf = sb("cbf", [128, 1536], BF16); self.cbfB = Buf("cbf")
            self.onesf = sb("onesf", [128, 128], F32); self.onesfB = Buf("onesf")
            self.avgf = sb("avgf", [128, 128], F32)
            self.cosT = sb("cosT", [128, SEQ], BF16); self.sinT = sb("sinT", [128, SEQ], BF16); self.csB = Buf("cs")
            self.krT = sb("krT", [128, SEQ], BF16); self.krB = [Buf() for _ in range(NTB)]
            self.modT = sb("modT", [128, DEPTH, 48], F32); self.modB = Buf("mod")
            self.vecT = sb("vecT", [128, DEPTH, NVEC], F32); self.vecB = Buf("vec")
            self.finT = sb("finT", [128, 8], F32)
            self.gsT = sb("gsT", [128, DEPTH, 16], F32); self.gsB = Buf("gs")
            self.rldT = sb("rldT", [128, DEPTH * 16], F32); self.rldB = Buf("rld")
            self.cact = sb("cact", [128, 8], BF16); self.cactB = Buf("cact")
            self.OVN = 58368
            self.OV = sb("OV", [128, self.OVN], BF16)
            self.ident_f = self.cst[:, 0:128]
            self.ident_b = self.cbf[:, 0:128]
            self.perm_b = self.cbf[:, 128:256]
            self.ones_b = self.cbf[:, 256:384]
            self.mask_b = self.cbf[:, 384:768]
            self.negm_b = self.cbf[:, 768:1152]
            self.negmT_b = self.cbf[:, 1152:1536]
            self.eps_ap = self.cst[:, 641:642]

            self.setup()
            for l in range(L):
                self.layer(l)
            self.final()
            if self.dbg:
                self.S.wait_all("sp", [self.dbgB])
            self.S.wait_all("sp", self.outB)
            self.S.emit()
        return nc

    def ovv(self, off, n, dt=BF16):
        nb = n * (2 if dt == F32 else 1)
        assert off + nb <= self.OVN, (off, nb, self.OVN)
        v = self.OV[:, off:off + nb]
        if dt == F32:
            v = v.bitcast(F32)
        return v, off + nb

    def wload(self, slots, src_ap, kp, nk, ncols):
        n = nk * ncols
        assert n <= 4096 * len(slots)
        s0 = slots[0]
        if len(slots) == 1:
            flat = self.RW[0:kp, s0, 0:n]
        else:
            flat = self.RW[0:kp, s0:s0 + len(slots), :].rearrange("p a b -> p (a b)")[:, 0:n]
        view = flat.rearrange("p (k n) -> p k n", n=ncols)
        bufs = [self.WB[s] for s in slots]
        self.dma("pool", view, src_ap, [], bufs)
        return view, bufs

    def setup(self):
        S = self.S
        o = 0
        self.dma("sp", self.cst[:], self.cst_d, [], [self.cstB])
        self.cp("act", self.cbf[:, 0:256], self.cst[:, 0:256], [self.cstB], [self.cbfB])
        self.memset("dve", self.cbf[:, 256:384], 1.0, [self.cbfB])
        self.cp("act", self.cbf[:, 384:768], self.cst[:, 256:640], [self.cstB], [self.cbfB])
        self.memset("dve", self.onesf[:], 1.0, [self.onesfB])
        self.memset("dve", self.avgf[:], 1.0 / 128, [self.onesfB])
        self.ts("dve", self.cbf[:, 768:1152], self.cst[:, 256:640], 1.0, 30000.0, ALU.subtract, ALU.mult, [self.cstB], [self.cbfB])
        for j in range(3):
            self.ts("dve", self.cbf[:, 1152 + j * 128:1152 + (j + 1) * 128], self.cst[:, 256 + (2 - j) * 128:256 + (3 - j) * 128], 1.0, 30000.0,
                    ALU.subtract, ALU.mult, [self.cstB], [self.cbfB])
        self.dma("sp", self.rldT[:], self.rld_d.partition_broadcast(128), [], [self.rldB])
        for l in range(DEPTH):
            self.ts("dve", self.rldT[:, l * 16 + 8:l * 16 + 16], self.rldT[:, l * 16 + 8:l * 16 + 16], -1.0, None, ALU.mult, None,
                    [self.rldB], [self.rldB])
        posi, o1 = self.ovv(0, SEQ, F32)
        posi = self.OV[:, 0:2 * SEQ].bitcast(I32)
        ang, o2 = self.ovv(o1, SEQ, F32)
        kf, o3 = self.ovv(o2, SEQ, F32)
        ki = self.OV[:, o3:o3 + 2 * SEQ].bitcast(I32)
        o4 = o3 + 2 * SEQ
        a2, o5 = self.ovv(o4, SEQ, F32)
        Bp, Ba, Bk, Bki, Ba2 = Buf(), Buf(), Buf(), Buf(), Buf()
        self.dma("sp", posi, self.pos_d.partition_broadcast(128), [], [Bp])
        self.cp("dve", ang, posi, [Bp], [Ba])
        self.ts("dve", ang, ang, self.cst[:, 640:641], None, ALU.mult, None, [Ba, self.cstB], [Ba])
        TWO_PI = float(2 * np.pi)

        def reduce_sin(src, dst_bf, shift):
            self.ts("dve", a2, src, float(shift), None, ALU.add, None, [Ba], [Ba2])
            self.ts("dve", kf, a2, float(1.0 / TWO_PI), None, ALU.mult, None, [Ba2], [Bk])
            self.cp("dve", ki, kf, [Bk], [Bki])
            self.cp("dve", kf, ki, [Bki], [Bk])
            self.stt("dve", a2, kf, -TWO_PI, a2, ALU.mult, ALU.add, [Bk, Ba2], [Ba2])
            self.ts("dve", kf, a2, float(np.pi), -TWO_PI, ALU.is_gt, ALU.mult, [Ba2], [Bk])
            self.tt("dve", a2, a2, kf, ALU.add, [Bk, Ba2], [Ba2])
            self.ts("dve", kf, a2, float(-np.pi), TWO_PI, ALU.is_lt, ALU.mult, [Ba2], [Bk])
            self.tt("dve", a2, a2, kf, ALU.add, [Bk, Ba2], [Ba2])
            self.act(dst_bf, a2, AF.Sin, [Ba2], [self.csB])
        reduce_sin(ang, self.sinT[:], 0.0)
        reduce_sin(ang, self.cosT[:], np.pi / 2)
        vst, o6 = self.ovv(o5, DEPTH * 128 + 128 + 128, F32)
        Bv = Buf()
        for l in range(DEPTH):
            self.dma("sp", vst[0:NVEC, l * 128:(l + 1) * 128], self.vec_d[l], [], [Bv])
        self.dma("sp", vst[0:8, 512:640], self.fin_d, [], [Bv])
        self.dma("sp", vst[0:8, 640:768], self.c_d.rearrange("o (k p) -> (o k) p", p=128), [], [Bv])
        for l in range(DEPTH):
            bk, bb = self.allbanks.next()
            self.tr(bk[:, 0:NVEC], vst[0:NVEC, l * 128:(l + 1) * 128], self.ident_f[0:NVEC, 0:NVEC], [Bv, self.cstB], [bb])
            self.cp("dve", self.vecT[:, l, :], bk[:, 0:NVEC], [bb], [self.vecB])
        bk, bb = self.allbanks.next()
        self.tr(bk[:, 0:8], vst[0:8, 512:640], self.ident_f[0:8, 0:8], [Bv, self.cstB], [bb])
        self.tr(bk[:, 8:16], vst[0:8, 640:768], self.ident_f[0:8, 0:8], [Bv, self.cstB], [bb])
        self.cp("act", self.finT[:], bk[:, 0:8], [bb], [self.vecB])
        self.act(self.cact[:], bk[:, 8:16], AF.Silu, [bb], [self.cactB])
        nblk = 0
        pending = []
        jobs = [(l, cb) for l in range(DEPTH) for cb in range(12)]

        def issue(i):
            l, cb = jobs[i]
            src = self.adaw_d[l].rearrange("(k p) n -> p k n", p=128)[:, :, cb * 512:(cb + 1) * 512]
            return self.wload([i % 4], src, 128, KC, 512)
        for i in range(min(3, len(jobs))):
            pending.append(issue(i))
        for i, (l, cb) in enumerate(jobs):
            W, wb = pending.pop(0)
            if i + 3 < len(jobs):
                pending.append(issue(i + 3))
            if cb % 12 == 0:
                mbk, mbb = self.allbanks.next()
            for j in range(4):
                col = cb * 4 + j
                for kc in range(KC):
                    self.mm(mbk[:, col:col + 1], W[:, kc, j * 128:(j + 1) * 128], self.cact[:, kc:kc + 1], kc == 0, kc == KC - 1,
                            wb + [self.cactB], [mbb], signal=(kc == KC - 1))
            if cb == 11:
                self.tt("dve", self.modT[:, l, :], mbk[:, 0:48], self.vecT[:, l, 0:48], ALU.add, [mbb, self.vecB], [self.modB])
                self.stt("dve", self.gsT[:, l, 0:8], self.modT[:, l, 8:16], 1.0, self.vecT[:, l, 48:56], ALU.add, ALU.mult,
                         [self.modB, self.vecB], [self.gsB])
                self.stt("dve", self.gsT[:, l, 8:16], self.modT[:, l, 32:40], 1.0, self.vecT[:, l, 56:64], ALU.add, ALU.mult,
                         [self.modB, self.vecB], [self.gsB])
        xt0, p = self.ovv(o6, 4 * D, F32)
        xt1, p = self.ovv(p, 4 * D, F32)
        xs0, p = self.ovv(p, KC * 512, F32)
        xs1, p = self.ovv(p, KC * 512, F32)
        xtr = Ring([(xt0.rearrange("p (a d) -> p a d", a=4), Buf()), (xt1.rearrange("p (a d) -> p a d", a=4), Buf())])
        xsr = Ring([(xs0.rearrange("p (k t) -> p k t", k=KC), Buf()), (xs1.rearrange("p (k t) -> p k t", k=KC), Buf())])
        xv = self.x_d.rearrange("(a p) d -> p a d", p=128)
        for tb in range(NTB):
            xt, xtB = xtr.next()
            self.dma("sp", xt, xv[:, 4 * tb:4 * tb + 4, :], [], [xtB])
            xo, xoB = xsr.next()
            for kc in range(KC):
                bk, bb = self.allbanks.next()
                for a in range(4):
                    self.tr(bk[:, a * 128:(a + 1) * 128], xt[:, a, kc * 128:(kc + 1) * 128], self.ident_f, [xtB, self.cstB], [bb])
                self.cp("act" if kc % 2 else "dve", xo[:, kc, :], bk[:, :], [bb], [xoB])
            self.dma("sp", self.xs.rearrange("k p t -> p k t")[:, :, tb * 512:(tb + 1) * 512], xo, [xoB], [self.xsB[tb]])
        S.barrier()

    def norm_phase(self, l, which, tbs, ov0, router=None):
        gs = self.gsT[:, l, 8 * which:8 * which + 8]
        sh = self.modT[:, l, (0 if which == 0 else 24):(0 if which == 0 else 24) + 8]
        p = ov0
        xi = []
        for i in range(2):
            v, p = self.ovv(p, KC * 512, F32)
            xi.append((v.rearrange("p (k t) -> p k t", k=KC), Buf()))
        xir = Ring(xi)
        sq = []
        for i in range(2):
            v, p = self.ovv(p, 512)
            sq.append((v, Buf()))
        sqr = Ring(sq)
        rt = []
        for i in range(2):
            v, p = self.ovv(p, 512, F32)
            rt.append((v, Buf()))
        rtr = Ring(rt)
        tm = []
        for i in range(3):
            v, p = self.ovv(p, 512, F32)
            tm.append((v, Buf()))
        tmr = Ring(tm)
        h32 = []
        if router is not None:
            for i in range(2):
                v, p = self.ovv(p, 512, F32)
                h32.append((v, Buf()))
            h32r = Ring(h32)
        xsv = self.xs.rearrange("k p t -> p k t")
        loaded = {}

        def load(tb):
            x, xB = xir.next()
            self.dma("sp", x, xsv[:, :, tb * 512:(tb + 1) * 512], [self.xsB[tb]], [xB])
            loaded[tb] = (x, xB)
        load(tbs[0])
        for i, tb in enumerate(tbs):
            if i + 1 < len(tbs):
                load(tbs[i + 1])
            x, xB = loaded.pop(tb)
            bk, bb = self.allbanks.next()
            for kc in range(KC):
                s, sB = sqr.next()
                self.act(s, x[:, kc, :], AF.Square, [xB], [sB])
                self.mm(bk[:, :], self.ones_b, s, kc == 0, kc == KC - 1, [sB, self.cbfB], [bb], signal=True)
            r, rB = rtr.next()
            self.act(r, bk[:, :], AF.Sqrt, [bb, self.cstB], [rB], scale=1.0 / D, bias=self.eps_ap)
            self.recip(r, r, [rB], [rB])
            if router is not None:
                lgbk, lgbb = self.allbanks.next()
            for kc in range(KC):
                t, tB = tmr.next()
                self.tt("dve", t, x[:, kc, :], r, ALU.mult, [xB, rB], [tB])
                self.act(self.RA[:, kc, tb * 512:(tb + 1) * 512], t, AF.Identity, [tB, self.gsB, self.modB], [self.RAb[kc][tb]],
                         scale=gs[:, kc:kc + 1], bias=sh[:, kc:kc + 1])
                if router is not None:
                    h, hB = h32r.next()
                    self.act(h, t, AF.Identity, [tB, self.gsB, self.modB], [hB], scale=gs[:, kc:kc + 1], bias=sh[:, kc:kc + 1])
                    wr, wrB = router["w"]
                    for a in range(4):
                        self.mm(lgbk[:, a * 8:(a + 1) * 8], h[:, a * 128:(a + 1) * 128], wr[:, kc, :], (kc == 0 and a == 0), kc == KC - 1,
                                [hB, wrB], [lgbb], signal=(a == 3))
            if router is not None:
                lg, lgB = router["lg"]
                tl = (tb % 2) * 4
                self.cp("dve", lg[:, tl:tl + 4, :], lgbk[:, 0:32].rearrange("p (a e) -> p a e", e=8), [lgbb], [lgB])
        return p

    def proj_fm(self, src, srcB, nk, kp, W, wb, wcol, ncols, handler):
        for tb in range(NTB):
            bk, bb = self.allbanks.next()
            for kc in range(nk):
                self.mm(bk[0:ncols, :], W[0:kp, kc, wcol:wcol + ncols], src[0:kp, kc, tb * 512:(tb + 1) * 512], kc == 0, kc == nk - 1,
                        wb + [srcB[kc][tb]], [bb], signal=(kc == nk - 1))
            self.flush_pe_deferred()
            self._pe_deferred = handler(tb, bk, bb)

    def flush_pe_deferred(self):
        d = getattr(self, "_pe_deferred", None)
        self._pe_deferred = None
        if d is not None:
            d()

    def rope_evac(self, tb, bk, bb, n, tmps, then):
        (cbt, cbB), (ubt, ubB) = tmps.next(), tmps.next()
        cs = slice(tb * 512, (tb + 1) * 512)
        self.tt("dve", cbt[0:n, :], bk[0:n, :], self.cosT[0:n, cs], ALU.mult, [bb, self.csB], [cbB])
        self.tt("dve", ubt[0:n, :], bk[0:n, :], self.sinT[0:n, cs], ALU.mult, [bb, self.csB], [ubB])

        def stage2():
            b2, b2B = self.allbanks.next()
            self.mm(b2[0:n, :], self.ident_b[0:n, 0:n], cbt[0:n, :], True, False, [cbB, self.cbfB], [b2B], signal=False)
            self.mm(b2[0:n, :], self.perm_b[0:n, 0:n], ubt[0:n, :], False, True, [ubB, self.cbfB], [b2B])
            then(b2, b2B)
        return stage2

    def layer(self, l):
        S = self.S
        self._pj_pre = [self.pj_issue(l, i) for i in range(3)]
        self.norm_phase(l, 0, list(range(NTB)), 0)
        S.barrier()
        self.proj_phase(l)
        S.barrier()
        self._br_pre = {0: self.br_issue(l, 0, [0, 1])}
        self.mla_attn(l)
        S.barrier()
        self._br_pre[1] = self.br_issue(l, 1, [2, 3])
        self.branch_out(l, 0)
        S.barrier()
        self.dil_attn(l)
        S.barrier()
        self._br_pre[2] = self.br_issue(l, 2, [0, 1])
        self.branch_out(l, 1)
        S.barrier()
        self._wout_pre = self.wload([2, 3], self.wout_d[l].rearrange("(k p) n -> p k n", p=128), 128, KC, 1024)
        self.ret_attn(l)
        S.barrier()
        self.branch_out(l, 2)
        S.barrier()
        self.ffn(l)
        S.barrier()

    def pj_blocks(self):
        blocks = []
        blocks.append((C_CQ, 512, "cq", 0)); blocks.append((C_CQ + 512, 256, "cq", 4))
        blocks.append((C_CKV, 512, "ckv", 0))
        blocks.append((C_KR, 64, "kr", 0))
        for g in range(3):
            blocks.append((C_DQ + g * 512, 512, "dq", g))
        for g in range(3):
            blocks.append((C_DK + g * 512, 512, "dk", g))
        for g in range(3):
            blocks.append((C_DV + g * 512, 512, "dv", g))
        blocks.append((C_RQ, 512, "rq", 0)); blocks.append((C_RK, 512, "rk", 0))
        blocks.append((C_RV, 512, "rv", 0)); blocks.append((C_RV + 512, 512, "rv", 1))
        blocks.append((C_RG, 512, "rg", 0)); blocks.append((C_RG + 512, 512, "rg", 1))
        for b3 in range(3):
            blocks.append((C_GA + b3 * 1024, 512, "gate", (b3, 0))); blocks.append((C_GA + b3 * 1024 + 512, 512, "gate", (b3, 1)))
        return blocks

    def pj_issue(self, l, i):
        c0, n, _, _ = self.pj_blocks()[i]
        winv = self.win_d[l].rearrange("(k p) n -> p k n", p=128)
        return self.wload([i % 4], winv[:, :, c0:c0 + n], 128, KC, n)

    def br_issue(self, l, b, slots):
        kp = 64 if b == 1 else 128
        if b == 0:
            src = self.wbm_d[l].rearrange("(k p) n -> p k n", p=128)
        elif b == 1:
            src = self.wbd_d[l].rearrange("(k p) n -> p k n", p=64)
        else:
            src = self.wbr_d[l].rearrange("(k p) n -> p k n", p=128)
        return self.wload(slots, src, kp, KC, 1024)

    def proj_phase(self, l):
        p = 0
        cq, p = self.ovv(p, 6 * SEQ)
        cq = cq.rearrange("p (k t) -> p k t", k=6)
        ckv, p = self.ovv(p, 4 * SEQ)
        ckv = ckv.rearrange("p (k t) -> p k t", k=4)
        cqB = [[Buf() for _ in range(NTB)] for _ in range(6)]
        ckvB = [[Buf() for _ in range(NTB)] for _ in range(4)]
        stg = []
        for i in range(3):
            v, p = self.ovv(p, SEQ)
            stg.append((v, Buf()))
        stgr = Ring(stg)
        rtm = []
        for i in range(6):
            v, p = self.ovv(p, 512)
            rtm.append((v, Buf()))
        rtmr = Ring(rtm)
        stv = []
        for i in range(3):
            v, p = self.ovv(p, 520)
            stv.append((v, Buf()))
            self.memset("dve", v, 1.0, [stv[-1][1]])
        stvr = Ring(stv)
        stw = []
        for i in range(3):
            v, p = self.ovv(p, 512)
            stw.append((v, Buf()))
        stwr = Ring(stw)
        sq = []
        for i in range(2):
            v, p = self.ovv(p, 512)
            sq.append((v, Buf()))
        sqr = Ring(sq)
        rt = []
        for i in range(2):
            v, p = self.ovv(p, 512, F32)
            rt.append((v, Buf()))
        rtr = Ring(rt)
        RA, RAb = self.RA, self.RAb
        winv = self.win_d[l].rearrange("(k p) n -> p k n", p=128)

        blocks = []
        blocks.append((C_CQ, 512, "cq", 0)); blocks.append((C_CQ + 512, 256, "cq", 4))
        blocks.append((C_CKV, 512, "ckv", 0))
        blocks.append((C_KR, 64, "kr", 0))
        for g in range(3):
            blocks.append((C_DQ + g * 512, 512, "dq", g))
        for g in range(3):
            blocks.append((C_DK + g * 512, 512, "dk", g))
        for g in range(3):
            blocks.append((C_DV + g * 512, 512, "dv", g))
        blocks.append((C_RQ, 512, "rq", 0)); blocks.append((C_RK, 512, "rk", 0))
        blocks.append((C_RV, 512, "rv", 0)); blocks.append((C_RV + 512, 512, "rv", 1))
        blocks.append((C_RG, 512, "rg", 0)); blocks.append((C_RG + 512, 512, "rg", 1))
        for b3 in range(3):
            blocks.append((C_GA + b3 * 1024, 512, "gate", (b3, 0))); blocks.append((C_GA + b3 * 1024 + 512, 512, "gate", (b3, 1)))
        nb = len(blocks)
        pend = []

        def issue(i):
            c0, n, _, _ = blocks[i]
            return self.wload([i % 4], winv[:, :, c0:c0 + n], 128, KC, n)
        pend.extend(self._pj_pre)

        def fm_store_handler(dram_rows_ap, dramB, n, func=AF.Copy, scale=None):
            st_, stB = stgr.next()

            def h(tb, bk, bb):
                self.act(st_[0:n, tb * 512:(tb + 1) * 512], bk[0:n, :], func, [bb], [stB], scale=scale)
                if tb == NTB - 1:
                    self.dma("sp", dram_rows_ap, st_[0:n, :], [stB], [dramB])
            return h

        def rope_store_handler(dram_rows_ap, dramB, n, d, scale=None, sbuf_dest=None, sbufB=None):
            if sbuf_dest is None:
                st_, stB = stgr.next()
            else:
                st_, stB = sbuf_dest, None

            def h(tb, bk, bb):
                def then(b2, b2B):
                    if d == 1:
                        dst = st_[0:n, tb * 512:(tb + 1) * 512]
                        src = b2[0:n, :]
                    else:
                        w = 512 // d
                        dst = st_[0:n, :].rearrange("p (r l) -> p r l", r=d)[:, :, tb * w:(tb + 1) * w]
                        src = b2[0:n, :].rearrange("p (j r) -> p r j", r=d)
                    wB = [stB] if sbuf_dest is None else [sbufB[tb]]
                    self.act(dst, src, AF.Copy, [b2B], wB, scale=scale)
                    if sbuf_dest is None and tb == NTB - 1:
                        self.dma("sp", dram_rows_ap, st_[0:n, :], [stB], [dramB])
                return self.rope_evac(tb, bk, bb, n, rtmr, then)
            return h

        for bi_, (c0, n, kind, info) in enumerate(blocks):
            W, wb = pend.pop(0)
            if bi_ + 3 < nb:
                pend.append(issue(bi_ + 3))
            if kind in ("cq", "ckv"):
                dstt, dB = (cq, cqB) if kind == "cq" else (ckv, ckvB)
                for j in range(n // 128):
                    c = info + j

                    def h(tb, bk, bb, c=c, dstt=dstt, dB=dB):
                        self.cp("act", dstt[:, c, tb * 512:(tb + 1) * 512], bk[:, :], [bb], [dB[c][tb]])
                    self.proj_fm(RA, RAb, KC, 128, W, wb, j * 128, 128, h)
            elif kind == "kr":
                self.proj_fm(RA, RAb, KC, 128, W, wb, 0, 64, rope_store_handler(None, None, 64, 1, sbuf_dest=self.krT, sbufB=self.krB))
            elif kind in ("dq", "dk"):
                g = info
                dr, dB = (self.dqT, self.dqTB) if kind == "dq" else (self.dkT, self.dkTB)
                for j in range(4):
                    self.proj_fm(RA, RAb, KC, 128, W, wb, j * 128, 128,
                                 rope_store_handler(dr[g, j * 128:(j + 1) * 128, :], dB[g][j], 128, DIL_D[g]))
            elif kind in ("rq", "rk"):
                dr, dB = (self.rqT, self.rqTB) if kind == "rq" else (self.rkT, self.rkTB)
                for j in range(4):
                    self.proj_fm(RA, RAb, KC, 128, W, wb, j * 128, 128,
                                 rope_store_handler(dr[j * 128:(j + 1) * 128, :], dB[j], 128, 1, scale=(0.125 if kind == "rk" else None)))
            elif kind == "rg":
                for j in range(4):
                    o = info * 4 + j
                    self.proj_fm(RA, RAb, KC, 128, W, wb, j * 128, 128, fm_store_handler(self.rgT[o * 128:(o + 1) * 128, :], self.rgTB[o], 128, AF.Silu))
            elif kind == "gate":
                b3, hf = info
                for j in range(4):
                    o = hf * 4 + j
                    self.proj_fm(RA, RAb, KC, 128, W, wb, j * 128, 128,
                                 fm_store_handler(self.gT[b3, o * 128:(o + 1) * 128, :], self.gTB[b3][o], 128, AF.Sigmoid))
            elif kind == "dv":
                self.flush_pe_deferred()
                g = info
                d = DIL_D[g]
                Lr = SEQ // d
                for tt_ in range(NTT):
                    r = (128 * tt_) // Lr
                    j0 = (128 * tt_) % Lr
                    t0 = r + d * j0
                    tsl = slice(t0, t0 + d * 127 + 1, d)
                    tbs = sorted(set([t0 // 512, (t0 + d * 127) // 512])) if d < 16 else list(range(NTB))
                    bk, bb = self.allbanks.next()
                    for kc in range(KC):
                        self.mm(bk[:, :], RA[:, kc, tsl], W[:, kc, :], kc == 0, kc == KC - 1, wb + [RAb[kc][t] for t in tbs], [bb],
                                signal=(kc == KC - 1))
                    sv, svB = stvr.next()
                    self.cp("act" if tt_ % 2 else "dve", sv.rearrange("p (h c) -> p h c", c=65)[:, :, 0:64],
                            bk[:, :].rearrange("p (h c) -> p h c", c=64), [bb], [svB])
                    self.dma("sp", self.dv[g, tt_ * 128:(tt_ + 1) * 128, :], sv, [svB], [self.dvB[g]])
            elif kind == "rv":
                self.flush_pe_deferred()
                hf = info
                for tt_ in range(NTT):
                    tb = tt_ // 4
                    bk, bb = self.allbanks.next()
                    for kc in range(KC):
                        self.mm(bk[:, :], RA[:, kc, tt_ * 128:(tt_ + 1) * 128], W[:, kc, :], kc == 0, kc == KC - 1, wb + [RAb[kc][tb]], [bb],
                                signal=(kc == KC - 1))
                    sw, swB = stwr.next()
                    self.cp("act" if tt_ % 2 else "dve", sw, bk[:, :], [bb], [swB])
                    self.dma("sp", self.rv[tt_ * 128:(tt_ + 1) * 128, hf * 512:(hf + 1) * 512], sw, [swB], [self.rvB[hf]])

        self.flush_pe_deferred()

        def rmsn(src, srcB, nk, gcol0, inv_n):
            for tb in range(NTB):
                bk, bb = self.allbanks.next()
                for c in range(nk):
                    s, sB = sqr.next()
                    self.act(s, src[:, c, tb * 512:(tb + 1) * 512], AF.Square, [srcB[c][tb]], [sB])
                    self.mm(bk[:, :], self.ones_b, s, c == 0, c == nk - 1, [sB, self.cbfB], [bb], signal=True)
                r, rB = rtr.next()
                self.act(r, bk[:, :], AF.Sqrt, [bb, self.cstB], [rB], scale=inv_n, bias=self.eps_ap)
                self.recip(r, r, [rB], [rB])
                for c in range(nk):
                    v = src[:, c, tb * 512:(tb + 1) * 512]
                    self.stt("dve", v, v, self.vecT[:, l, gcol0 + c:gcol0 + c + 1], r, ALU.mult, ALU.mult, [srcB[c][tb], rB, self.vecB],
                             [srcB[c][tb]])
        rmsn(cq, cqB, 6, 64, 1.0 / 768)
        rmsn(ckv, ckvB, 4, 70, 1.0 / 512)

        wuqv = self.wuq_d[l].rearrange("(k p) n -> p k n", p=128)
        wukvv = self.wukv_d[l].rearrange("(k p) n -> p k n", p=128)
        jobs = []
        for hp in range(4):
            jobs.append(("q", hp))
            jobs.append(("kv", hp))
        pend = []

        def issue2(i):
            kind, hp = jobs[i]
            if kind == "q":
                return self.wload([i % 4], wuqv[:, :, hp * 384:(hp + 1) * 384], 128, 6, 384)
            return self.wload([i % 4], wukvv[:, :, hp * 512:(hp + 1) * 512], 128, 4, 512)
        for i in range(3):
            pend.append(issue2(i))
        for i, (kind, hp) in enumerate(jobs):
            W, wb = pend.pop(0)
            if i + 3 < len(jobs):
                pend.append(issue2(i + 3))
            for hh in range(2):
                h_ = 2 * hp + hh
                if kind == "q":
                    self.proj_fm(cq, cqB, 6, 128, W, wb, hh * 192, 128, fm_store_handler(self.qT[h_, 0:128, :], self.qTB[h_], 128))
                    self.proj_fm(cq, cqB, 6, 128, W, wb, hh * 192 + 128, 64, rope_store_handler(self.qT[h_, 128:192, :], self.qTB[h_], 64, 1))
                else:
                    self.proj_fm(ckv, ckvB, 4, 128, W, wb, hh * 256, 128, fm_store_handler(self.knT[h_], self.knTB[h_], 128))
            self.flush_pe_deferred()
            if kind == "kv":
                Wv = W.rearrange("p k (h c) -> p k h c", c=256)[:, :, :, 128:256]
                for tt_ in range(NTT):
                    tb = tt_ // 4
                    bk, bb = self.allbanks.next()
                    for c in range(4):
                        self.mm(bk[:, 0:256].rearrange("p (h c) -> p h c", c=128), ckv[:, c, tt_ * 128:(tt_ + 1) * 128], Wv[:, c, :, :],
                                c == 0, c == 3, wb + [ckvB[c][tb]], [bb], signal=(c == 3))
                    sw, swB = stwr.next()
                    self.cp("act" if tt_ % 2 else "dve", sw[:, 0:256], bk[:, 0:256], [bb], [swB])
                    self.dma("sp", self.vm[tt_ * 128:(tt_ + 1) * 128, hp * 256:(hp + 1) * 256], sw[:, 0:256], [swB], [self.vmB[hp // 2]])

    def mla_attn(self, l):
        p = 0
        Ld = []
        for i in range(2):
            d_ = {}
            for nm, n in (("qn", SEQ), ("qr", SEQ), ("kn", SEQ), ("vh", NTT * 128)):
                v, p = self.ovv(p, n)
                d_[nm] = (v, Buf())
            Ld.append(d_)
        Et = []
        for i in range(6):
            v, p = self.ovv(p, 512)
            Et.append((v, Buf()))
        Er = Ring(Et)
        rcs = []
        for i in range(2):
            v, p = self.ovv(p, 512, F32)
            rcs.append((v, Buf()))
        rcr = Ring(rcs)
        ess = []
        for i in range(4):
            v, p = self.ovv(p, 512, F32)
            ess.append((v, Buf()))
        esr = Ring(ess)
        Sr = Ring([(self.pb[i], self.PB[i]) for i in (0, 1, 2)])
        Or = Ring([(self.pb[i], self.PB[i]) for i in (3, 4)])
        Dr = Ring([(self.pb[i], self.PB[i]) for i in (5, 6)])
        vmv = self.vm.rearrange("(t p) f -> p t f", p=128)
        scale = float(192 ** -0.5)

        def loads(h):
            d_ = Ld[h % 2]
            self.dma("sp", d_["qn"][0], self.qT[h, 0:128, :], [self.qTB[h]], [d_["qn"][1]])
            self.dma("sp", d_["qr"][0][0:64, :], self.qT[h, 128:192, :], [self.qTB[h]], [d_["qr"][1]])
            self.dma("sp", d_["kn"][0], self.knT[h], [self.knTB[h]], [d_["kn"][1]])
            self.dma("sp", d_["vh"][0].rearrange("p (t f) -> p t f", f=128), vmv[:, :, h * 128:(h + 1) * 128], [self.vmB[h // 4]], [d_["vh"][1]])
        loads(0)
        for h in range(8):
            if h + 1 < 8:
                loads(h + 1)
            d_ = Ld[h % 2]
            qn, qnB = d_["qn"]; qr, qrB = d_["qr"]; kn, knB = d_["kn"]; vh, vhB = d_["vh"]
            vh3 = vh.rearrange("p (t f) -> p t f", f=128)
            for qb in range(NTB):
                qs = slice(qb * 512, (qb + 1) * 512)
                ob, obB = Or.next()
                db, dbB = Dr.next()

                def smm(kt):
                    sb_, sbB = Sr.next()
                    ks = slice(kt * 128, (kt + 1) * 128)
                    self.mm(sb_[:, :], kn[:, ks], qn[:, qs], True, False, [knB, qnB], [sbB], signal=False)
                    self.mm(sb_[:, :], self.krT[0:64, ks], qr[0:64, qs], False, True, [self.krB[kt // 4], qrB], [sbB])
                    e, eB = Er.next()
                    self.act(e, sb_[:, :], AF.Exp, [sbB], [eB], scale=scale)
                    return e, eB
                esA, esAB = esr.next()
                esBt, esBB = esr.next()
                cur, nxt = smm(0), smm(1)
                for kt in range(NTT):
                    nn = smm(kt + 2) if kt + 2 < NTT else None
                    e, eB = cur
                    self.mm(ob[:, :], vh3[:, kt, :], e, kt == 0, kt == NTT - 1, [vhB, eB], [obB], signal=True)
                    if kt == 0:
                        self.cp("dve", esA, e, [eB], [esAB])
                    elif kt == 1:
                        self.cp("pool", esBt, e, [eB], [esBB])
                    elif kt % 2 == 0:
                        self.tt("dve", esA, esA, e, ALU.add, [eB, esAB], [esAB])
                    else:
                        self.tt("pool", esBt, esBt, e, ALU.add, [eB, esBB], [esBB])
                    cur, nxt = nxt, nn
                self.mm(db[:, :], self.onesf[:], esA, True, False, [esAB, self.onesfB], [dbB], signal=False)
                self.mm(db[:, :], self.onesf[:], esBt, False, True, [esBB, self.onesfB], [dbB])
                r, rB = rcr.next()
                self.recip(r, db[:, :], [dbB], [rB])
                self.tt("dve", self.RA[:, h, qs], ob[:, :], r, ALU.mult, [obB, rB], [self.RAb[h][qb]])
        self.dbg_dump_y(0)

    def dbg_dump_y(self, b, kp=128):
        if self.ydbg is None:
            return
        for k in range(KC):
            self.dma("sp", self.ydbg[b, k * 128:k * 128 + kp, :], self.RA[0:kp, k, :], [self.RAb[k][t] for t in range(NTB)], [self.dbgB])

    def branch_out(self, l, b):
        kp = 64 if b == 1 else 128
        if b == 0:
            src = self.wbm_d[l].rearrange("(k p) n -> p k n", p=128)
        elif b == 1:
            src = self.wbd_d[l].rearrange("(k p) n -> p k n", p=64)
        else:
            src = self.wbr_d[l].rearrange("(k p) n -> p k n", p=128)
        Wb, wbb = self._br_pre[b]
        last = (b == 2)
        if last:
            Wo, wob = self._wout_pre
        p = 0
        xi = []
        for i in range(2):
            v, p = self.ovv(p, KC * 512, F32)
            xi.append((v.rearrange("p (k t) -> p k t", k=KC), Buf()))
        xir = Ring(xi)
        mi = []
        for i in range(2):
            v, p = self.ovv(p, KC * 512, F32)
            mi.append((v.rearrange("p (k t) -> p k t", k=KC), Buf()))
        mir = Ring(mi)
        gts = []
        for i in range(2):
            v, p = self.ovv(p, KC * 512)
            gts.append((v.rearrange("p (k t) -> p k t", k=KC), Buf()))
        gtr = Ring(gts)
        ms = []
        for i in range(2):
            v, p = self.ovv(p, KC * 512)
            ms.append((v.rearrange("p (k t) -> p k t", k=KC), Buf()))
        msr = Ring(ms)
        tps = []
        for i in range(2):
            v, p = self.ovv(p, 512, F32)
            tps.append((v, Buf()))
        tpr = Ring(tps)
        xsv = self.xs.rearrange("k p t -> p k t")
        mgv = self.mg.rearrange("k p t -> p k t")
        gv = self.gT[b].rearrange("(o p) t -> p o t", p=128)
        gate1 = self.modT[:, l, 16:24]
        pre = {}

        def load(tb):
            ts_ = slice(tb * 512, (tb + 1) * 512)
            g, gB = gtr.next()
            self.dma("sp", g, gv[:, :, ts_], self.gTB[b], [gB])
            mgt, mgB = mir.next()
            if b > 0:
                self.dma("sp", mgt, mgv[:, :, ts_], [self.mgB[tb]], [mgB])
            x, xB = (None, None)
            if last:
                x, xB = xir.next()
                self.dma("sp", x, xsv[:, :, ts_], [self.xsB[tb]], [xB])
            pre[tb] = (x, xB, g, gB, mgt, mgB)
        load(0)
        for tb in range(NTB):
            if tb + 1 < NTB:
                load(tb + 1)
            x, xB, g, gB, mgt, mgB = pre.pop(tb)
            ts_ = slice(tb * 512, (tb + 1) * 512)
            if last:
                m, mB = msr.next()
            for o in range(KC):
                bk, bb = self.allbanks.next()
                for kc in range(KC):
                    self.mm(bk[:, :], Wb[0:kp, kc, o * 128:(o + 1) * 128], self.RA[0:kp, kc, ts_], kc == 0, kc == KC - 1,
                            wbb + [self.RAb[kc][tb]], [bb], signal=(kc == KC - 1))
                if b == 0:
                    self.tt("dve", mgt[:, o, :], bk[:, :], g[:, o, :], ALU.mult, [bb, gB], [mgB])
                else:
                    t_, tB = tpr.next()
                    self.tt("dve", t_, bk[:, :], g[:, o, :], ALU.mult, [bb, gB], [tB])
                    if last:
                        self.tt("pool", m[:, o, :], t_, mgt[:, o, :], ALU.add, [tB, mgB], [mB])
                    else:
                        self.tt("pool", mgt[:, o, :], t_, mgt[:, o, :], ALU.add, [tB, mgB], [mgB])
            if not last:
                self.dma("sp", mgv[:, :, ts_], mgt, [mgB], [self.mgB[tb]])
                continue
            for o2 in range(KC):
                bk, bb = self.allbanks.next()
                for o in range(KC):
                    self.mm(bk[:, :], Wo[:, o, o2 * 128:(o2 + 1) * 128], m[:, o, :], o == 0, o == KC - 1, wob + [mB], [bb], signal=(o == KC - 1))
                self.stt("dve", x[:, o2, :], bk[:, :], gate1[:, o2:o2 + 1], x[:, o2, :], ALU.mult, ALU.add, [bb, xB, self.modB], [xB])
            self.dma("sp", xsv[:, :, ts_], x, [xB], [self.xsB[tb]])

    def dil_attn(self, l):
        p = 0
        va = []
        for g in range(3):
            v, p = self.ovv(p, NTT * 520)
            va.append((v.rearrange("p (t f) -> p t f", f=520), Buf()))
            self.dma("sp", va[g][0], self.dv[g].rearrange("(t p) f -> p t f", p=128), [self.dvB[g]], [va[g][1]])
        qk = []
        for g in range(3):
            q_, p = self.ovv(p, SEQ)
            k_, p = self.ovv(p, SEQ)
            qk.append((q_, Buf(), k_, Buf()))
        accs = []
        for i in range(2):
            v, p = self.ovv(p, SEQ, F32)
            accs.append((v, Buf()))
        Et = []
        for i in range(4):
            v, p = self.ovv(p, 384)
            Et.append((v, Buf()))
        Er = Ring(Et)
        rrv, p = self.ovv(p, SEQ, F32)
        rrB = Buf()
        bcs = []
        for i in range(2):
            v, p = self.ovv(p, 512, F32)
            bcs.append((v, Buf()))
        bcr = Ring(bcs)
        Sr = Ring([(self.pb[i], self.PB[i]) for i in (0, 1, 2)])
        obs = [(self.pb[i], self.PB[i]) for i in (3, 4, 5, 6)]
        Br = Ring([(self.pb[i], self.PB[i]) for i in (7,)])

        def loads(hp):
            for g in range(3):
                q_, qB, k_, kB = qk[g]
                self.dma("sp", q_, self.dqT[g, hp * 128:(hp + 1) * 128, :], [self.dqTB[g][hp]], [qB])
                self.dma("sp", k_, self.dkT[g, hp * 128:(hp + 1) * 128, :], [self.dkTB[g][hp]], [kB])
        for hp in range(4):
            loads(hp)
            for hh in range(2):
                h = 2 * hp + hh
                rows = slice(64 * hh, 64 * hh + 64)
                acc, accB = accs[hh]
                for g in range(3):
                    d = DIL_D[g]
                    TPR = (SEQ // d) // 128
                    q_, qB, k_, kB = qk[g]
                    vg, vgB = va[g]

                    def qblocks(n):
                        return [i for i in (n - 1, n, n + 1) if 0 <= i < NTT and i // TPR == n // TPR]

                    def s_stage(n):
                        qb_ = qblocks(n)
                        qlo, qhi = qb_[0] * 128, (qb_[-1] + 1) * 128
                        W = qhi - qlo
                        m0 = (qb_[0] - (n - 1)) * 128
                        sb_, sbB = Sr.next()
                        self.mm(sb_[:, 0:W], k_[rows, n * 128:(n + 1) * 128], q_[rows, qlo:qhi], True, False, [kB, qB], [sbB], signal=False)
                        self.mm(sb_[:, 0:W], self.ident_b, self.negmT_b[:, m0:m0 + W], False, True, [self.cbfB], [sbB], signal=True)
                        e, eB = Er.next()
                        self.act(e[:, 0:W], sb_[:, 0:W], AF.Exp, [sbB], [eB], scale=0.125)
                        return (e, eB, qlo, qhi)
                    last_n = [max(n for n in range(NTT) if any(i // 4 == b4 for i in qblocks(n))) for b4 in range(4)]
                    stages = {0: s_stage(0), 1: s_stage(1)}
                    started = [False] * 4
                    for n in range(NTT):
                        if n + 2 < NTT:
                            stages[n + 2] = s_stage(n + 2)
                        e, eB, qlo, qhi = stages.pop(n)
                        c = qlo
                        while c < qhi:
                            b4 = c // 512
                            ce = min(qhi, (b4 + 1) * 512)
                            ob, obB = obs[b4]
                            self.mm(ob[0:65, c - b4 * 512:ce - b4 * 512], vg[:, n, h * 65:(h + 1) * 65], e[:, c - qlo:ce - qlo],
                                    not started[b4], False, [vgB, eB], [obB], signal=True)
                            started[b4] = True
                            c = ce
                        for b4 in range(4):
                            if last_n[b4] != n:
                                continue
                            ob, obB = obs[b4]
                            if g == 0:
                                self.cp("dve", acc[0:65, b4 * 512:(b4 + 1) * 512], ob[0:65, :], [obB], [accB])
                            elif g == 1:
                                dst = acc[0:65, :].rearrange("p (j r) -> p r j", r=4)[:, b4, :]
                                self.tt("dve", dst, dst, ob[0:65, :], ALU.add, [obB, accB], [accB])
                            else:
                                dst = acc[0:65, :].rearrange("p (j r) -> p r j", r=16)[:, 4 * b4:4 * b4 + 4, :]
                                self.tt("dve", dst, dst, ob[0:65, :].rearrange("p (b j) -> p b j", b=4), ALU.add, [obB, accB], [accB])
                self.act(rrv[64:65, :], acc[64:65, :], AF.Ln, [accB], [rrB])
                self.act(rrv[64:65, :], rrv[64:65, :], AF.Exp, [rrB], [rrB], scale=-1.0)
                for tb in range(NTB):
                    ts_ = slice(tb * 512, (tb + 1) * 512)
                    bk, bb = Br.next()
                    self.mm(bk[0:64, :], self.onesf[64:65, 0:64], rrv[64:65, ts_], True, True, [rrB, self.onesfB], [bb])
                    bc, bcB = bcr.next()
                    self.cp("act", bc[0:64, :], bk[0:64, :], [bb], [bcB])
                    self.tt("dve", self.RA[0:64, h, ts_], acc[0:64, ts_], bc[0:64, :], ALU.mult, [accB, bcB], [self.RAb[h][tb]])
        self.dbg_dump_y(1, 64)

    def ret_attn(self, l):
        p = 0
        dpos, p = self.ovv(p, STRIPW, F32)
        dneg, p = self.ovv(p, STRIPW, F32)
        dB = Buf()
        self.dma("sp", dpos, self.dpn_d[0], [], [dB])
        self.dma("sp", dneg, self.dpn_d[1], [], [dB])
        HW_ = STRIPW // 2
        ef, p = self.ovv(p, HW_)
        eb, p = self.ovv(p, HW_)
        efB, ebB = Buf(), Buf()
        strips = []
        for i in range(2):
            v, p = self.ovv(p, STRIPW)
            strips.append((v, Buf()))
        qk = []
        for i in range(2):
            q_, p = self.ovv(p, SEQ)
            k_, p = self.ovv(p, SEQ)
            qk.append((q_, Buf(), k_, Buf()))
        hv = []
        for i in range(2):
            v_, p = self.ovv(p, NTT * 128)
            g_, p = self.ovv(p, SEQ)
            hv.append((v_.rearrange("p (t f) -> p t f", f=128), Buf(), g_, Buf()))
        SD = []
        for i in range(6):
            v, p = self.ovv(p, 512)
            SD.append((v, Buf()))
        SDr = Ring(SD)
        SC = []
        for i in range(3):
            v, p = self.ovv(p, 512)
            SC.append((v, Buf()))
        SCr = Ring(SC)
        ysbs, sqs = [], []
        for i in range(2):
            v, p = self.ovv(p, 512, F32)
            ysbs.append((v, Buf()))
            v, p = self.ovv(p, 512, F32)
            sqs.append((v, Buf()))
        ysbr, sqr_ = Ring(ysbs), Ring(sqs)
        tmp = {}
        for nm in ("msq", "var", "rstd"):
            v, p = self.ovv(p, 512, F32)
            tmp[nm] = (v, Buf())
        Sr = Ring([(self.pb[i], self.PB[i]) for i in (0, 1, 2)])
        Yr = Ring([(self.pb[i], self.PB[i]) for i in (3, 4)])
        Mr = Ring([(self.pb[i], self.PB[i]) for i in (5,)])
        Vb, VbB = self.pb[6], self.PB[6]
        dmy, dmyB = self.pb[7], self.PB[7]
        rvv = self.rv.rearrange("(t p) f -> p t f", p=128)

        def loads_pair(hp):
            q_, qB, k_, kB = qk[hp % 2]
            self.dma("sp", q_, self.rqT[hp * 128:(hp + 1) * 128, :], [self.rqTB[hp]], [qB])
            self.dma("sp", k_, self.rkT[hp * 128:(hp + 1) * 128, :], [self.rkTB[hp]], [kB])

        def loads_head(h):
            v_, vB, g_, gB = hv[h % 2]
            self.dma("sp", v_, rvv[:, :, h * 128:(h + 1) * 128], [self.rvB[h // 4]], [vB])
            self.dma("sp", g_, self.rgT[h * 128:(h + 1) * 128, :], [self.rgTB[h]], [gB])

        def strip_steps(h):
            strip, stripB = strips[h % 2]
            lgf = self.rldT[:, l * 16 + h:l * 16 + h + 1]
            nlgb = self.rldT[:, l * 16 + 8 + h:l * 16 + 8 + h + 1]
            steps = []
            for half in range(2):
                cs = slice(half * HW_, (half + 1) * HW_)
                steps.append(lambda cs=cs: self.act(ef, dpos[:, cs], AF.Exp, [dB, self.rldB], [efB], scale=lgf))
                steps.append(lambda cs=cs: self.act(eb, dneg[:, cs], AF.Exp, [dB, self.rldB], [ebB], scale=nlgb))
                steps.append(lambda cs=cs: self.tt("dve", strip[:, cs], ef, eb, ALU.mult, [efB, ebB], [stripB]))
            return steps

        def stats_steps(h, qb, yb, ybB, g_, gB):
            qs = slice(qb * 512, (qb + 1) * 512)
            ysb, ysbB = ysbr.next()
            sq, sqB = sqr_.next()
            msq, msqB = tmp["msq"]; var, varB = tmp["var"]; rstd, rstdB = tmp["rstd"]
            st = {}

            def s3():
                st["mb"], st["mbB"] = Mr.next()
                self.mm(st["mb"][:, :], self.avgf[:], ysb, True, True, [ysbB, self.onesfB], [st["mbB"]])
                self.mm(Vb[:, :], self.avgf[:], sq, True, True, [sqB, self.onesfB], [VbB])
            return [
                lambda: self.cp("act", ysb, yb[:, :], [ybB], [ysbB]),
                lambda: self.act(sq, yb[:, :], AF.Square, [ybB], [sqB]),
                s3,
                lambda: self.act(msq, st["mb"][:, :], AF.Square, [st["mbB"]], [msqB]),
                lambda: self.tt("dve", var, Vb[:, :], msq, ALU.subtract, [VbB, msqB], [varB]),
                lambda: self.ts("dve", var, var, 0.0, EPS, ALU.max, ALU.add, [varB], [varB]),
                lambda: self.act(rstd, var, AF.Ln, [varB], [rstdB]),
                lambda: self.act(rstd, rstd, AF.Exp, [rstdB], [rstdB], scale=-0.5),
                lambda: self.tt("dve", ysb, ysb, st["mb"][:, :], ALU.subtract, [ysbB, st["mbB"]], [ysbB]),
                lambda: self.tt("pool", ysb, ysb, rstd, ALU.mult, [ysbB, rstdB], [ysbB]),
                lambda: self.stt("dve", self.RA[:, h, qs], ysb, self.vecT[:, l, 74 + h:75 + h], g_[:, qs], ALU.mult, ALU.mult,
                                 [ysbB, self.vecB, gB], [self.RAb[h][qb]]),
            ]
        loads_pair(0)
        loads_head(0)
        for s in strip_steps(0):
            s()
        pending = []
        for h in range(8):
            hp, hh = h // 2, h % 2
            if hh == 0 and hp + 1 < 4:
                loads_pair(hp + 1)
            rows = slice(64 * hh, 64 * hh + 64)
            q_, qB, k_, kB = qk[hp % 2]
            v_, vB, g_, gB = hv[h % 2]
            strip, stripB = strips[h % 2]
            for qb in range(NTB):
                qs = slice(qb * 512, (qb + 1) * 512)
                yb, ybB = Yr.next()
                if qb == 1 and h + 1 < 8:
                    loads_head(h + 1)
                    pending.extend(strip_steps(h + 1))

                def smm(kt):
                    sb_, sbB = Sr.next()
                    self.mm(sb_[:, :], k_[rows, kt * 128:(kt + 1) * 128], q_[rows, qs], True, True, [kB, qB], [sbB])
                    sd, sdB = SDr.next()
                    off = 512 * qb - 128 * kt + 1920
                    if kt % 2 == 0:
                        self.tt("dve", sd, sb_[:, :], strip[:, off:off + 512], ALU.mult, [sbB, stripB], [sdB])
                    else:
                        sc, scB = SCr.next()
                        self.cp("act", sc, sb_[:, :], [sbB], [scB])
                        self.tt("dve", sd, sc, strip[:, off:off + 512], ALU.mult, [scB, stripB], [sdB])
                    return sd, sdB
                cur, nxt = smm(0), smm(1)
                for kt in range(NTT):
                    nn = smm(kt + 2) if kt + 2 < NTT else None
                    sd, sdB = cur
                    self.mm(yb[:, :], v_[:, kt, :], sd, kt == 0, kt == NTT - 1, [vB, sdB], [ybB], signal=True)
                    cur, nxt = nxt, nn
                    if pending:
                        pending.pop(0)()
                while pending:
                    pending.pop(0)()
                pending.extend(stats_steps(h, qb, yb, ybB, g_, gB))
        while pending:
            pending.pop(0)()
        self.dbg_dump_y(2)

    def ffn(self, l):
        moe = (l % 2 == 1)
        li = l // 2
        NF = (D_FFE if moe else D_FF) // 128
        nexp = NEXP if moe else 1
        p0 = 0
        gTt, p0 = self.ovv(p0, NF * 1024)
        gTt = gTt.rearrange("p (f t) -> p f t", t=1024)
        gB = [[Buf() for _ in range(2)] for _ in range(NF)]
        facc, p0 = self.ovv(p0, KC * 1024, F32)
        facc = facc.rearrange("p (k t) -> p k t", t=1024)
        faccB = [[Buf() for _ in range(2)] for _ in range(KC)]
        gate2 = self.modT[:, l, 40:48]
        router = None
        if moe:
            wr, p0 = self.ovv(p0, KC * 8, F32)
            wr = wr.rearrange("p (k e) -> p k e", e=8)
            wrB = Buf()
            self.dma("sp", wr, self.mr_d[li].rearrange("(k p) e -> p k e", p=128), [], [wrB])
            lg, p0 = self.ovv(p0, 64, F32)
            lg = lg.rearrange("p (a e) -> p a e", e=8)
            lgB = Buf()
            router = {"w": (wr, wrB), "lg": (lg, lgB)}
            small = {}
            for nm, n in (("m1", 8), ("m2", 8), ("eq1", 64), ("eq2", 64), ("lg2", 64), ("dm", 8), ("w1", 8), ("w2", 8), ("wts", 64)):
                v, p0 = self.ovv(p0, n, F32)
                small[nm] = v
            smB = Buf()
            dg, p0 = self.ovv(p0, 128, F32)
            dgs = [(dg, Buf())]
            v, p0 = self.ovv(p0, 128, F32)
            dgs.append((v, Buf()))
            dgr = Ring(dgs)
            wbt, p0 = self.ovv(p0, NEXP * 1024)
            wbt = wbt.rearrange("p (e t) -> p e t", t=1024)
            wbB = [Buf() for _ in range(NEXP)]
        tms = []
        for i in range(2):
            s_, p0 = self.ovv(p0, 512, F32)
            g0, p0 = self.ovv(p0, 512)
            tms.append((s_, Buf(), g0, Buf()))
        tmr = Ring(tms)
        pn = 0

        steps = [(f0, min(4, NF - f0)) for f0 in range(0, NF, 4)]

        def wviews(ex):
            if moe:
                return (self.m1_d[li, ex].rearrange("(k p) n -> p k n", p=128), self.m3_d[li, ex].rearrange("(k p) n -> p k n", p=128),
                        self.m2_d[li, ex].rearrange("(f p) n -> p f n", p=128))
            return (self.f1_d[li].rearrange("(k p) n -> p k n", p=128), self.f3_d[li].rearrange("(k p) n -> p k n", p=128),
                    self.f2_d[li].rearrange("(f p) n -> p f n", p=128))
        jobs = []
        jidx = {}
        for ex in range(nexp):
            w1v, w3v, w2v = wviews(ex)
            for i, (f0, nf) in enumerate(steps):
                jidx[("f1", ex, i)] = len(jobs)
                jobs.append([(w1v[:, :, f0 * 128:(f0 + nf) * 128], KC, nf * 128), (w3v[:, :, f0 * 128:(f0 + nf) * 128], KC, nf * 128)])
            for o in range(KC):
                jidx[("f2", ex, o)] = len(jobs)
                jobs.append([(w2v[:, :, o * 128:(o + 1) * 128], NF, 128)])

        for hf in range(2):
            tbs = [2 * hf, 2 * hf + 1]
            ws = WStream(self, jobs)
            ws._try_issue()
            self.norm_phase(l, 1, tbs, pn, router=router)
            if moe:
                lg3 = lg
                m1, m2, eq1, eq2, lg2, dm, w1, w2, wts = (small[k] for k in ("m1", "m2", "eq1", "eq2", "lg2", "dm", "w1", "w2", "wts"))
                e3 = lambda v: v.rearrange("p (a e) -> p a e", e=8)
                b3 = lambda v: v.unsqueeze(2).to_broadcast([128, 8, 8])
                self.S.op("dve", lambda e: e.tensor_reduce(out=m1, in_=lg3, axis=mybir.AxisListType.X, op=ALU.max), [lgB], [smB])
                self.tt("dve", e3(eq1), lg3, b3(m1), ALU.is_ge, [lgB, smB], [smB])
                self.stt("dve", e3(lg2), e3(eq1), -1e30, lg3, ALU.mult, ALU.add, [smB, lgB], [smB])
                self.S.op("dve", lambda e: e.tensor_reduce(out=m2, in_=e3(lg2), axis=mybir.AxisListType.X, op=ALU.max), [smB], [smB])
                self.tt("dve", e3(eq2), e3(lg2), b3(m2), ALU.is_ge, [smB], [smB])
                self.tt("dve", dm, m1, m2, ALU.subtract, [smB], [smB])
                self.act(w1, dm, AF.Sigmoid, [smB], [smB])
                self.act(w2, dm, AF.Sigmoid, [smB], [smB], scale=-1.0)
                self.tt("dve", e3(eq1), e3(eq1), b3(w1), ALU.mult, [smB], [smB])
                self.tt("dve", e3(eq2), e3(eq2), b3(w2), ALU.mult, [smB], [smB])
                self.tt("dve", e3(wts), e3(eq1), e3(eq2), ALU.add, [smB], [smB])
                for ex in range(NEXP):
                    for a4 in range(2):
                        bk, bb = self.allbanks.next()
                        for a in range(4):
                            ta = a4 * 4 + a
                            dgt, dgB = dgr.next()
                            self.ts("dve", dgt, self.ident_f, e3(wts)[:, ta, ex:ex + 1], None, ALU.mult, None, [smB, self.cstB], [dgB])
                            self.mm(bk[:, a * 128:(a + 1) * 128], self.onesf[:], dgt, True, True, [dgB, self.onesfB], [bb])
                        self.cp("act", wbt[:, ex, a4 * 512:(a4 + 1) * 512], bk[:, :], [bb], [wbB[ex]])
            self.S.barrier()
            for ex in range(nexp):
                if moe:
                    w1v = self.m1_d[li, ex].rearrange("(k p) n -> p k n", p=128)
                    w3v = self.m3_d[li, ex].rearrange("(k p) n -> p k n", p=128)
                    w2v = self.m2_d[li, ex].rearrange("(f p) n -> p f n", p=128)
                else:
                    w1v = self.f1_d[li].rearrange("(k p) n -> p k n", p=128)
                    w3v = self.f3_d[li].rearrange("(k p) n -> p k n", p=128)
                    w2v = self.f2_d[li].rearrange("(f p) n -> p f n", p=128)
                for i, (f0, nf) in enumerate(steps):
                    jn = jidx[("f1", ex, i)]
                    (W1, w1b), (W3, w3b) = ws.get(jn)
                    for j in range(nf):
                        f = f0 + j
                        for t2 in range(2):
                            tb = tbs[t2]
                            ts_ = slice(tb * 512, (tb + 1) * 512)
                            b1, b1B = self.allbanks.next()
                            b3_, b3B = self.allbanks.next()
                            for kc in range(KC):
                                self.mm(b1[:, :], W1[:, kc, j * 128:(j + 1) * 128], self.RA[:, kc, ts_], kc == 0, kc == KC - 1,
                                        w1b + [self.RAb[kc][tb]], [b1B], signal=(kc == KC - 1))
                            for kc in range(KC):
                                self.mm(b3_[:, :], W3[:, kc, j * 128:(j + 1) * 128], self.RA[:, kc, ts_], kc == 0, kc == KC - 1,
                                        w3b + [self.RAb[kc][tb]], [b3B], signal=(kc == KC - 1))
                            s_, sB, g0, g0B = tmr.next()
                            self.act(s_, b1[:, :], AF.Silu, [b1B], [sB])
                            gd = gTt[:, f, t2 * 512:(t2 + 1) * 512]
                            if moe:
                                self.tt("dve", g0, b3_[:, :], s_, ALU.mult, [b3B, sB], [g0B])
                                self.tt("pool", gd, g0, wbt[:, ex, t2 * 512:(t2 + 1) * 512], ALU.mult, [g0B, wbB[ex]], [gB[f][t2]])
                            else:
                                self.tt("dve", gd, b3_[:, :], s_, ALU.mult, [b3B, sB], [gB[f][t2]])
                    ws.finish(jn)
                for o in range(KC):
                    jn = jidx[("f2", ex, o)]
                    ((W2, w2b),) = ws.get(jn)
                    for t2 in range(2):
                        bk, bb = self.allbanks.next()
                        for f in range(NF):
                            self.mm(bk[:, :], W2[:, f, :], gTt[:, f, t2 * 512:(t2 + 1) * 512], f == 0, f == NF - 1, w2b + [gB[f][t2]], [bb],
                                    signal=(f == NF - 1))
                        fa = facc[:, o, t2 * 512:(t2 + 1) * 512]
                        if ex == 0:
                            self.cp("act", fa, bk[:, :], [bb], [faccB[o][t2]])
                        else:
                            self.tt("dve", fa, fa, bk[:, :], ALU.add, [bb, faccB[o][t2]], [faccB[o][t2]])
                    ws.finish(jn)
            xsv = self.xs.rearrange("k p t -> p k t")
            pp = pn
            xi = []
            for i in range(2):
                v, pp = self.ovv(pp, KC * 512, F32)
                xi.append((v.rearrange("p (k t) -> p k t", k=KC), Buf()))
            self.S.barrier()
            for t2 in range(2):
                tb = tbs[t2]
                x, xB = xi[t2]
                self.dma("sp", x, xsv[:, :, tb * 512:(tb + 1) * 512], [self.xsB[tb]], [xB])
                for o in range(KC):
                    self.stt("dve", x[:, o, :], facc[:, o, t2 * 512:(t2 + 1) * 512], gate2[:, o:o + 1], x[:, o, :], ALU.mult, ALU.add,
                             [faccB[o][t2], xB, self.modB], [xB])
                self.dma("sp", xsv[:, :, tb * 512:(tb + 1) * 512], x, [xB], [self.xsB[tb]])
            self.S.barrier()

    def final(self):
        p = 0
        xi = []
        for i in range(2):
            v, p = self.ovv(p, KC * 512, F32)
            xi.append((v.rearrange("p (k t) -> p k t", k=KC), Buf()))
        xir = Ring(xi)
        sq = []
        for i in range(2):
            v, p = self.ovv(p, 512)
            sq.append((v, Buf()))
        sqr = Ring(sq)
        rt = []
        for i in range(2):
            v, p = self.ovv(p, 512, F32)
            rt.append((v, Buf()))
        rtr = Ring(rt)
        tm = []
        for i in range(3):
            v, p = self.ovv(p, 512, F32)
            tm.append((v, Buf()))
        tmr = Ring(tm)
        ob = []
        for i in range(2):
            v, p = self.ovv(p, 4 * D, F32)
            ob.append((v.rearrange("p (a k m) -> p a k m", a=4, k=KC), Buf()))
        obr = Ring(ob)
        xsv = self.xs.rearrange("k p t -> p k t")
        ov = self.out_d.rearrange("(a p) d -> p a d", p=128)
        for tb in range(NTB):
            x, xB = xir.next()
            self.dma("sp", x, xsv[:, :, tb * 512:(tb + 1) * 512], [self.xsB[tb]], [xB])
            bk, bb = self.allbanks.next()
            for kc in range(KC):
                s, sB = sqr.next()
                self.act(s, x[:, kc, :], AF.Square, [xB], [sB])
                self.mm(bk[:, :], self.ones_b, s, kc == 0, kc == KC - 1, [sB, self.cbfB], [bb], signal=True)
            r, rB = rtr.next()
            self.act(r, bk[:, :], AF.Sqrt, [bb, self.cstB], [rB], scale=1.0 / D, bias=self.eps_ap)
            self.recip(r, r, [rB], [rB])
            o_, oB = obr.next()
            for kc in range(KC):
                t, tB = tmr.next()
                self.stt("dve", t, x[:, kc, :], self.finT[:, kc:kc + 1], r, ALU.mult, ALU.mult, [xB, rB, self.vecB], [tB])
                b2, b2B = self.allbanks.next()
                for a in range(4):
                    self.tr(b2[:, a * 128:(a + 1) * 128], t[:, a * 128:(a + 1) * 128], self.ident_f, [tB, self.cstB], [b2B])
                self.cp("act", o_[:, :, kc, :], b2[:, :].rearrange("p (a m) -> p a m", a=4), [b2B], [oB])
            self.dma("sp", ov[:, 4 * tb:4 * tb + 4, :], o_.rearrange("p a k m -> p a (k m)"), [oB], [self.outB[tb]])


_CACHE = {}


def _vecpack(inputs):
    vp = np.zeros((DEPTH, NVEC, 128), np.float32)
    for l in range(DEPTH):
        vp[l, 0:48] = np.asarray(inputs["ada_b"][l], np.float32).reshape(48, 128)
        vp[l, 48:56] = np.asarray(inputs["norm_mix"][l], np.float32).reshape(8, 128)
        vp[l, 56:64] = np.asarray(inputs["norm_ffn"][l], np.float32).reshape(8, 128)
        vp[l, 64:70] = np.asarray(inputs["mla_q_norm"][l], np.float32).reshape(6, 128)
        vp[l, 70:74] = np.asarray(inputs["mla_kv_norm"][l], np.float32).reshape(4, 128)
        vp[l, 74:82] = np.asarray(inputs["ret_norm"][l], np.float32).reshape(8, 128)
    return vp


def make_in_maps(inputs, ncores=NCORES):
    cst, dpn = make_consts()
    f = lambda k: np.ascontiguousarray(np.asarray(inputs[k], np.float32))
    shared = {
        "ada_w": f("ada_w"), "vecpack": _vecpack(inputs), "final_norm": f("final_norm").reshape(8, 128),
        "w_in": f("w_in"), "mla_w_uq": f("mla_w_uq"), "mla_w_ukv": f("mla_w_ukv"),
        "ret_log_decay": f("ret_log_decay").reshape(1, DEPTH * 16),
        "w_br_mla": f("w_br_mla"), "w_br_dil": f("w_br_dil"), "w_br_ret": f("w_br_ret"), "w_out": f("w_out"),
        "ffn_w1": f("ffn_w1"), "ffn_w3": f("ffn_w3"), "ffn_w2": f("ffn_w2"),
        "moe_router": f("moe_router"), "moe_w1": f("moe_w1"), "moe_w3": f("moe_w3"), "moe_w2": f("moe_w2"),
        "consts": cst, "dposneg": dpn,
    }
    x = f("x")
    c = f("c")
    pos = np.ascontiguousarray(np.asarray(inputs["positions"], np.int32))
    maps = []
    for b in range(ncores):
        m = dict(shared)
        m["x"] = x[b]
        m["c"] = c[b:b + 1]
        m["positions"] = pos[b:b + 1]
        maps.append(m)
    return maps


def kernel(**inputs):
    if "nc" not in _CACHE:
        _CACHE["nc"] = Builder().build()
    nc = _CACHE["nc"]
    maps = make_in_maps(inputs)
    res = run_bass_kernel_spmd(nc, maps, core_ids=list(range(NCORES)))
    return np.stack([np.asarray(r["out"], np.float32) for r in res.results], axis=0)
```

```python
import numpy as np
from contextlib import ExitStack
import concourse.bass as bass
import concourse.mybir as mybir
from concourse.bass_utils import run_bass_kernel_spmd

F32 = mybir.dt.float32
BF16 = mybir.dt.bfloat16
I32 = mybir.dt.int32
AF = mybir.ActivationFunctionType
ALU = mybir.AluOpType

D = 1024
SEQ = 2048
DEPTH = 4
NCORES = 8
KC = 8
NTB = 4
NTT = 16
EPS = 1e-6
IN_COLS = 12096
C_CQ, C_CKV, C_KR, C_DQ, C_DK, C_DV, C_RQ, C_RK, C_RV, C_RG, C_GA, C_GB, C_GC = (
    0, 768, 1280, 1344, 2880, 4416, 5952, 6464, 6976, 8000, 9024, 10048, 11072)
DIL_D = (1, 4, 16)
D_FF = 2816
D_FFE = 3584
NEXP = 8
STRIPW = 3968
NVEC = 82

SEM_LIMIT = 30000
DMA_SEMS_PER_QUEUE = 20


class Buf:
    __slots__ = ("name", "w", "r")

    def __init__(self, name=""):
        self.name = name
        self.w = {}
        self.r = {}


class Sched:
    ENGS = ("pe", "act", "dve", "pool", "sp")

    def __init__(self, nc, stack, nsem=96):
        self.nc = nc
        self.sems = [stack.enter_context(nc.semaphore(f"s{i}")) for i in range(nsem)]
        self.sem_next = 0
        self.streams = {e: [] for e in self.ENGS}
        self.cur = {}
        for e in self.ENGS:
            self.cur[e] = [self._new_sem(), 0]
        self.seen = {e: {} for e in self.ENGS}
        self.dq = {}
        for q in ("sp", "pool"):
            self.dq[q] = {"sems": [[self._new_sem(), 0] for _ in range(DMA_SEMS_PER_QUEUE)], "next": 0}
        self.n_ins = 0

    def _new_sem(self):
        i = self.sem_next
        self.sem_next += 1
        assert i < len(self.sems), "out of semaphores"
        return i

    def _wait(self, eng, sidx, val):
        if self.seen[eng].get(sidx, 0) >= val:
            return
        self.seen[eng][sidx] = val
        sem = self.sems[sidx]
        self.streams[eng].append(lambda e, sem=sem, val=val: e.wait_ge(sem, val))

    def _collect(self, eng, reads, writes, same_engine_ok=False):
        deps = {}
        for b in reads:
            for s, v in b.w.items():
                if deps.get(s, 0) < v:
                    deps[s] = v
        for b in writes:
            for s, v in b.w.items():
                if deps.get(s, 0) < v:
                    deps[s] = v
            for s, v in b.r.items():
                if deps.get(s, 0) < v:
                    deps[s] = v
        own = self.cur[eng][0]
        for s, v in deps.items():
            if same_engine_ok and s == own:
                continue
            self._wait(eng, s, v)

    def _mark(self, ticket, reads, writes):
        s, v = ticket
        for b in reads:
            if b.r.get(s, 0) < v:
                b.r[s] = v
        for b in writes:
            b.w = {s: v}
            b.r = {}

    def op(self, eng, fn, reads=(), writes=(), signal=True):
        self._collect(eng, reads, writes, same_engine_ok=(eng == "pe"))
        cur = self.cur[eng]
        if signal:
            if cur[1] >= SEM_LIMIT:
                cur[0] = self._new_sem()
                cur[1] = 0
            cur[1] += 1
            sem = self.sems[cur[0]]
            self.streams[eng].append(lambda e, fn=fn, sem=sem: fn(e).then_inc(sem, 1))
            ticket = (cur[0], cur[1])
        else:
            assert eng == "pe"
            if cur[1] + 1 > SEM_LIMIT:
                cur[0] = self._new_sem()
                cur[1] = 0
            self.streams[eng].append(lambda e, fn=fn: fn(e))
            ticket = (cur[0], cur[1] + 1)
        self._mark(ticket, reads, writes)
        self.n_ins += 1
        return ticket

    def dma(self, q, fns, reads=(), writes=()):
        if not isinstance(fns, (list, tuple)):
            fns = [fns]
        self._collect(q, reads, writes)
        pool = self.dq[q]
        slot = pool["sems"][pool["next"]]
        pool["next"] = (pool["next"] + 1) % len(pool["sems"])
        if slot[1] > 0:
            self._wait(q, slot[0], slot[1])
        if slot[1] + 16 * len(fns) > SEM_LIMIT:
            slot[0] = self._new_sem()
            slot[1] = 0
        sem = self.sems[slot[0]]
        for fn in fns:
            slot[1] += 16
            self.streams[q].append(lambda e, fn=fn, sem=sem: fn(e).then_inc(sem, 16))
        ticket = (slot[0], slot[1])
        self._mark(ticket, reads, writes)
        self.n_ins += len(fns)
        return ticket

    def barrier(self):
        tickets = []
        for e in self.ENGS:
            c = self.cur[e]
            if c[1] > 0:
                tickets.append((c[0], c[1]))
        for q in self.dq.values():
            for s in q["sems"]:
                if s[1] > 0:
                    tickets.append((s[0], s[1]))
        for e in self.ENGS:
            for s, v in tickets:
                if s == self.cur[e][0] and e == "pe":
                    continue
                self._wait(e, s, v)

    def wait_all(self, eng, bufs):
        for b in bufs:
            for s, v in b.w.items():
                self._wait(eng, s, v)

    def emit(self):
        nc = self.nc
        streams = self.streams
        with nc.Block() as block:
            @block.tensor
            def _(e):
                for f in streams["pe"]:
                    f(e)

            @block.scalar
            def _(e):
                for f in streams["act"]:
                    f(e)

            @block.vector
            def _(e):
                for f in streams["dve"]:
                    f(e)

            @block.gpsimd
            def _(e):
                for f in streams["pool"]:
                    f(e)

            @block.sync
            def _(e):
                for f in streams["sp"]:
                    f(e)


class WStream:
    def __init__(self, bld, jobs):
        self.b = bld
        self.jobs = jobs
        self.views = {}
        self.nxt = 0
        self.done = -1
        self.slot_ctr = 0
        self.slot_job = [-1, -1, -1, -1]

    def _try_issue(self):
        while self.nxt < len(self.jobs):
            need = len(self.jobs[self.nxt])
            slots = [(self.slot_ctr + i) % 4 for i in range(need)]
            if any(self.slot_job[s_] > self.done for s_ in slots):
                return
            vs = []
            for s_, (src, nk, ncols) in zip(slots, self.jobs[self.nxt]):
                vs.append(self.b.wload([s_], src, 128, nk, ncols))
                self.slot_job[s_] = self.nxt
            self.slot_ctr = (self.slot_ctr + need) % 4
            self.views[self.nxt] = vs
            self.nxt += 1

    def get(self, j):
        self._try_issue()
        assert j in self.views, (j, self.nxt, self.done, self.slot_job)
        return self.views.pop(j)

    def finish(self, j):
        self.done = j
        self._try_issue()


class Ring:
    def __init__(self, items):
        self.items = items
        self.i = 0

    def next(self):
        it = self.items[self.i]
        self.i = (self.i + 1) % len(self.items)
        return it


def make_consts():
    c = np.zeros((128, 648), np.float32)
    c[:, 0:128] = np.eye(128, dtype=np.float32)
    for m in range(128):
        if m % 64 < 32:
            c[m + 32, 128 + m] = -1.0
        else:
            c[m - 32, 128 + m] = 1.0
    a = np.arange(128)[:, None]
    b = np.arange(128)[None, :]
    c[:, 256:384] = (a >= b + 64)
    c[:, 384:512] = (np.abs(a - b) <= 64)
    c[:, 512:640] = (a <= b - 64)
    inv_freq = (10000.0 ** (-np.arange(0, 64, 2, dtype=np.float32) / 64.0)).astype(np.float32)
    c[:, 640] = inv_freq[np.arange(128) % 32]
    c[:, 641] = EPS
    dm = (np.arange(STRIPW)[None, :] - 1920 - np.arange(128)[:, None]).astype(np.float32)
    dpn = np.stack([np.maximum(dm, 0.0), np.minimum(dm, 0.0)]).astype(np.float32)
    return c, dpn


class Builder:
    def __init__(self, n_layers=DEPTH, dbg=None):
        self.n_layers = n_layers
        self.dbg = dbg or {}
        self.nc = bass.Bass("TRN2", target_bir_lowering=False)

    def mm(self, out, lhsT, rhs, start, stop, reads, writes, signal=True):
        self.S.op("pe", lambda e: e.matmul(out, lhsT=lhsT, rhs=rhs, start=start, stop=stop), reads, writes, signal)

    def tr(self, out, in_, ident, reads, writes):
        self.S.op("pe", lambda e: e.transpose(out=out, in_=in_, identity=ident), reads, writes)

    def act(self, out, in_, func, reads, writes, scale=None, bias=None):
        kw = {}
        if scale is not None:
            kw["scale"] = scale
            if func == AF.Copy:
                func = AF.Identity
        if bias is not None:
            kw["bias"] = bias
        self.S.op("act", lambda e: e.activation(out=out, in_=in_, func=func, **kw), reads, writes)

    def tt(self, eng, out, in0, in1, op, reads, writes):
        self.S.op(eng, lambda e: e.tensor_tensor(out=out, in0=in0, in1=in1, op=op), reads, writes)

    def ts(self, eng, out, in0, s1, s2, op0, op1, reads, writes):
        if op1 is None:
            self.S.op(eng, lambda e: e.tensor_scalar(out=out, in0=in0, scalar1=s1, scalar2=None, op0=op0), reads, writes)
        else:
            self.S.op(eng, lambda e: e.tensor_scalar(out=out, in0=in0, scalar1=s1, scalar2=s2, op0=op0, op1=op1), reads, writes)

    def stt(self, eng, out, in0, scalar, in1, op0, op1, reads, writes):
        self.S.op(eng, lambda e: e.scalar_tensor_tensor(out=out, in0=in0, scalar=scalar, in1=in1, op0=op0, op1=op1), reads, writes)

    def cp(self, eng, out, in_, reads, writes):
        if eng == "act":
            self.act(out, in_, AF.Copy, reads, writes)
        else:
            self.S.op(eng, lambda e: e.tensor_copy(out=out, in_=in_), reads, writes)

    def recip(self, out, in_, reads, writes):
        self.S.op("dve", lambda e: e.reciprocal(out=out, in_=in_), reads, writes)

    def memset(self, eng, ap, val, writes):
        self.S.op(eng, lambda e: e.memset(ap, val), (), writes)

    def dma(self, q, out, in_, reads, writes, slow=False):
        if slow:
            self.S.dma(q, lambda e: e.dma_start(out=out, in_=in_, allow_slow_non_contiguous=True), reads, writes)
        else:
            self.S.dma(q, lambda e: e.dma_start(out=out, in_=in_), reads, writes)

    def bank(self, ring):
        return ring.next()

    def build(self):
        nc = self.nc
        L = self.n_layers
        din = lambda name, shape, dt=F32: nc.dram_tensor(name, list(shape), dt, kind="ExternalInput").ap()
        self.x_d = din("x", [SEQ, D])
        self.c_d = din("c", [1, D])
        self.pos_d = din("positions", [1, SEQ], I32)
        self.adaw_d = din("ada_w", [DEPTH, D, 6 * D])
        self.vec_d = din("vecpack", [DEPTH, NVEC, 128])
        self.fin_d = din("final_norm", [8, 128])
        self.win_d = din("w_in", [DEPTH, D, IN_COLS])
        self.wuq_d = din("mla_w_uq", [DEPTH, 768, 1536])
        self.wukv_d = din("mla_w_ukv", [DEPTH, 512, 2048])
        self.rld_d = din("ret_log_decay", [1, DEPTH * 16])
        self.wbm_d = din("w_br_mla", [DEPTH, 1024, 1024])
        self.wbd_d = din("w_br_dil", [DEPTH, 512, 1024])
        self.wbr_d = din("w_br_ret", [DEPTH, 1024, 1024])
        self.wout_d = din("w_out", [DEPTH, 1024, 1024])
        self.f1_d = din("ffn_w1", [2, D, D_FF])
        self.f3_d = din("ffn_w3", [2, D, D_FF])
        self.f2_d = din("ffn_w2", [2, D_FF, D])
        self.mr_d = din("moe_router", [2, D, NEXP])
        self.m1_d = din("moe_w1", [2, NEXP, D, D_FFE])
        self.m3_d = din("moe_w3", [2, NEXP, D, D_FFE])
        self.m2_d = din("moe_w2", [2, NEXP, D_FFE, D])
        self.cst_d = din("consts", [128, 648])
        self.dpn_d = din("dposneg", [2, 128, STRIPW])
        self.out_d = nc.dram_tensor("out", [SEQ, D], F32, kind="ExternalOutput").ap()

        def scratch(name, shape, dt=BF16):
            kind = "ExternalOutput" if name in self.dbg else "Internal"
            return nc.dram_tensor(name, list(shape), dt, kind=kind).ap()
        self.xs = scratch("xs", [KC, 128, SEQ], F32)
        self.mg = scratch("mg", [KC, 128, SEQ], F32)
        self.qT = scratch("qT", [8, 192, SEQ])
        self.knT = scratch("knT", [8, 128, SEQ])
        self.vm = scratch("vm", [SEQ, 1024])
        self.dqT = scratch("dqT", [3, 512, SEQ])
        self.dkT = scratch("dkT", [3, 512, SEQ])
        self.dv = scratch("dv", [3, SEQ, 520])
        self.rqT = scratch("rqT", [512, SEQ])
        self.rkT = scratch("rkT", [512, SEQ])
        self.rv = scratch("rv", [SEQ, 1024])
        self.rgT = scratch("rgT", [1024, SEQ])
        self.gT = scratch("gT", [3, 1024, SEQ])
        self.ydbg = scratch("ydbg", [3, 1024, SEQ]) if "ydbg" in self.dbg else None
        self.xsB = [Buf("xs%d" % t) for t in range(NTB)]
        self.mgB = [Buf("mg%d" % t) for t in range(NTB)]
        self.qTB = [Buf() for _ in range(8)]
        self.knTB = [Buf() for _ in range(8)]
        self.vmB = [Buf() for _ in range(2)]
        self.dqTB = [[Buf() for _ in range(4)] for _ in range(3)]
        self.dkTB = [[Buf() for _ in range(4)] for _ in range(3)]
        self.dvB = [Buf() for _ in range(3)]
        self.rqTB = [Buf() for _ in range(4)]
        self.rkTB = [Buf() for _ in range(4)]
        self.rvB = [Buf() for _ in range(2)]
        self.rgTB = [Buf() for _ in range(8)]
        self.gTB = [[Buf() for _ in range(8)] for _ in range(3)]
        self.outB = [Buf() for _ in range(NTB)]
        self.dbgB = Buf()

        with ExitStack() as st:
            self.st = st
            self.S = Sched(nc, st)
            sb = lambda n, sh, dt: st.enter_context(nc.sbuf_tensor(n, sh, dt))
            self.pb = [st.enter_context(nc.psum_tensor("pb%d" % i, [128, 512], F32)) for i in range(8)]
            self.PB = [Buf("pb%d" % i) for i in range(8)]
            self.allbanks = Ring([(self.pb[i], self.PB[i]) for i in range(8)])
            self.RA = sb("RA", [128, KC, SEQ], BF16)
            self.RAb = [[Buf("RA%d_%d" % (k, t)) for t in range(NTB)] for k in range(KC)]
            self.RW = sb("RW", [128, 4, 4096], BF16)
            self.WB = [Buf("W%d" % i) for i in range(4)]
            self.cst = sb("cst", [128, 648], F32); self.cstB = Buf("cst")
            self.cbf = sb("cbf", [128, 1920], BF16); self.cbfB = Buf("cbf")
            self.onesf = sb("onesf", [128, 128], F32); self.onesfB = Buf("onesf")
            self.avgf = sb("avgf", [128, 128], F32)
            self.cosT = sb("cosT", [128, SEQ], BF16); self.sinT = sb("sinT", [128, SEQ], BF16); self.csB = Buf("cs")
            self.krT = sb("krT", [128, SEQ], BF16); self.krB = [Buf() for _ in range(NTB)]
            self.modT = sb("modT", [128, DEPTH, 48], F32); self.modB = Buf("mod")
            self.vecT = sb("vecT", [128, DEPTH, NVEC], F32); self.vecB = Buf("vec")
            self.finT = sb("finT", [128, 8], F32)
            self.gsT = sb("gsT", [128, DEPTH, 16], F32); self.gsB = Buf("gs")
            self.rldT = sb("rldT", [128, DEPTH * 16], F32); self.rldB = Buf("rld")
            self.cact = sb("cact", [128, 8], BF16); self.cactB = Buf("cact")
            self.OVN = 58368
            self.OV = sb("OV", [128, self.OVN], BF16)
            self.ident_f = self.cst[:, 0:128]
            self.ident_b = self.cbf[:, 0:128]
            self.perm_b = self.cbf[:, 128:256]
            self.ones_b = self.cbf[:, 256:384]
            self.mask_b = self.cbf[:, 384:768]
            self.negm_b = self.cbf[:, 768:1152]
            self.negmT_b = self.cbf[:, 1152:1536]
            self.maskT_b = self.cbf[:, 1536:1920]
            self.eps_ap = self.cst[:, 641:642]

            self.setup()
            for l in range(L):
                self.layer(l)
            self.final()
            if self.dbg:
                self.S.wait_all("sp", [self.dbgB])
            self.S.wait_all("sp", self.outB)
            self.S.emit()
        return nc

    def ovv(self, off, n, dt=BF16):
        nb = n * (2 if dt == F32 else 1)
        assert off + nb <= self.OVN, (off, nb, self.OVN)
        v = self.OV[:, off:off + nb]
        if dt == F32:
            v = v.bitcast(F32)
        return v, off + nb

    def wload(self, slots, src_ap, kp, nk, ncols):
        n = nk * ncols
        assert n <= 4096 * len(slots)
        s0 = slots[0]
        if len(slots) == 1:
            flat = self.RW[0:kp, s0, 0:n]
        else:
            flat = self.RW[0:kp, s0:s0 + len(slots), :].rearrange("p a b -> p (a b)")[:, 0:n]
        view = flat.rearrange("p (k n) -> p k n", n=ncols)
        bufs = [self.WB[s] for s in slots]
        self.dma("pool", view, src_ap, [], bufs)
        return view, bufs

    def setup(self):
        S = self.S
        o = 0
        self.dma("sp", self.cst[:], self.cst_d, [], [self.cstB])
        self.cp("act", self.cbf[:, 0:256], self.cst[:, 0:256], [self.cstB], [self.cbfB])
        self.memset("dve", self.cbf[:, 256:384], 1.0, [self.cbfB])
        self.cp("act", self.cbf[:, 384:768], self.cst[:, 256:640], [self.cstB], [self.cbfB])
        self.memset("dve", self.onesf[:], 1.0, [self.onesfB])
        self.memset("dve", self.avgf[:], 1.0 / 128, [self.onesfB])
        self.ts("dve", self.cbf[:, 768:1152], self.cst[:, 256:640], 1.0, 30000.0, ALU.subtract, ALU.mult, [self.cstB], [self.cbfB])
        for j in range(3):
            self.cp("dve", self.cbf[:, 1536 + j * 128:1536 + (j + 1) * 128], self.cst[:, 256 + (2 - j) * 128:256 + (3 - j) * 128], [self.cstB], [self.cbfB])
        for j in range(3):
            self.ts("dve", self.cbf[:, 1152 + j * 128:1152 + (j + 1) * 128], self.cst[:, 256 + (2 - j) * 128:256 + (3 - j) * 128], 1.0, 30000.0,
                    ALU.subtract, ALU.mult, [self.cstB], [self.cbfB])
        self.dma("sp", self.rldT[:], self.rld_d.partition_broadcast(128), [], [self.rldB])
        for l in range(DEPTH):
            self.ts("dve", self.rldT[:, l * 16 + 8:l * 16 + 16], self.rldT[:, l * 16 + 8:l * 16 + 16], -1.0, None, ALU.mult, None,
                    [self.rldB], [self.rldB])
        posi, o1 = self.ovv(0, SEQ, F32)
        posi = self.OV[:, 0:2 * SEQ].bitcast(I32)
        ang, o2 = self.ovv(o1, SEQ, F32)
        kf, o3 = self.ovv(o2, SEQ, F32)
        ki = self.OV[:, o3:o3 + 2 * SEQ].bitcast(I32)
        o4 = o3 + 2 * SEQ
        a2, o5 = self.ovv(o4, SEQ, F32)
        Bp, Ba, Bk, Bki, Ba2 = Buf(), Buf(), Buf(), Buf(), Buf()
        self.dma("sp", posi, self.pos_d.partition_broadcast(128), [], [Bp])
        self.cp("dve", ang, posi, [Bp], [Ba])
        self.ts("dve", ang, ang, self.cst[:, 640:641], None, ALU.mult, None, [Ba, self.cstB], [Ba])
        TWO_PI = float(2 * np.pi)

        def reduce_sin(src, dst_bf, shift):
            self.ts("dve", a2, src, float(shift), None, ALU.add, None, [Ba], [Ba2])
            self.ts("dve", kf, a2, float(1.0 / TWO_PI), None, ALU.mult, None, [Ba2], [Bk])
            self.cp("dve", ki, kf, [Bk], [Bki])
            self.cp("dve", kf, ki, [Bki], [Bk])
            self.stt("dve", a2, kf, -TWO_PI, a2, ALU.mult, ALU.add, [Bk, Ba2], [Ba2])
            self.ts("dve", kf, a2, float(np.pi), -TWO_PI, ALU.is_gt, ALU.mult, [Ba2], [Bk])
            self.tt("dve", a2, a2, kf, ALU.add, [Bk, Ba2], [Ba2])
            self.ts("dve", kf, a2, float(-np.pi), TWO_PI, ALU.is_lt, ALU.mult, [Ba2], [Bk])
            self.tt("dve", a2, a2, kf, ALU.add, [Bk, Ba2], [Ba2])
            self.act(dst_bf, a2, AF.Sin, [Ba2], [self.csB])
        reduce_sin(ang, self.sinT[:], 0.0)
        reduce_sin(ang, self.cosT[:], np.pi / 2)
        vst, o6 = self.ovv(o5, DEPTH * 128 + 128 + 128, F32)
        Bv = Buf()
        for l in range(DEPTH):
            self.dma("sp", vst[0:NVEC, l * 128:(l + 1) * 128], self.vec_d[l], [], [Bv])
        self.dma("sp", vst[0:8, 512:640], self.fin_d, [], [Bv])
        self.dma("sp", vst[0:8, 640:768], self.c_d.rearrange("o (k p) -> (o k) p", p=128), [], [Bv])
        for l in range(DEPTH):
            bk, bb = self.allbanks.next()
            self.tr(bk[:, 0:NVEC], vst[0:NVEC, l * 128:(l + 1) * 128], self.ident_f[0:NVEC, 0:NVEC], [Bv, self.cstB], [bb])
            self.cp("dve", self.vecT[:, l, :], bk[:, 0:NVEC], [bb], [self.vecB])
        bk, bb = self.allbanks.next()
        self.tr(bk[:, 0:8], vst[0:8, 512:640], self.ident_f[0:8, 0:8], [Bv, self.cstB], [bb])
        self.tr(bk[:, 8:16], vst[0:8, 640:768], self.ident_f[0:8, 0:8], [Bv, self.cstB], [bb])
        self.cp("act", self.finT[:], bk[:, 0:8], [bb], [self.vecB])
        self.act(self.cact[:], bk[:, 8:16], AF.Silu, [bb], [self.cactB])
        nblk = 0
        pending = []
        jobs = [(l, cb) for l in range(DEPTH) for cb in range(12)]

        def issue(i):
            l, cb = jobs[i]
            src = self.adaw_d[l].rearrange("(k p) n -> p k n", p=128)[:, :, cb * 512:(cb + 1) * 512]
            return self.wload([i % 4], src, 128, KC, 512)
        for i in range(min(3, len(jobs))):
            pending.append(issue(i))
        for i, (l, cb) in enumerate(jobs):
            W, wb = pending.pop(0)
            if i + 3 < len(jobs):
                pending.append(issue(i + 3))
            if cb % 12 == 0:
                mbk, mbb = self.allbanks.next()
            for j in range(4):
                col = cb * 4 + j
                for kc in range(KC):
                    self.mm(mbk[:, col:col + 1], W[:, kc, j * 128:(j + 1) * 128], self.cact[:, kc:kc + 1], kc == 0, kc == KC - 1,
                            wb + [self.cactB], [mbb], signal=(kc == KC - 1))
            if cb == 11:
                self.tt("dve", self.modT[:, l, :], mbk[:, 0:48], self.vecT[:, l, 0:48], ALU.add, [mbb, self.vecB], [self.modB])
                self.stt("dve", self.gsT[:, l, 0:8], self.modT[:, l, 8:16], 1.0, self.vecT[:, l, 48:56], ALU.add, ALU.mult,
                         [self.modB, self.vecB], [self.gsB])
                self.stt("dve", self.gsT[:, l, 8:16], self.modT[:, l, 32:40], 1.0, self.vecT[:, l, 56:64], ALU.add, ALU.mult,
                         [self.modB, self.vecB], [self.gsB])
        xt0, p = self.ovv(o6, 4 * D, F32)
        xt1, p = self.ovv(p, 4 * D, F32)
        xs0, p = self.ovv(p, KC * 512, F32)
        xs1, p = self.ovv(p, KC * 512, F32)
        xtr = Ring([(xt0.rearrange("p (a d) -> p a d", a=4), Buf()), (xt1.rearrange("p (a d) -> p a d", a=4), Buf())])
        xsr = Ring([(xs0.rearrange("p (k t) -> p k t", k=KC), Buf()), (xs1.rearrange("p (k t) -> p k t", k=KC), Buf())])
        xv = self.x_d.rearrange("(a p) d -> p a d", p=128)
        for tb in range(NTB):
            xt, xtB = xtr.next()
            self.dma("sp", xt, xv[:, 4 * tb:4 * tb + 4, :], [], [xtB])
            xo, xoB = xsr.next()
            for kc in range(KC):
                bk, bb = self.allbanks.next()
                for a in range(4):
                    self.tr(bk[:, a * 128:(a + 1) * 128], xt[:, a, kc * 128:(kc + 1) * 128], self.ident_f, [xtB, self.cstB], [bb])
                self.cp("act" if kc % 2 else "dve", xo[:, kc, :], bk[:, :], [bb], [xoB])
            self.dma("sp", self.xs.rearrange("k p t -> p k t")[:, :, tb * 512:(tb + 1) * 512], xo, [xoB], [self.xsB[tb]])
        S.barrier()

    def norm_phase(self, l, which, tbs, ov0, router=None):
        gs = self.gsT[:, l, 8 * which:8 * which + 8]
        sh = self.modT[:, l, (0 if which == 0 else 24):(0 if which == 0 else 24) + 8]
        p = ov0
        xi = []
        for i in range(2):
            v, p = self.ovv(p, KC * 512, F32)
            xi.append((v.rearrange("p (k t) -> p k t", k=KC), Buf()))
        xir = Ring(xi)
        sq = []
        for i in range(2):
            v, p = self.ovv(p, 512)
            sq.append((v, Buf()))
        sqr = Ring(sq)
        rt = []
        for i in range(2):
            v, p = self.ovv(p, 512, F32)
            rt.append((v, Buf()))
        rtr = Ring(rt)
        tm = []
        for i in range(3):
            v, p = self.ovv(p, 512, F32)
            tm.append((v, Buf()))
        tmr = Ring(tm)
        h32 = []
        if router is not None:
            for i in range(2):
                v, p = self.ovv(p, 512, F32)
                h32.append((v, Buf()))
            h32r = Ring(h32)
        xsv = self.xs.rearrange("k p t -> p k t")
        loaded = {}

        def load(tb):
            x, xB = xir.next()
            self.dma("sp", x, xsv[:, :, tb * 512:(tb + 1) * 512], [self.xsB[tb]], [xB])
            loaded[tb] = (x, xB)
        load(tbs[0])
        for i, tb in enumerate(tbs):
            if i + 1 < len(tbs):
                load(tbs[i + 1])
            x, xB = loaded.pop(tb)
            bk, bb = self.allbanks.next()
            for kc in range(KC):
                s, sB = sqr.next()
                self.act(s, x[:, kc, :], AF.Square, [xB], [sB])
                self.mm(bk[:, :], self.ones_b, s, kc == 0, kc == KC - 1, [sB, self.cbfB], [bb], signal=True)
            r, rB = rtr.next()
            self.act(r, bk[:, :], AF.Sqrt, [bb, self.cstB], [rB], scale=1.0 / D, bias=self.eps_ap)
            self.recip(r, r, [rB], [rB])
            if router is not None:
                lgbk, lgbb = self.allbanks.next()
            for kc in range(KC):
                t, tB = tmr.next()
                self.tt("dve", t, x[:, kc, :], r, ALU.mult, [xB, rB], [tB])
                self.act(self.RA[:, kc, tb * 512:(tb + 1) * 512], t, AF.Identity, [tB, self.gsB, self.modB], [self.RAb[kc][tb]],
                         scale=gs[:, kc:kc + 1], bias=sh[:, kc:kc + 1])
                if router is not None:
                    h, hB = h32r.next()
                    self.act(h, t, AF.Identity, [tB, self.gsB, self.modB], [hB], scale=gs[:, kc:kc + 1], bias=sh[:, kc:kc + 1])
                    wr, wrB = router["w"]
                    for a in range(4):
                        self.mm(lgbk[:, a * 8:(a + 1) * 8], h[:, a * 128:(a + 1) * 128], wr[:, kc, :], (kc == 0 and a == 0), kc == KC - 1,
                                [hB, wrB], [lgbb], signal=(a == 3))
            if router is not None:
                lg, lgB = router["lg"]
                tl = (tb % 2) * 4
                self.cp("dve", lg[:, tl:tl + 4, :], lgbk[:, 0:32].rearrange("p (a e) -> p a e", e=8), [lgbb], [lgB])
        return p

    def proj_fm(self, src, srcB, nk, kp, W, wb, wcol, ncols, handler):
        for tb in range(NTB):
            bk, bb = self.allbanks.next()
            for kc in range(nk):
                self.mm(bk[0:ncols, :], W[0:kp, kc, wcol:wcol + ncols], src[0:kp, kc, tb * 512:(tb + 1) * 512], kc == 0, kc == nk - 1,
                        wb + [srcB[kc][tb]], [bb], signal=(kc == nk - 1))
            self.flush_pe_deferred()
            self._pe_deferred = handler(tb, bk, bb)

    def flush_pe_deferred(self):
        d = getattr(self, "_pe_deferred", None)
        self._pe_deferred = None
        if d is not None:
            d()

    def rope_evac(self, tb, bk, bb, n, tmps, then):
        (cbt, cbB), (ubt, ubB) = tmps.next(), tmps.next()
        cs = slice(tb * 512, (tb + 1) * 512)
        self.tt("dve", cbt[0:n, :], bk[0:n, :], self.cosT[0:n, cs], ALU.mult, [bb, self.csB], [cbB])
        self.tt("dve", ubt[0:n, :], bk[0:n, :], self.sinT[0:n, cs], ALU.mult, [bb, self.csB], [ubB])

        def stage2():
            b2, b2B = self.allbanks.next()
            self.mm(b2[0:n, :], self.ident_b[0:n, 0:n], cbt[0:n, :], True, False, [cbB, self.cbfB], [b2B], signal=False)
            self.mm(b2[0:n, :], self.perm_b[0:n, 0:n], ubt[0:n, :], False, True, [ubB, self.cbfB], [b2B])
            then(b2, b2B)
        return stage2

    def layer(self, l):
        S = self.S
        self._pj_pre = [self.pj_issue(l, i) for i in range(3)]
        self.norm_phase(l, 0, list(range(NTB)), 0)
        S.barrier()
        self.proj_phase(l)
        S.barrier()
        self._br_pre = {0: self.br_issue(l, 0, [0, 1])}
        self.mla_attn(l)
        S.barrier()
        self._br_pre[1] = self.br_issue(l, 1, [2, 3])
        self.branch_out(l, 0)
        S.barrier()
        self.dil_attn(l)
        S.barrier()
        self._br_pre[2] = self.br_issue(l, 2, [0, 1])
        self.branch_out(l, 1)
        S.barrier()
        self._wout_pre = self.wload([2, 3], self.wout_d[l].rearrange("(k p) n -> p k n", p=128), 128, KC, 1024)
        self.ret_attn(l)
        S.barrier()
        self.branch_out(l, 2)
        S.barrier()
        self.ffn(l)
        S.barrier()

    def pj_blocks(self):
        blocks = []
        blocks.append((C_CQ, 512, "cq", 0)); blocks.append((C_CQ + 512, 256, "cq", 4))
        blocks.append((C_CKV, 512, "ckv", 0))
        blocks.append((C_KR, 64, "kr", 0))
        for g in range(3):
            blocks.append((C_DQ + g * 512, 512, "dq", g))
        for g in range(3):
            blocks.append((C_DK + g * 512, 512, "dk", g))
        for g in range(3):
            blocks.append((C_DV + g * 512, 512, "dv", g))
        blocks.append((C_RQ, 512, "rq", 0)); blocks.append((C_RK, 512, "rk", 0))
        blocks.append((C_RV, 512, "rv", 0)); blocks.append((C_RV + 512, 512, "rv", 1))
        blocks.append((C_RG, 512, "rg", 0)); blocks.append((C_RG + 512, 512, "rg", 1))
        for b3 in range(3):
            blocks.append((C_GA + b3 * 1024, 512, "gate", (b3, 0))); blocks.append((C_GA + b3 * 1024 + 512, 512, "gate", (b3, 1)))
        return blocks

    def pj_issue(self, l, i):
        c0, n, _, _ = self.pj_blocks()[i]
        winv = self.win_d[l].rearrange("(k p) n -> p k n", p=128)
        return self.wload([i % 4], winv[:, :, c0:c0 + n], 128, KC, n)

    def br_issue(self, l, b, slots):
        kp = 64 if b == 1 else 128
        if b == 0:
            src = self.wbm_d[l].rearrange("(k p) n -> p k n", p=128)
        elif b == 1:
            src = self.wbd_d[l].rearrange("(k p) n -> p k n", p=64)
        else:
            src = self.wbr_d[l].rearrange("(k p) n -> p k n", p=128)
        return self.wload(slots, src, kp, KC, 1024)

    def proj_phase(self, l):
        p = 0
        cq, p = self.ovv(p, 6 * SEQ)
        cq = cq.rearrange("p (k t) -> p k t", k=6)
        ckv, p = self.ovv(p, 4 * SEQ)
        ckv = ckv.rearrange("p (k t) -> p k t", k=4)
        cqB = [[Buf() for _ in range(NTB)] for _ in range(6)]
        ckvB = [[Buf() for _ in range(NTB)] for _ in range(4)]
        stg = []
        for i in range(3):
            v, p = self.ovv(p, SEQ)
            stg.append((v, Buf()))
        stgr = Ring(stg)
        rtm = []
        for i in range(6):
            v, p = self.ovv(p, 512)
            rtm.append((v, Buf()))
        rtmr = Ring(rtm)
        stv = []
        for i in range(3):
            v, p = self.ovv(p, 520)
            stv.append((v, Buf()))
            self.memset("dve", v, 1.0, [stv[-1][1]])
        stvr = Ring(stv)
        stw = []
        for i in range(3):
            v, p = self.ovv(p, 512)
            stw.append((v, Buf()))
        stwr = Ring(stw)
        sq = []
        for i in range(2):
            v, p = self.ovv(p, 512)
            sq.append((v, Buf()))
        sqr = Ring(sq)
        rt = []
        for i in range(2):
            v, p = self.ovv(p, 512, F32)
            rt.append((v, Buf()))
        rtr = Ring(rt)
        RA, RAb = self.RA, self.RAb
        winv = self.win_d[l].rearrange("(k p) n -> p k n", p=128)

        blocks = []
        blocks.append((C_CQ, 512, "cq", 0)); blocks.append((C_CQ + 512, 256, "cq", 4))
        blocks.append((C_CKV, 512, "ckv", 0))
        blocks.append((C_KR, 64, "kr", 0))
        for g in range(3):
            blocks.append((C_DQ + g * 512, 512, "dq", g))
        for g in range(3):
            blocks.append((C_DK + g * 512, 512, "dk", g))
        for g in range(3):
            blocks.append((C_DV + g * 512, 512, "dv", g))
        blocks.append((C_RQ, 512, "rq", 0)); blocks.append((C_RK, 512, "rk", 0))
        blocks.append((C_RV, 512, "rv", 0)); blocks.append((C_RV + 512, 512, "rv", 1))
        blocks.append((C_RG, 512, "rg", 0)); blocks.append((C_RG + 512, 512, "rg", 1))
        for b3 in range(3):
            blocks.append((C_GA + b3 * 1024, 512, "gate", (b3, 0))); blocks.append((C_GA + b3 * 1024 + 512, 512, "gate", (b3, 1)))
        nb = len(blocks)
        pend = []

        def issue(i):
            c0, n, _, _ = blocks[i]
            return self.wload([i % 4], winv[:, :, c0:c0 + n], 128, KC, n)
        pend.extend(self._pj_pre)

        def fm_store_handler(dram_rows_ap, dramB, n, func=AF.Copy, scale=None):
            st_, stB = stgr.next()

            def h(tb, bk, bb):
                self.act(st_[0:n, tb * 512:(tb + 1) * 512], bk[0:n, :], func, [bb], [stB], scale=scale)
                if tb == NTB - 1:
                    self.dma("sp", dram_rows_ap, st_[0:n, :], [stB], [dramB])
            return h

        def rope_store_handler(dram_rows_ap, dramB, n, d, scale=None, sbuf_dest=None, sbufB=None):
            if sbuf_dest is None:
                st_, stB = stgr.next()
            else:
                st_, stB = sbuf_dest, None

            def h(tb, bk, bb):
                def then(b2, b2B):
                    if d == 1:
                        dst = st_[0:n, tb * 512:(tb + 1) * 512]
                        src = b2[0:n, :]
                    else:
                        w = 512 // d
                        dst = st_[0:n, :].rearrange("p (r l) -> p r l", r=d)[:, :, tb * w:(tb + 1) * w]
                        src = b2[0:n, :].rearrange("p (j r) -> p r j", r=d)
                    wB = [stB] if sbuf_dest is None else [sbufB[tb]]
                    self.act(dst, src, AF.Copy, [b2B], wB, scale=scale)
                    if sbuf_dest is None and tb == NTB - 1:
                        self.dma("sp", dram_rows_ap, st_[0:n, :], [stB], [dramB])
                return self.rope_evac(tb, bk, bb, n, rtmr, then)
            return h

        for bi_, (c0, n, kind, info) in enumerate(blocks):
            W, wb = pend.pop(0)
            if bi_ + 3 < nb:
                pend.append(issue(bi_ + 3))
            if kind in ("cq", "ckv"):
                dstt, dB = (cq, cqB) if kind == "cq" else (ckv, ckvB)
                for j in range(n // 128):
                    c = info + j

                    def h(tb, bk, bb, c=c, dstt=dstt, dB=dB):
                        self.cp("act", dstt[:, c, tb * 512:(tb + 1) * 512], bk[:, :], [bb], [dB[c][tb]])
                    self.proj_fm(RA, RAb, KC, 128, W, wb, j * 128, 128, h)
            elif kind == "kr":
                self.proj_fm(RA, RAb, KC, 128, W, wb, 0, 64, rope_store_handler(None, None, 64, 1, sbuf_dest=self.krT, sbufB=self.krB))
            elif kind in ("dq", "dk"):
                g = info
                dr, dB = (self.dqT, self.dqTB) if kind == "dq" else (self.dkT, self.dkTB)
                for j in range(4):
                    self.proj_fm(RA, RAb, KC, 128, W, wb, j * 128, 128,
                                 rope_store_handler(dr[g, j * 128:(j + 1) * 128, :], dB[g][j], 128, DIL_D[g]))
            elif kind in ("rq", "rk"):
                dr, dB = (self.rqT, self.rqTB) if kind == "rq" else (self.rkT, self.rkTB)
                for j in range(4):
                    self.proj_fm(RA, RAb, KC, 128, W, wb, j * 128, 128,
                                 rope_store_handler(dr[j * 128:(j + 1) * 128, :], dB[j], 128, 1, scale=(0.125 if kind == "rk" else None)))
            elif kind == "rg":
                for j in range(4):
                    o = info * 4 + j
                    self.proj_fm(RA, RAb, KC, 128, W, wb, j * 128, 128, fm_store_handler(self.rgT[o * 128:(o + 1) * 128, :], self.rgTB[o], 128, AF.Silu))
            elif kind == "gate":
                b3, hf = info
                for j in range(4):
                    o = hf * 4 + j
                    self.proj_fm(RA, RAb, KC, 128, W, wb, j * 128, 128,
                                 fm_store_handler(self.gT[b3, o * 128:(o + 1) * 128, :], self.gTB[b3][o], 128, AF.Sigmoid))
            elif kind == "dv":
                self.flush_pe_deferred()
                g = info
                d = DIL_D[g]
                Lr = SEQ // d
                for tt_ in range(NTT):
                    r = (128 * tt_) // Lr
                    j0 = (128 * tt_) % Lr
                    t0 = r + d * j0
                    tsl = slice(t0, t0 + d * 127 + 1, d)
                    tbs = sorted(set([t0 // 512, (t0 + d * 127) // 512])) if d < 16 else list(range(NTB))
                    bk, bb = self.allbanks.next()
                    for kc in range(KC):
                        self.mm(bk[:, :], RA[:, kc, tsl], W[:, kc, :], kc == 0, kc == KC - 1, wb + [RAb[kc][t] for t in tbs], [bb],
                                signal=(kc == KC - 1))
                    sv, svB = stvr.next()
                    self.cp("act" if tt_ % 2 else "dve", sv.rearrange("p (h c) -> p h c", c=65)[:, :, 0:64],
                            bk[:, :].rearrange("p (h c) -> p h c", c=64), [bb], [svB])
                    self.dma("sp", self.dv[g, tt_ * 128:(tt_ + 1) * 128, :], sv, [svB], [self.dvB[g]])
            elif kind == "rv":
                self.flush_pe_deferred()
                hf = info
                for tt_ in range(NTT):
                    tb = tt_ // 4
                    bk, bb = self.allbanks.next()
                    for kc in range(KC):
                        self.mm(bk[:, :], RA[:, kc, tt_ * 128:(tt_ + 1) * 128], W[:, kc, :], kc == 0, kc == KC - 1, wb + [RAb[kc][tb]], [bb],
                                signal=(kc == KC - 1))
                    sw, swB = stwr.next()
                    self.cp("act" if tt_ % 2 else "dve", sw, bk[:, :], [bb], [swB])
                    self.dma("sp", self.rv[tt_ * 128:(tt_ + 1) * 128, hf * 512:(hf + 1) * 512], sw, [swB], [self.rvB[hf]])

        self.flush_pe_deferred()

        def rmsn(src, srcB, nk, gcol0, inv_n):
            for tb in range(NTB):
                bk, bb = self.allbanks.next()
                for c in range(nk):
                    s, sB = sqr.next()
                    self.act(s, src[:, c, tb * 512:(tb + 1) * 512], AF.Square, [srcB[c][tb]], [sB])
                    self.mm(bk[:, :], self.ones_b, s, c == 0, c == nk - 1, [sB, self.cbfB], [bb], signal=True)
                r, rB = rtr.next()
                self.act(r, bk[:, :], AF.Sqrt, [bb, self.cstB], [rB], scale=inv_n, bias=self.eps_ap)
                self.recip(r, r, [rB], [rB])
                for c in range(nk):
                    v = src[:, c, tb * 512:(tb + 1) * 512]
                    self.stt("dve", v, v, self.vecT[:, l, gcol0 + c:gcol0 + c + 1], r, ALU.mult, ALU.mult, [srcB[c][tb], rB, self.vecB],
                             [srcB[c][tb]])
        rmsn(cq, cqB, 6, 64, 1.0 / 768)
        rmsn(ckv, ckvB, 4, 70, 1.0 / 512)

        wuqv = self.wuq_d[l].rearrange("(k p) n -> p k n", p=128)
        wukvv = self.wukv_d[l].rearrange("(k p) n -> p k n", p=128)
        jobs = []
        for hp in range(4):
            jobs.append(("q", hp))
            jobs.append(("kv", hp))
        pend = []

        def issue2(i):
            kind, hp = jobs[i]
            if kind == "q":
                return self.wload([i % 4], wuqv[:, :, hp * 384:(hp + 1) * 384], 128, 6, 384)
            return self.wload([i % 4], wukvv[:, :, hp * 512:(hp + 1) * 512], 128, 4, 512)
        for i in range(3):
            pend.append(issue2(i))
        for i, (kind, hp) in enumerate(jobs):
            W, wb = pend.pop(0)
            if i + 3 < len(jobs):
                pend.append(issue2(i + 3))
            for hh in range(2):
                h_ = 2 * hp + hh
                if kind == "q":
                    self.proj_fm(cq, cqB, 6, 128, W, wb, hh * 192, 128, fm_store_handler(self.qT[h_, 0:128, :], self.qTB[h_], 128))
                    self.proj_fm(cq, cqB, 6, 128, W, wb, hh * 192 + 128, 64, rope_store_handler(self.qT[h_, 128:192, :], self.qTB[h_], 64, 1))
                else:
                    self.proj_fm(ckv, ckvB, 4, 128, W, wb, hh * 256, 128, fm_store_handler(self.knT[h_], self.knTB[h_], 128))
            self.flush_pe_deferred()
            if kind == "kv":
                Wv = W.rearrange("p k (h c) -> p k h c", c=256)[:, :, :, 128:256]
                for tt_ in range(NTT):
                    tb = tt_ // 4
                    bk, bb = self.allbanks.next()
                    for c in range(4):
                        self.mm(bk[:, 0:256].rearrange("p (h c) -> p h c", c=128), ckv[:, c, tt_ * 128:(tt_ + 1) * 128], Wv[:, c, :, :],
                                c == 0, c == 3, wb + [ckvB[c][tb]], [bb], signal=(c == 3))
                    sw, swB = stwr.next()
                    self.cp("act" if tt_ % 2 else "dve", sw[:, 0:256], bk[:, 0:256], [bb], [swB])
                    self.dma("sp", self.vm[tt_ * 128:(tt_ + 1) * 128, hp * 256:(hp + 1) * 256], sw[:, 0:256], [swB], [self.vmB[hp // 2]])

    def mla_attn(self, l):
        p = 0
        Ld = []
        for i in range(2):
            d_ = {}
            for nm, n in (("qn", SEQ), ("qr", SEQ), ("kn", SEQ), ("vh", NTT * 128)):
                v, p = self.ovv(p, n)
                d_[nm] = (v, Buf())
            Ld.append(d_)
        Et = []
        for i in range(6):
            v, p = self.ovv(p, 512)
            Et.append((v, Buf()))
        Er = Ring(Et)
        rcs = []
        for i in range(2):
            v, p = self.ovv(p, 512, F32)
            rcs.append((v, Buf()))
        rcr = Ring(rcs)
        ess = []
        for i in range(4):
            v, p = self.ovv(p, 512, F32)
            ess.append((v, Buf()))
        esr = Ring(ess)
        Sr = Ring([(self.pb[i], self.PB[i]) for i in (0, 1, 2)])
        Or = Ring([(self.pb[i], self.PB[i]) for i in (3, 4)])
        Dr = Ring([(self.pb[i], self.PB[i]) for i in (5, 6)])
        vmv = self.vm.rearrange("(t p) f -> p t f", p=128)
        scale = float(192 ** -0.5)

        def loads(h):
            d_ = Ld[h % 2]
            self.dma("sp", d_["qn"][0], self.qT[h, 0:128, :], [self.qTB[h]], [d_["qn"][1]])
            self.dma("sp", d_["qr"][0][0:64, :], self.qT[h, 128:192, :], [self.qTB[h]], [d_["qr"][1]])
            self.dma("sp", d_["kn"][0], self.knT[h], [self.knTB[h]], [d_["kn"][1]])
            self.dma("sp", d_["vh"][0].rearrange("p (t f) -> p t f", f=128), vmv[:, :, h * 128:(h + 1) * 128], [self.vmB[h // 4]], [d_["vh"][1]])
        loads(0)
        for h in range(8):
            if h + 1 < 8:
                loads(h + 1)
            d_ = Ld[h % 2]
            qn, qnB = d_["qn"]; qr, qrB = d_["qr"]; kn, knB = d_["kn"]; vh, vhB = d_["vh"]
            vh3 = vh.rearrange("p (t f) -> p t f", f=128)
            for qb in range(NTB):
                qs = slice(qb * 512, (qb + 1) * 512)
                ob, obB = Or.next()
                db, dbB = Dr.next()

                def smm(kt):
                    sb_, sbB = Sr.next()
                    ks = slice(kt * 128, (kt + 1) * 128)
                    self.mm(sb_[:, :], kn[:, ks], qn[:, qs], True, False, [knB, qnB], [sbB], signal=False)
                    self.mm(sb_[:, :], self.krT[0:64, ks], qr[0:64, qs], False, True, [self.krB[kt // 4], qrB], [sbB])
                    e, eB = Er.next()
                    self.act(e, sb_[:, :], AF.Exp, [sbB], [eB], scale=scale)
                    return e, eB
                esA, esAB = esr.next()
                esBt, esBB = esr.next()
                cur, nxt = smm(0), smm(1)
                for kt in range(NTT):
                    nn = smm(kt + 2) if kt + 2 < NTT else None
                    e, eB = cur
                    self.mm(ob[:, :], vh3[:, kt, :], e, kt == 0, kt == NTT - 1, [vhB, eB], [obB], signal=True)
                    if kt == 0:
                        self.cp("dve", esA, e, [eB], [esAB])
                    elif kt == 1:
                        self.cp("pool", esBt, e, [eB], [esBB])
                    elif kt % 2 == 0:
                        self.tt("dve", esA, esA, e, ALU.add, [eB, esAB], [esAB])
                    else:
                        self.tt("pool", esBt, esBt, e, ALU.add, [eB, esBB], [esBB])
                    cur, nxt = nxt, nn
                self.mm(db[:, :], self.onesf[:], esA, True, False, [esAB, self.onesfB], [dbB], signal=False)
                self.mm(db[:, :], self.onesf[:], esBt, False, True, [esBB, self.onesfB], [dbB])
                r, rB = rcr.next()
                self.recip(r, db[:, :], [dbB], [rB])
                self.tt("dve", self.RA[:, h, qs], ob[:, :], r, ALU.mult, [obB, rB], [self.RAb[h][qb]])
        self.dbg_dump_y(0)

    def dbg_dump_y(self, b, kp=128):
        if self.ydbg is None:
            return
        for k in range(KC):
            self.dma("sp", self.ydbg[b, k * 128:k * 128 + kp, :], self.RA[0:kp, k, :], [self.RAb[k][t] for t in range(NTB)], [self.dbgB])

    def branch_out(self, l, b):
        kp = 64 if b == 1 else 128
        if b == 0:
            src = self.wbm_d[l].rearrange("(k p) n -> p k n", p=128)
        elif b == 1:
            src = self.wbd_d[l].rearrange("(k p) n -> p k n", p=64)
        else:
            src = self.wbr_d[l].rearrange("(k p) n -> p k n", p=128)
        Wb, wbb = self._br_pre[b]
        last = (b == 2)
        if last:
            Wo, wob = self._wout_pre
        p = 0
        xi = []
        for i in range(2):
            v, p = self.ovv(p, KC * 512, F32)
            xi.append((v.rearrange("p (k t) -> p k t", k=KC), Buf()))
        xir = Ring(xi)
        mi = []
        for i in range(2):
            v, p = self.ovv(p, KC * 512, F32)
            mi.append((v.rearrange("p (k t) -> p k t", k=KC), Buf()))
        mir = Ring(mi)
        gts = []
        for i in range(2):
            v, p = self.ovv(p, KC * 512)
            gts.append((v.rearrange("p (k t) -> p k t", k=KC), Buf()))
        gtr = Ring(gts)
        ms = []
        for i in range(2):
            v, p = self.ovv(p, KC * 512)
            ms.append((v.rearrange("p (k t) -> p k t", k=KC), Buf()))
        msr = Ring(ms)
        tps = []
        for i in range(2):
            v, p = self.ovv(p, 512, F32)
            tps.append((v, Buf()))
        tpr = Ring(tps)
        xsv = self.xs.rearrange("k p t -> p k t")
        mgv = self.mg.rearrange("k p t -> p k t")
        gv = self.gT[b].rearrange("(o p) t -> p o t", p=128)
        gate1 = self.modT[:, l, 16:24]
        pre = {}

        def load(tb):
            ts_ = slice(tb * 512, (tb + 1) * 512)
            g, gB = gtr.next()
            self.dma("sp", g, gv[:, :, ts_], self.gTB[b], [gB])
            mgt, mgB = mir.next()
            if b > 0:
                self.dma("sp", mgt, mgv[:, :, ts_], [self.mgB[tb]], [mgB])
            x, xB = (None, None)
            if last:
                x, xB = xir.next()
                self.dma("sp", x, xsv[:, :, ts_], [self.xsB[tb]], [xB])
            pre[tb] = (x, xB, g, gB, mgt, mgB)
        load(0)
        for tb in range(NTB):
            if tb + 1 < NTB:
                load(tb + 1)
            x, xB, g, gB, mgt, mgB = pre.pop(tb)
            ts_ = slice(tb * 512, (tb + 1) * 512)
            if last:
                m, mB = msr.next()
            for o in range(KC):
                bk, bb = self.allbanks.next()
                for kc in range(KC):
                    self.mm(bk[:, :], Wb[0:kp, kc, o * 128:(o + 1) * 128], self.RA[0:kp, kc, ts_], kc == 0, kc == KC - 1,
                            wbb + [self.RAb[kc][tb]], [bb], signal=(kc == KC - 1))
                if b == 0:
                    self.tt("dve", mgt[:, o, :], bk[:, :], g[:, o, :], ALU.mult, [bb, gB], [mgB])
                else:
                    t_, tB = tpr.next()
                    self.tt("dve", t_, bk[:, :], g[:, o, :], ALU.mult, [bb, gB], [tB])
                    if last:
                        self.tt("pool", m[:, o, :], t_, mgt[:, o, :], ALU.add, [tB, mgB], [mB])
                    else:
                        self.tt("pool", mgt[:, o, :], t_, mgt[:, o, :], ALU.add, [tB, mgB], [mgB])
            if not last:
                self.dma("sp", mgv[:, :, ts_], mgt, [mgB], [self.mgB[tb]])
                continue
            for o2 in range(KC):
                bk, bb = self.allbanks.next()
                for o in range(KC):
                    self.mm(bk[:, :], Wo[:, o, o2 * 128:(o2 + 1) * 128], m[:, o, :], o == 0, o == KC - 1, wob + [mB], [bb], signal=(o == KC - 1))
                self.stt("dve", x[:, o2, :], bk[:, :], gate1[:, o2:o2 + 1], x[:, o2, :], ALU.mult, ALU.add, [bb, xB, self.modB], [xB])
            self.dma("sp", xsv[:, :, ts_], x, [xB], [self.xsB[tb]])

    def dil_attn(self, l):
        p = 0
        va = []
        for g in range(3):
            v, p = self.ovv(p, NTT * 520)
            va.append((v.rearrange("p (t f) -> p t f", f=520), Buf()))
            self.dma("sp", va[g][0], self.dv[g].rearrange("(t p) f -> p t f", p=128), [self.dvB[g]], [va[g][1]])
        qk = []
        for g in range(3):
            q_, p = self.ovv(p, SEQ)
            k_, p = self.ovv(p, SEQ)
            qk.append((q_, Buf(), k_, Buf()))
        accs = []
        for i in range(2):
            v, p = self.ovv(p, SEQ, F32)
            accs.append((v, Buf()))
        Et = []
        for i in range(4):
            v, p = self.ovv(p, 384)
            Et.append((v, Buf()))
        Er = Ring(Et)
        rrv, p = self.ovv(p, SEQ, F32)
        rrB = Buf()
        bcs = []
        for i in range(2):
            v, p = self.ovv(p, 512, F32)
            bcs.append((v, Buf()))
        bcr = Ring(bcs)
        Sr = Ring([(self.pb[i], self.PB[i]) for i in (0, 1, 2)])
        obs = [(self.pb[i], self.PB[i]) for i in (3, 4, 5, 6)]
        Br = Ring([(self.pb[i], self.PB[i]) for i in (7,)])

        def loads(hp):
            for g in range(3):
                q_, qB, k_, kB = qk[g]
                self.dma("sp", q_, self.dqT[g, hp * 128:(hp + 1) * 128, :], [self.dqTB[g][hp]], [qB])
                self.dma("sp", k_, self.dkT[g, hp * 128:(hp + 1) * 128, :], [self.dkTB[g][hp]], [kB])
        for hp in range(4):
            loads(hp)
            for hh in range(2):
                h = 2 * hp + hh
                rows = slice(64 * hh, 64 * hh + 64)
                acc, accB = accs[hh]
                for g in range(3):
                    d = DIL_D[g]
                    TPR = (SEQ // d) // 128
                    q_, qB, k_, kB = qk[g]
                    vg, vgB = va[g]

                    def qblocks(n):
                        return [i for i in (n - 1, n, n + 1) if 0 <= i < NTT and i // TPR == n // TPR]

                    def s_stage(n):
                        qb_ = qblocks(n)
                        qlo, qhi = qb_[0] * 128, (qb_[-1] + 1) * 128
                        W = qhi - qlo
                        m0 = (qb_[0] - (n - 1)) * 128
                        sb_, sbB = Sr.next()
                        self.mm(sb_[:, 0:W], k_[rows, n * 128:(n + 1) * 128], q_[rows, qlo:qhi], True, True, [kB, qB], [sbB], signal=True)
                        e, eB = Er.next()
                        self.act(e[:, 0:W], sb_[:, 0:W], AF.Exp, [sbB], [eB], scale=0.125)
                        self.tt("dve", e[:, 0:W], e[:, 0:W], self.maskT_b[:, m0:m0 + W], ALU.mult, [eB, self.cbfB], [eB])
                        return (e, eB, qlo, qhi)
                    last_n = [max(n for n in range(NTT) if any(i // 4 == b4 for i in qblocks(n))) for b4 in range(4)]
                    stages = {0: s_stage(0), 1: s_stage(1)}
                    started = [False] * 4
                    for n in range(NTT):
                        if n + 2 < NTT:
                            stages[n + 2] = s_stage(n + 2)
                        e, eB, qlo, qhi = stages.pop(n)
                        c = qlo
                        while c < qhi:
                            b4 = c // 512
                            ce = min(qhi, (b4 + 1) * 512)
                            ob, obB = obs[b4]
                            self.mm(ob[0:65, c - b4 * 512:ce - b4 * 512], vg[:, n, h * 65:(h + 1) * 65], e[:, c - qlo:ce - qlo],
                                    not started[b4], False, [vgB, eB], [obB], signal=True)
                            started[b4] = True
                            c = ce
                        for b4 in range(4):
                            if last_n[b4] != n:
                                continue
                            ob, obB = obs[b4]
                            if g == 0:
                                self.cp("dve", acc[0:65, b4 * 512:(b4 + 1) * 512], ob[0:65, :], [obB], [accB])
                            elif g == 1:
                                dst = acc[0:65, :].rearrange("p (j r) -> p r j", r=4)[:, b4, :]
                                self.tt("dve", dst, dst, ob[0:65, :], ALU.add, [obB, accB], [accB])
                            else:
                                dst = acc[0:65, :].rearrange("p (j r) -> p r j", r=16)[:, 4 * b4:4 * b4 + 4, :]
                                self.tt("dve", dst, dst, ob[0:65, :].rearrange("p (b j) -> p b j", b=4), ALU.add, [obB, accB], [accB])
                self.act(rrv[64:65, :], acc[64:65, :], AF.Ln, [accB], [rrB])
                self.act(rrv[64:65, :], rrv[64:65, :], AF.Exp, [rrB], [rrB], scale=-1.0)
                for tb in range(NTB):
                    ts_ = slice(tb * 512, (tb + 1) * 512)
                    bk, bb = Br.next()
                    self.mm(bk[0:64, :], self.onesf[64:65, 0:64], rrv[64:65, ts_], True, True, [rrB, self.onesfB], [bb])
                    bc, bcB = bcr.next()
                    self.cp("act", bc[0:64, :], bk[0:64, :], [bb], [bcB])
                    self.tt("dve", self.RA[0:64, h, ts_], acc[0:64, ts_], bc[0:64, :], ALU.mult, [accB, bcB], [self.RAb[h][tb]])
        self.dbg_dump_y(1, 64)

    def ret_attn(self, l):
        p = 0
        dpos, p = self.ovv(p, STRIPW, F32)
        dneg, p = self.ovv(p, STRIPW, F32)
        dB = Buf()
        self.dma("sp", dpos, self.dpn_d[0], [], [dB])
        self.dma("sp", dneg, self.dpn_d[1], [], [dB])
        HW_ = STRIPW // 2
        ef, p = self.ovv(p, HW_)
        eb, p = self.ovv(p, HW_)
        efB, ebB = Buf(), Buf()
        strips = []
        for i in range(2):
            v, p = self.ovv(p, STRIPW)
            strips.append((v, Buf()))
        qk = []
        for i in range(2):
            q_, p = self.ovv(p, SEQ)
            k_, p = self.ovv(p, SEQ)
            qk.append((q_, Buf(), k_, Buf()))
        hv = []
        for i in range(2):
            v_, p = self.ovv(p, NTT * 128)
            g_, p = self.ovv(p, SEQ)
            hv.append((v_.rearrange("p (t f) -> p t f", f=128), Buf(), g_, Buf()))
        SD = []
        for i in range(6):
            v, p = self.ovv(p, 512)
            SD.append((v, Buf()))
        SDr = Ring(SD)
        SC = []
        for i in range(3):
            v, p = self.ovv(p, 512)
            SC.append((v, Buf()))
        SCr = Ring(SC)
        ysbs, sqs = [], []
        for i in range(2):
            v, p = self.ovv(p, 512, F32)
            ysbs.append((v, Buf()))
            v, p = self.ovv(p, 512, F32)
            sqs.append((v, Buf()))
        ysbr, sqr_ = Ring(ysbs), Ring(sqs)
        tmp = {}
        for nm in ("msq", "var", "rstd"):
            v, p = self.ovv(p, 512, F32)
            tmp[nm] = (v, Buf())
        Sr = Ring([(self.pb[i], self.PB[i]) for i in (0, 1, 2)])
        Yr = Ring([(self.pb[i], self.PB[i]) for i in (3, 4)])
        Mr = Ring([(self.pb[i], self.PB[i]) for i in (5,)])
        Vb, VbB = self.pb[6], self.PB[6]
        dmy, dmyB = self.pb[7], self.PB[7]
        rvv = self.rv.rearrange("(t p) f -> p t f", p=128)

        def loads_pair(hp):
            q_, qB, k_, kB = qk[hp % 2]
            self.dma("sp", q_, self.rqT[hp * 128:(hp + 1) * 128, :], [self.rqTB[hp]], [qB])
            self.dma("sp", k_, self.rkT[hp * 128:(hp + 1) * 128, :], [self.rkTB[hp]], [kB])

        def loads_head(h):
            v_, vB, g_, gB = hv[h % 2]
            self.dma("sp", v_, rvv[:, :, h * 128:(h + 1) * 128], [self.rvB[h // 4]], [vB])
            self.dma("sp", g_, self.rgT[h * 128:(h + 1) * 128, :], [self.rgTB[h]], [gB])

        def strip_steps(h):
            strip, stripB = strips[h % 2]
            lgf = self.rldT[:, l * 16 + h:l * 16 + h + 1]
            nlgb = self.rldT[:, l * 16 + 8 + h:l * 16 + 8 + h + 1]
            steps = []
            for half in range(2):
                cs = slice(half * HW_, (half + 1) * HW_)
                steps.append(lambda cs=cs: self.act(ef, dpos[:, cs], AF.Exp, [dB, self.rldB], [efB], scale=lgf))
                steps.append(lambda cs=cs: self.act(eb, dneg[:, cs], AF.Exp, [dB, self.rldB], [ebB], scale=nlgb))
                steps.append(lambda cs=cs: self.tt("dve", strip[:, cs], ef, eb, ALU.mult, [efB, ebB], [stripB]))
            return steps

        def stats_steps(h, qb, yb, ybB, g_, gB):
            qs = slice(qb * 512, (qb + 1) * 512)
            ysb, ysbB = ysbr.next()
            sq, sqB = sqr_.next()
            msq, msqB = tmp["msq"]; var, varB = tmp["var"]; rstd, rstdB = tmp["rstd"]
            st = {}

            def s3():
                st["mb"], st["mbB"] = Mr.next()
                self.mm(st["mb"][:, :], self.avgf[:], ysb, True, True, [ysbB, self.onesfB], [st["mbB"]])
                self.mm(Vb[:, :], self.avgf[:], sq, True, True, [sqB, self.onesfB], [VbB])
            return [
                lambda: self.cp("act", ysb, yb[:, :], [ybB], [ysbB]),
                lambda: self.act(sq, yb[:, :], AF.Square, [ybB], [sqB]),
                s3,
                lambda: self.act(msq, st["mb"][:, :], AF.Square, [st["mbB"]], [msqB]),
                lambda: self.tt("dve", var, Vb[:, :], msq, ALU.subtract, [VbB, msqB], [varB]),
                lambda: self.ts("dve", var, var, 0.0, EPS, ALU.max, ALU.add, [varB], [varB]),
                lambda: self.act(rstd, var, AF.Ln, [varB], [rstdB]),
                lambda: self.act(rstd, rstd, AF.Exp, [rstdB], [rstdB], scale=-0.5),
                lambda: self.tt("dve", ysb, ysb, st["mb"][:, :], ALU.subtract, [ysbB, st["mbB"]], [ysbB]),
                lambda: self.tt("pool", ysb, ysb, rstd, ALU.mult, [ysbB, rstdB], [ysbB]),
                lambda: self.stt("dve", self.RA[:, h, qs], ysb, self.vecT[:, l, 74 + h:75 + h], g_[:, qs], ALU.mult, ALU.mult,
                                 [ysbB, self.vecB, gB], [self.RAb[h][qb]]),
            ]
        loads_pair(0)
        loads_head(0)
        for s in strip_steps(0):
            s()
        pending = []
        for h in range(8):
            hp, hh = h // 2, h % 2
            if hh == 0 and hp + 1 < 4:
                loads_pair(hp + 1)
            rows = slice(64 * hh, 64 * hh + 64)
            q_, qB, k_, kB = qk[hp % 2]
            v_, vB, g_, gB = hv[h % 2]
            strip, stripB = strips[h % 2]
            for qb in range(NTB):
                qs = slice(qb * 512, (qb + 1) * 512)
                yb, ybB = Yr.next()
                if qb == 1 and h + 1 < 8:
                    loads_head(h + 1)
                    pending.extend(strip_steps(h + 1))

                def smm(kt):
                    sb_, sbB = Sr.next()
                    self.mm(sb_[:, :], k_[rows, kt * 128:(kt + 1) * 128], q_[rows, qs], True, True, [kB, qB], [sbB])
                    sd, sdB = SDr.next()
                    off = 512 * qb - 128 * kt + 1920
                    if kt % 2 == 0:
                        self.tt("dve", sd, sb_[:, :], strip[:, off:off + 512], ALU.mult, [sbB, stripB], [sdB])
                    else:
                        sc, scB = SCr.next()
                        self.cp("act", sc, sb_[:, :], [sbB], [scB])
                        self.tt("dve", sd, sc, strip[:, off:off + 512], ALU.mult, [scB, stripB], [sdB])
                    return sd, sdB
                cur, nxt = smm(0), smm(1)
                for kt in range(NTT):
                    nn = smm(kt + 2) if kt + 2 < NTT else None
                    sd, sdB = cur
                    self.mm(yb[:, :], v_[:, kt, :], sd, kt == 0, kt == NTT - 1, [vB, sdB], [ybB], signal=True)
                    cur, nxt = nxt, nn
                    if pending:
                        pending.pop(0)()
                while pending:
                    pending.pop(0)()
                pending.extend(stats_steps(h, qb, yb, ybB, g_, gB))
        while pending:
            pending.pop(0)()
        self.dbg_dump_y(2)

    def ffn(self, l):
        moe = (l % 2 == 1)
        li = l // 2
        NF = (D_FFE if moe else D_FF) // 128
        nexp = NEXP if moe else 1
        p0 = 0
        gTt, p0 = self.ovv(p0, NF * 1024)
        gTt = gTt.rearrange("p (f t) -> p f t", t=1024)
        gB = [[Buf() for _ in range(2)] for _ in range(NF)]
        facc, p0 = self.ovv(p0, KC * 1024, F32)
        facc = facc.rearrange("p (k t) -> p k t", t=1024)
        faccB = [[Buf() for _ in range(2)] for _ in range(KC)]
        gate2 = self.modT[:, l, 40:48]
        router = None
        if moe:
            wr, p0 = self.ovv(p0, KC * 8, F32)
            wr = wr.rearrange("p (k e) -> p k e", e=8)
            wrB = Buf()
            self.dma("sp", wr, self.mr_d[li].rearrange("(k p) e -> p k e", p=128), [], [wrB])
            lg, p0 = self.ovv(p0, 64, F32)
            lg = lg.rearrange("p (a e) -> p a e", e=8)
            lgB = Buf()
            router = {"w": (wr, wrB), "lg": (lg, lgB)}
            small = {}
            for nm, n in (("m1", 8), ("m2", 8), ("eq1", 64), ("eq2", 64), ("lg2", 64), ("dm", 8), ("w1", 8), ("w2", 8), ("wts", 64)):
                v, p0 = self.ovv(p0, n, F32)
                small[nm] = v
            smB = Buf()
            dg, p0 = self.ovv(p0, 128, F32)
            dgs = [(dg, Buf())]
            v, p0 = self.ovv(p0, 128, F32)
            dgs.append((v, Buf()))
            dgr = Ring(dgs)
            wbt, p0 = self.ovv(p0, NEXP * 1024)
            wbt = wbt.rearrange("p (e t) -> p e t", t=1024)
            wbB = [Buf() for _ in range(NEXP)]
        tms = []
        for i in range(2):
            s_, p0 = self.ovv(p0, 512, F32)
            g0, p0 = self.ovv(p0, 512)
            tms.append((s_, Buf(), g0, Buf()))
        tmr = Ring(tms)
        pn = 0

        steps = [(f0, min(4, NF - f0)) for f0 in range(0, NF, 4)]

        def wviews(ex):
            if moe:
                return (self.m1_d[li, ex].rearrange("(k p) n -> p k n", p=128), self.m3_d[li, ex].rearrange("(k p) n -> p k n", p=128),
                        self.m2_d[li, ex].rearrange("(f p) n -> p f n", p=128))
            return (self.f1_d[li].rearrange("(k p) n -> p k n", p=128), self.f3_d[li].rearrange("(k p) n -> p k n", p=128),
                    self.f2_d[li].rearrange("(f p) n -> p f n", p=128))
        jobs = []
        jidx = {}
        for ex in range(nexp):
            w1v, w3v, w2v = wviews(ex)
            for i, (f0, nf) in enumerate(steps):
                jidx[("f1", ex, i)] = len(jobs)
                jobs.append([(w1v[:, :, f0 * 128:(f0 + nf) * 128], KC, nf * 128), (w3v[:, :, f0 * 128:(f0 + nf) * 128], KC, nf * 128)])
            for o in range(KC):
                jidx[("f2", ex, o)] = len(jobs)
                jobs.append([(w2v[:, :, o * 128:(o + 1) * 128], NF, 128)])

        for hf in range(2):
            tbs = [2 * hf, 2 * hf + 1]
            ws = WStream(self, jobs)
            ws._try_issue()
            self.norm_phase(l, 1, tbs, pn, router=router)
            if moe:
                lg3 = lg
                m1, m2, eq1, eq2, lg2, dm, w1, w2, wts = (small[k] for k in ("m1", "m2", "eq1", "eq2", "lg2", "dm", "w1", "w2", "wts"))
                e3 = lambda v: v.rearrange("p (a e) -> p a e", e=8)
                b3 = lambda v: v.unsqueeze(2).to_broadcast([128, 8, 8])
                self.S.op("dve", lambda e: e.tensor_reduce(out=m1, in_=lg3, axis=mybir.AxisListType.X, op=ALU.max), [lgB], [smB])
                self.tt("dve", e3(eq1), lg3, b3(m1), ALU.is_ge, [lgB, smB], [smB])
                self.stt("dve", e3(lg2), e3(eq1), -1e30, lg3, ALU.mult, ALU.add, [smB, lgB], [smB])
                self.S.op("dve", lambda e: e.tensor_reduce(out=m2, in_=e3(lg2), axis=mybir.AxisListType.X, op=ALU.max), [smB], [smB])
                self.tt("dve", e3(eq2), e3(lg2), b3(m2), ALU.is_ge, [smB], [smB])
                self.tt("dve", dm, m1, m2, ALU.subtract, [smB], [smB])
                self.act(w1, dm, AF.Sigmoid, [smB], [smB])
                self.act(w2, dm, AF.Sigmoid, [smB], [smB], scale=-1.0)
                self.tt("dve", e3(eq1), e3(eq1), b3(w1), ALU.mult, [smB], [smB])
                self.tt("dve", e3(eq2), e3(eq2), b3(w2), ALU.mult, [smB], [smB])
                self.tt("dve", e3(wts), e3(eq1), e3(eq2), ALU.add, [smB], [smB])
                for ex in range(NEXP):
                    for a4 in range(2):
                        bk, bb = self.allbanks.next()
                        for a in range(4):
                            ta = a4 * 4 + a
                            dgt, dgB = dgr.next()
                            self.ts("dve", dgt, self.ident_f, e3(wts)[:, ta, ex:ex + 1], None, ALU.mult, None, [smB, self.cstB], [dgB])
                            self.mm(bk[:, a * 128:(a + 1) * 128], self.onesf[:], dgt, True, True, [dgB, self.onesfB], [bb])
                        self.cp("act", wbt[:, ex, a4 * 512:(a4 + 1) * 512], bk[:, :], [bb], [wbB[ex]])
            self.S.barrier()
            for ex in range(nexp):
                if moe:
                    w1v = self.m1_d[li, ex].rearrange("(k p) n -> p k n", p=128)
                    w3v = self.m3_d[li, ex].rearrange("(k p) n -> p k n", p=128)
                    w2v = self.m2_d[li, ex].rearrange("(f p) n -> p f n", p=128)
                else:
                    w1v = self.f1_d[li].rearrange("(k p) n -> p k n", p=128)
                    w3v = self.f3_d[li].rearrange("(k p) n -> p k n", p=128)
                    w2v = self.f2_d[li].rearrange("(f p) n -> p f n", p=128)
                for i, (f0, nf) in enumerate(steps):
                    jn = jidx[("f1", ex, i)]
                    (W1, w1b), (W3, w3b) = ws.get(jn)
                    for j in range(nf):
                        f = f0 + j
                        for t2 in range(2):
                            tb = tbs[t2]
                            ts_ = slice(tb * 512, (tb + 1) * 512)
                            b1, b1B = self.allbanks.next()
                            b3_, b3B = self.allbanks.next()
                            for kc in range(KC):
                                self.mm(b1[:, :], W1[:, kc, j * 128:(j + 1) * 128], self.RA[:, kc, ts_], kc == 0, kc == KC - 1,
                                        w1b + [self.RAb[kc][tb]], [b1B], signal=(kc == KC - 1))
                            for kc in range(KC):
                                self.mm(b3_[:, :], W3[:, kc, j * 128:(j + 1) * 128], self.RA[:, kc, ts_], kc == 0, kc == KC - 1,
                                        w3b + [self.RAb[kc][tb]], [b3B], signal=(kc == KC - 1))
                            s_, sB, g0, g0B = tmr.next()
                            self.act(s_, b1[:, :], AF.Silu, [b1B], [sB])
                            gd = gTt[:, f, t2 * 512:(t2 + 1) * 512]
                            if moe:
                                self.tt("dve", g0, b3_[:, :], s_, ALU.mult, [b3B, sB], [g0B])
                                self.tt("pool", gd, g0, wbt[:, ex, t2 * 512:(t2 + 1) * 512], ALU.mult, [g0B, wbB[ex]], [gB[f][t2]])
                            else:
                                self.tt("dve", gd, b3_[:, :], s_, ALU.mult, [b3B, sB], [gB[f][t2]])
                    ws.finish(jn)
                for o in range(KC):
                    jn = jidx[("f2", ex, o)]
                    ((W2, w2b),) = ws.get(jn)
                    for t2 in range(2):
                        bk, bb = self.allbanks.next()
                        for f in range(NF):
                            self.mm(bk[:, :], W2[:, f, :], gTt[:, f, t2 * 512:(t2 + 1) * 512], f == 0, f == NF - 1, w2b + [gB[f][t2]], [bb],
                                    signal=(f == NF - 1))
                        fa = facc[:, o, t2 * 512:(t2 + 1) * 512]
                        if ex == 0:
                            self.cp("act", fa, bk[:, :], [bb], [faccB[o][t2]])
                        else:
                            self.tt("dve", fa, fa, bk[:, :], ALU.add, [bb, faccB[o][t2]], [faccB[o][t2]])
                    ws.finish(jn)
            xsv = self.xs.rearrange("k p t -> p k t")
            pp = pn
            xi = []
            for i in range(2):
                v, pp = self.ovv(pp, KC * 512, F32)
                xi.append((v.rearrange("p (k t) -> p k t", k=KC), Buf()))
            self.S.barrier()
            for t2 in range(2):
                tb = tbs[t2]
                x, xB = xi[t2]
                self.dma("sp", x, xsv[:, :, tb * 512:(tb + 1) * 512], [self.xsB[tb]], [xB])
                for o in range(KC):
                    self.stt("dve", x[:, o, :], facc[:, o, t2 * 512:(t2 + 1) * 512], gate2[:, o:o + 1], x[:, o, :], ALU.mult, ALU.add,
                             [faccB[o][t2], xB, self.modB], [xB])
                self.dma("sp", xsv[:, :, tb * 512:(tb + 1) * 512], x, [xB], [self.xsB[tb]])
            self.S.barrier()

    def final(self):
        p = 0
        xi = []
        for i in range(2):
            v, p = self.ovv(p, KC * 512, F32)
            xi.append((v.rearrange("p (k t) -> p k t", k=KC), Buf()))
        xir = Ring(xi)
        sq = []
        for i in range(2):
            v, p = self.ovv(p, 512)
            sq.append((v, Buf()))
        sqr = Ring(sq)
        rt = []
        for i in range(2):
            v, p = self.ovv(p, 512, F32)
            rt.append((v, Buf()))
        rtr = Ring(rt)
        tm = []
        for i in range(3):
            v, p = self.ovv(p, 512, F32)
            tm.append((v, Buf()))
        tmr = Ring(tm)
        ob = []
        for i in range(2):
            v, p = self.ovv(p, 4 * D, F32)
            ob.append((v.rearrange("p (a k m) -> p a k m", a=4, k=KC), Buf()))
        obr = Ring(ob)
        xsv = self.xs.rearrange("k p t -> p k t")
        ov = self.out_d.rearrange("(a p) d -> p a d", p=128)
        for tb in range(NTB):
            x, xB = xir.next()
            self.dma("sp", x, xsv[:, :, tb * 512:(tb + 1) * 512], [self.xsB[tb]], [xB])
            bk, bb = self.allbanks.next()
            for kc in range(KC):
                s, sB = sqr.next()
                self.act(s, x[:, kc, :], AF.Square, [xB], [sB])
                self.mm(bk[:, :], self.ones_b, s, kc == 0, kc == KC - 1, [sB, self.cbfB], [bb], signal=True)
            r, rB = rtr.next()
            self.act(r, bk[:, :], AF.Sqrt, [bb, self.cstB], [rB], scale=1.0 / D, bias=self.eps_ap)
            self.recip(r, r, [rB], [rB])
            o_, oB = obr.next()
            for kc in range(KC):
                t, tB = tmr.next()
                self.stt("dve", t, x[:, kc, :], self.finT[:, kc:kc + 1], r, ALU.mult, ALU.mult, [xB, rB, self.vecB], [tB])
                b2, b2B = self.allbanks.next()
                for a in range(4):
                    self.tr(b2[:, a * 128:(a + 1) * 128], t[:, a * 128:(a + 1) * 128], self.ident_f, [tB, self.cstB], [b2B])
                self.cp("act", o_[:, :, kc, :], b2[:, :].rearrange("p (a m) -> p a m", a=4), [b2B], [oB])
            self.dma("sp", ov[:, 4 * tb:4 * tb + 4, :], o_.rearrange("p a k m -> p a (k m)"), [oB], [self.outB[tb]])


_CACHE = {}


def _vecpack(inputs):
    vp = np.zeros((DEPTH, NVEC, 128), np.float32)
    for l in range(DEPTH):
        vp[l, 0:48] = np.asarray(inputs["ada_b"][l], np.float32).reshape(48, 128)
        vp[l, 48:56] = np.asarray(inputs["norm_mix"][l], np.float32).reshape(8, 128)
        vp[l, 56:64] = np.asarray(inputs["norm_ffn"][l], np.float32).reshape(8, 128)
        vp[l, 64:70] = np.asarray(inputs["mla_q_norm"][l], np.float32).reshape(6, 128)
        vp[l, 70:74] = np.asarray(inputs["mla_kv_norm"][l], np.float32).reshape(4, 128)
        vp[l, 74:82] = np.asarray(inputs["ret_norm"][l], np.float32).reshape(8, 128)
    return vp


def make_in_maps(inputs, ncores=NCORES):
    cst, dpn = make_consts()
    f = lambda k: np.ascontiguousarray(np.asarray(inputs[k], np.float32))
    shared = {
        "ada_w": f("ada_w"), "vecpack": _vecpack(inputs), "final_norm": f("final_norm").reshape(8, 128),
        "w_in": f("w_in"), "mla_w_uq": f("mla_w_uq"), "mla_w_ukv": f("mla_w_ukv"),
        "ret_log_decay": f("ret_log_decay").reshape(1, DEPTH * 16),
        "w_br_mla": f("w_br_mla"), "w_br_dil": f("w_br_dil"), "w_br_ret": f("w_br_ret"), "w_out": f("w_out"),
        "ffn_w1": f("ffn_w1"), "ffn_w3": f("ffn_w3"), "ffn_w2": f("ffn_w2"),
        "moe_router": f("moe_router"), "moe_w1": f("moe_w1"), "moe_w3": f("moe_w3"), "moe_w2": f("moe_w2"),
        "consts": cst, "dposneg": dpn,
    }
    x = f("x")
    c = f("c")
    pos = np.ascontiguousarray(np.asarray(inputs["positions"], np.int32))
    maps = []
    for b in range(ncores):
        m = dict(shared)
        m["x"] = x[b]
        m["c"] = c[b:b + 1]
        m["positions"] = pos[b:b + 1]
        maps.append(m)
    return maps


def kernel(**inputs):
    if "nc" not in _CACHE:
        _CACHE["nc"] = Builder().build()
    nc = _CACHE["nc"]
    maps = make_in_maps(inputs)
    res = run_bass_kernel_spmd(nc, maps, core_ids=list(range(NCORES)))
    return np.stack([np.asarray(r["out"], np.float32) for r in res.results], axis=0)
```

```python
import numpy as np
from contextlib import ExitStack
import concourse.bass as bass
import concourse.mybir as mybir
from concourse.bass_utils import run_bass_kernel_spmd

F32 = mybir.dt.float32
BF16 = mybir.dt.bfloat16
I32 = mybir.dt.int32
AF = mybir.ActivationFunctionType
ALU = mybir.AluOpType

D = 1024
SEQ = 2048
DEPTH = 4
NCORES = 8
KC = 8
NTB = 4
NTT = 16
EPS = 1e-6
IN_COLS = 12096
C_CQ, C_CKV, C_KR, C_DQ, C_DK, C_DV, C_RQ, C_RK, C_RV, C_RG, C_GA, C_GB, C_GC = (
    0, 768, 1280, 1344, 2880, 4416, 5952, 6464, 6976, 8000, 9024, 10048, 11072)
DIL_D = (1, 4, 16)
D_FF = 2816
D_FFE = 3584
NEXP = 8
STRIPW = 3968
NVEC = 82

SEM_LIMIT = 30000
DMA_SEMS_PER_QUEUE = 20


class Buf:
    __slots__ = ("name", "w", "r")

    def __init__(self, name=""):
        self.name = name
        self.w = {}
        self.r = {}


class Sched:
    ENGS = ("pe", "act", "dve", "pool", "sp")

    def __init__(self, nc, stack, nsem=96):
        self.nc = nc
        self.sems = [stack.enter_context(nc.semaphore(f"s{i}")) for i in range(nsem)]
        self.sem_next = 0
        self.streams = {e: [] for e in self.ENGS}
        self.cur = {}
        for e in self.ENGS:
            self.cur[e] = [self._new_sem(), 0]
        self.seen = {e: {} for e in self.ENGS}
        self.dq = {}
        for q in ("sp", "pool"):
            self.dq[q] = {"sems": [[self._new_sem(), 0] for _ in range(DMA_SEMS_PER_QUEUE)], "next": 0}
        self.n_ins = 0

    def _new_sem(self):
        i = self.sem_next
        self.sem_next += 1
        assert i < len(self.sems), "out of semaphores"
        return i

    def _wait(self, eng, sidx, val):
        if self.seen[eng].get(sidx, 0) >= val:
            return
        self.seen[eng][sidx] = val
        sem = self.sems[sidx]
        self.streams[eng].append(lambda e, sem=sem, val=val: e.wait_ge(sem, val))

    def _collect(self, eng, reads, writes, same_engine_ok=False):
        deps = {}
        for b in reads:
            for s, v in b.w.items():
                if deps.get(s, 0) < v:
                    deps[s] = v
        for b in writes:
            for s, v in b.w.items():
                if deps.get(s, 0) < v:
                    deps[s] = v
            for s, v in b.r.items():
                if deps.get(s, 0) < v:
                    deps[s] = v
        own = self.cur[eng][0]
        for s, v in deps.items():
            if same_engine_ok and s == own:
                continue
            self._wait(eng, s, v)

    def _mark(self, ticket, reads, writes):
        s, v = ticket
        for b in reads:
            if b.r.get(s, 0) < v:
                b.r[s] = v
        for b in writes:
            b.w = {s: v}
            b.r = {}

    def op(self, eng, fn, reads=(), writes=(), signal=True):
        self._collect(eng, reads, writes, same_engine_ok=(eng == "pe"))
        cur = self.cur[eng]
        if signal:
            if cur[1] >= SEM_LIMIT:
                cur[0] = self._new_sem()
                cur[1] = 0
            cur[1] += 1
            sem = self.sems[cur[0]]
            self.streams[eng].append(lambda e, fn=fn, sem=sem: fn(e).then_inc(sem, 1))
            ticket = (cur[0], cur[1])
        else:
            assert eng == "pe"
            if cur[1] + 1 > SEM_LIMIT:
                cur[0] = self._new_sem()
                cur[1] = 0
            self.streams[eng].append(lambda e, fn=fn: fn(e))
            ticket = (cur[0], cur[1] + 1)
        self._mark(ticket, reads, writes)
        self.n_ins += 1
        return ticket

    def dma(self, q, fns, reads=(), writes=()):
        if not isinstance(fns, (list, tuple)):
            fns = [fns]
        self._collect(q, reads, writes)
        pool = self.dq[q]
        slot = pool["sems"][pool["next"]]
        pool["next"] = (pool["next"] + 1) % len(pool["sems"])
        if slot[1] > 0:
            self._wait(q, slot[0], slot[1])
        if slot[1] + 16 * len(fns) > SEM_LIMIT:
            slot[0] = self._new_sem()
            slot[1] = 0
        sem = self.sems[slot[0]]
        for fn in fns:
            slot[1] += 16
            self.streams[q].append(lambda e, fn=fn, sem=sem: fn(e).then_inc(sem, 16))
        ticket = (slot[0], slot[1])
        self._mark(ticket, reads, writes)
        self.n_ins += len(fns)
        return ticket

    def barrier(self):
        tickets = []
        for e in self.ENGS:
            c = self.cur[e]
            if c[1] > 0:
                tickets.append((c[0], c[1]))
        for q in self.dq.values():
            for s in q["sems"]:
                if s[1] > 0:
                    tickets.append((s[0], s[1]))
        for e in self.ENGS:
            for s, v in tickets:
                if s == self.cur[e][0] and e == "pe":
                    continue
                self._wait(e, s, v)

    def wait_all(self, eng, bufs):
        for b in bufs:
            for s, v in b.w.items():
                self._wait(eng, s, v)

    def emit(self):
        nc = self.nc
        streams = self.streams
        with nc.Block() as block:
            @block.tensor
            def _(e):
                for f in streams["pe"]:
                    f(e)

            @block.scalar
            def _(e):
                for f in streams["act"]:
                    f(e)

            @block.vector
            def _(e):
                for f in streams["dve"]:
                    f(e)

            @block.gpsimd
            def _(e):
                for f in streams["pool"]:
                    f(e)

            @block.sync
            def _(e):
                for f in streams["sp"]:
                    f(e)


class WStream:
    def __init__(self, bld, jobs):
        self.b = bld
        self.jobs = jobs
        self.views = {}
        self.nxt = 0
        self.done = -1
        self.slot_ctr = 0
        self.slot_job = [-1, -1, -1, -1]

    def _try_issue(self):
        while self.nxt < len(self.jobs):
            need = len(self.jobs[self.nxt])
            slots = [(self.slot_ctr + i) % 4 for i in range(need)]
            if any(self.slot_job[s_] > self.done for s_ in slots):
                return
            vs = []
            for s_, (src, nk, ncols) in zip(slots, self.jobs[self.nxt]):
                vs.append(self.b.wload([s_], src, 128, nk, ncols))
                self.slot_job[s_] = self.nxt
            self.slot_ctr = (self.slot_ctr + need) % 4
            self.views[self.nxt] = vs
            self.nxt += 1

    def get(self, j):
        self._try_issue()
        assert j in self.views, (j, self.nxt, self.done, self.slot_job)
        return self.views.pop(j)

    def finish(self, j):
        self.done = j
        self._try_issue()


class Ring:
    def __init__(self, items):
        self.items = items
        self.i = 0

    def next(self):
        it = self.items[self.i]
        self.i = (self.i + 1) % len(self.items)
        return it


def make_consts():
    c = np.zeros((128, 648), np.float32)
    c[:, 0:128] = np.eye(128, dtype=np.float32)
    for m in range(128):
        if m % 64 < 32:
            c[m + 32, 128 + m] = -1.0
        else:
            c[m - 32, 128 + m] = 1.0
    a = np.arange(128)[:, None]
    b = np.arange(128)[None, :]
    c[:, 256:384] = (a >= b + 64)
    c[:, 384:512] = (np.abs(a - b) <= 64)
    c[:, 512:640] = (a <= b - 64)
    inv_freq = (10000.0 ** (-np.arange(0, 64, 2, dtype=np.float32) / 64.0)).astype(np.float32)
    c[:, 640] = inv_freq[np.arange(128) % 32]
    c[:, 641] = EPS
    dm = (np.arange(STRIPW)[None, :] - 1920 - np.arange(128)[:, None]).astype(np.float32)
    dpn = np.stack([np.maximum(dm, 0.0), np.minimum(dm, 0.0)]).astype(np.float32)
    return c, dpn


class Builder:
    def __init__(self, n_layers=DEPTH, dbg=None):
        self.n_layers = n_layers
        self.dbg = dbg or {}
        self.nc = bass.Bass("TRN2", target_bir_lowering=False)

    def mm(self, out, lhsT, rhs, start, stop, reads, writes, signal=True):
        self.S.op("pe", lambda e: e.matmul(out, lhsT=lhsT, rhs=rhs, start=start, stop=stop), reads, writes, signal)

    def tr(self, out, in_, ident, reads, writes):
        self.S.op("pe", lambda e: e.transpose(out=out, in_=in_, identity=ident), reads, writes)

    def act(self, out, in_, func, reads, writes, scale=None, bias=None):
        kw = {}
        if scale is not None:
            kw["scale"] = scale
            if func == AF.Copy:
                func = AF.Identity
        if bias is not None:
            kw["bias"] = bias
        self.S.op("act", lambda e: e.activation(out=out, in_=in_, func=func, **kw), reads, writes)

    def tt(self, eng, out, in0, in1, op, reads, writes):
        self.S.op(eng, lambda e: e.tensor_tensor(out=out, in0=in0, in1=in1, op=op), reads, writes)

    def ts(self, eng, out, in0, s1, s2, op0, op1, reads, writes):
        if op1 is None:
            self.S.op(eng, lambda e: e.tensor_scalar(out=out, in0=in0, scalar1=s1, scalar2=None, op0=op0), reads, writes)
        else:
            self.S.op(eng, lambda e: e.tensor_scalar(out=out, in0=in0, scalar1=s1, scalar2=s2, op0=op0, op1=op1), reads, writes)

    def stt(self, eng, out, in0, scalar, in1, op0, op1, reads, writes):
        self.S.op(eng, lambda e: e.scalar_tensor_tensor(out=out, in0=in0, scalar=scalar, in1=in1, op0=op0, op1=op1), reads, writes)

    def cp(self, eng, out, in_, reads, writes):
        if eng == "act":
            self.act(out, in_, AF.Copy, reads, writes)
        else:
            self.S.op(eng, lambda e: e.tensor_copy(out=out, in_=in_), reads, writes)

    def recip(self, out, in_, reads, writes):
        self.S.op("dve", lambda e: e.reciprocal(out=out, in_=in_), reads, writes)

    def memset(self, eng, ap, val, writes):
        self.S.op(eng, lambda e: e.memset(ap, val), (), writes)

    def dma(self, q, out, in_, reads, writes, slow=False):
        if slow:
            self.S.dma(q, lambda e: e.dma_start(out=out, in_=in_, allow_slow_non_contiguous=True), reads, writes)
        else:
            self.S.dma(q, lambda e: e.dma_start(out=out, in_=in_), reads, writes)

    def bank(self, ring):
        return ring.next()

    def build(self):
        nc = self.nc
        L = self.n_layers
        din = lambda name, shape, dt=F32: nc.dram_tensor(name, list(shape), dt, kind="ExternalInput").ap()
        self.x_d = din("x", [SEQ, D])
        self.c_d = din("c", [1, D])
        self.pos_d = din("positions", [1, SEQ], I32)
        self.adaw_d = din("ada_w", [DEPTH, D, 6 * D])
        self.vec_d = din("vecpack", [DEPTH, NVEC, 128])
        self.fin_d = din("final_norm", [8, 128])
        self.win_d = din("w_in", [DEPTH, D, IN_COLS])
        self.wuq_d = din("mla_w_uq", [DEPTH, 768, 1536])
        self.wukv_d = din("mla_w_ukv", [DEPTH, 512, 2048])
        self.rld_d = din("ret_log_decay", [1, DEPTH * 16])
        self.wbm_d = din("w_br_mla", [DEPTH, 1024, 1024])
        self.wbd_d = din("w_br_dil", [DEPTH, 512, 1024])
        self.wbr_d = din("w_br_ret", [DEPTH, 1024, 1024])
        self.wout_d = din("w_out", [DEPTH, 1024, 1024])
        self.f1_d = din("ffn_w1", [2, D, D_FF])
        self.f3_d = din("ffn_w3", [2, D, D_FF])
        self.f2_d = din("ffn_w2", [2, D_FF, D])
        self.mr_d = din("moe_router", [2, D, NEXP])
        self.m1_d = din("moe_w1", [2, NEXP, D, D_FFE])
        self.m3_d = din("moe_w3", [2, NEXP, D, D_FFE])
        self.m2_d = din("moe_w2", [2, NEXP, D_FFE, D])
        self.cst_d = din("consts", [128, 648])
        self.dpn_d = din("dposneg", [2, 128, STRIPW])
        self.out_d = nc.dram_tensor("out", [SEQ, D], F32, kind="ExternalOutput").ap()

        def scratch(name, shape, dt=BF16):
            kind = "ExternalOutput" if name in self.dbg else "Internal"
            return nc.dram_tensor(name, list(shape), dt, kind=kind).ap()
        self.xs = scratch("xs", [KC, 128, SEQ], F32)
        self.mg = scratch("mg", [KC, 128, SEQ], F32)
        self.qT = scratch("qT", [8, 192, SEQ])
        self.knT = scratch("knT", [8, 128, SEQ])
        self.vm = scratch("vm", [SEQ, 1024])
        self.dqT = scratch("dqT", [3, 512, SEQ])
        self.dkT = scratch("dkT", [3, 512, SEQ])
        self.dv = scratch("dv", [3, SEQ, 520])
        self.rqT = scratch("rqT", [512, SEQ])
        self.rkT = scratch("rkT", [512, SEQ])
        self.rv = scratch("rv", [SEQ, 1024])
        self.rgT = scratch("rgT", [1024, SEQ])
        self.gT = scratch("gT", [3, 1024, SEQ])
        self.ydbg = scratch("ydbg", [3, 1024, SEQ]) if "ydbg" in self.dbg else None
        self.xsB = [Buf("xs%d" % t) for t in range(NTB)]
        self.mgB = [Buf("mg%d" % t) for t in range(NTB)]
        self.qTB = [Buf() for _ in range(8)]
        self.knTB = [Buf() for _ in range(8)]
        self.vmB = [Buf() for _ in range(2)]
        self.dqTB = [[Buf() for _ in range(4)] for _ in range(3)]
        self.dkTB = [[Buf() for _ in range(4)] for _ in range(3)]
        self.dvB = [Buf() for _ in range(3)]
        self.rqTB = [Buf() for _ in range(4)]
        self.rkTB = [Buf() for _ in range(4)]
        self.rvB = [Buf() for _ in range(2)]
        self.rgTB = [Buf() for _ in range(8)]
        self.gTB = [[Buf() for _ in range(8)] for _ in range(3)]
        self.outB = [Buf() for _ in range(NTB)]
        self.dbgB = Buf()

        with ExitStack() as st:
            self.st = st
            self.S = Sched(nc, st)
            sb = lambda n, sh, dt: st.enter_context(nc.sbuf_tensor(n, sh, dt))
            self.pb = [st.enter_context(nc.psum_tensor("pb%d" % i, [128, 512], F32)) for i in range(8)]
            self.PB = [Buf("pb%d" % i) for i in range(8)]
            self.allbanks = Ring([(self.pb[i], self.PB[i]) for i in range(8)])
            self.RA = sb("RA", [128, KC, SEQ], BF16)
            self.RAb = [[Buf("RA%d_%d" % (k, t)) for t in range(NTB)] for k in range(KC)]
            self.RW = sb("RW", [128, 4, 4096], BF16)
            self.WB = [Buf("W%d" % i) for i in range(4)]
            self.cst = sb("cst", [128, 648], F32); self.cstB = Buf("cst")
            self.cbf = sb("cbf", [128, 1920], BF16); self.cbfB = Buf("cbf")
            self.onesf = sb("onesf", [128, 128], F32); self.onesfB = Buf("onesf")
            self.avgf = sb("avgf", [128, 128], F32)
            self.cosT = sb("cosT", [128, SEQ], BF16); self.sinT = sb("sinT", [128, SEQ], BF16); self.csB = Buf("cs")
            self.krT = sb("krT", [128, SEQ], BF16); self.krB = [Buf() for _ in range(NTB)]
            self.modT = sb("modT", [128, DEPTH, 48], F32); self.modB = Buf("mod")
            self.vecT = sb("vecT", [128, DEPTH, NVEC], F32); self.vecB = Buf("vec")
            self.finT = sb("finT", [128, 8], F32)
            self.gsT = sb("gsT", [128, DEPTH, 16], F32); self.gsB = Buf("gs")
            self.rldT = sb("rldT", [128, DEPTH * 16], F32); self.rldB = Buf("rld")
            self.cact = sb("cact", [128, 8], BF16); self.cactB = Buf("cact")
            self.OVN = 58368
            self.OV = sb("OV", [128, self.OVN], BF16)
            self.ident_f = self.cst[:, 0:128]
            self.ident_b = self.cbf[:, 0:128]
            self.perm_b = self.cbf[:, 128:256]
            self.ones_b = self.cbf[:, 256:384]
            self.mask_b = self.cbf[:, 384:768]
            self.negm_b = self.cbf[:, 768:1152]
            self.negmT_b = self.cbf[:, 1152:1536]
            self.maskT_b = self.cbf[:, 1536:1920]
            self.eps_ap = self.cst[:, 641:642]

            self.setup()
            for l in range(L):
                self.layer(l)
            self.final()
            if self.dbg:
                self.S.wait_all("sp", [self.dbgB])
            self.S.wait_all("sp", self.outB)
            self.S.emit()
        return nc

    def ovv(self, off, n, dt=BF16):
        nb = n * (2 if dt == F32 else 1)
        assert off + nb <= self.OVN, (off, nb, self.OVN)
        v = self.OV[:, off:off + nb]
        if dt == F32:
            v = v.bitcast(F32)
        return v, off + nb

    def wload(self, slots, src_ap, kp, nk, ncols):
        n = nk * ncols
        assert n <= 4096 * len(slots)
        s0 = slots[0]
        if len(slots) == 1:
            flat = self.RW[0:kp, s0, 0:n]
        else:
            flat = self.RW[0:kp, s0:s0 + len(slots), :].rearrange("p a b -> p (a b)")[:, 0:n]
        view = flat.rearrange("p (k n) -> p k n", n=ncols)
        bufs = [self.WB[s] for s in slots]
        self.dma("pool", view, src_ap, [], bufs)
        return view, bufs

    def setup(self):
        S = self.S
        o = 0
        self.dma("sp", self.cst[:], self.cst_d, [], [self.cstB])
        self.cp("act", self.cbf[:, 0:256], self.cst[:, 0:256], [self.cstB], [self.cbfB])
        self.memset("dve", self.cbf[:, 256:384], 1.0, [self.cbfB])
        self.cp("act", self.cbf[:, 384:768], self.cst[:, 256:640], [self.cstB], [self.cbfB])
        self.memset("dve", self.onesf[:], 1.0, [self.onesfB])
        self.memset("dve", self.avgf[:], 1.0 / 128, [self.onesfB])
        self.ts("dve", self.cbf[:, 768:1152], self.cst[:, 256:640], 1.0, 30000.0, ALU.subtract, ALU.mult, [self.cstB], [self.cbfB])
        for j in range(3):
            self.cp("dve", self.cbf[:, 1536 + j * 128:1536 + (j + 1) * 128], self.cst[:, 256 + (2 - j) * 128:256 + (3 - j) * 128], [self.cstB], [self.cbfB])
        for j in range(3):
            self.ts("dve", self.cbf[:, 1152 + j * 128:1152 + (j + 1) * 128], self.cst[:, 256 + (2 - j) * 128:256 + (3 - j) * 128], 1.0, 30000.0,
                    ALU.subtract, ALU.mult, [self.cstB], [self.cbfB])
        self.dma("sp", self.rldT[:], self.rld_d.partition_broadcast(128), [], [self.rldB])
        for l in range(DEPTH):
            self.ts("dve", self.rldT[:, l * 16 + 8:l * 16 + 16], self.rldT[:, l * 16 + 8:l * 16 + 16], -1.0, None, ALU.mult, None,
                    [self.rldB], [self.rldB])
        posi, o1 = self.ovv(0, SEQ, F32)
        posi = self.OV[:, 0:2 * SEQ].bitcast(I32)
        ang, o2 = self.ovv(o1, SEQ, F32)
        kf, o3 = self.ovv(o2, SEQ, F32)
        ki = self.OV[:, o3:o3 + 2 * SEQ].bitcast(I32)
        o4 = o3 + 2 * SEQ
        a2, o5 = self.ovv(o4, SEQ, F32)
        Bp, Ba, Bk, Bki, Ba2 = Buf(), Buf(), Buf(), Buf(), Buf()
        self.dma("sp", posi, self.pos_d.partition_broadcast(128), [], [Bp])
        self.cp("dve", ang, posi, [Bp], [Ba])
        self.ts("dve", ang, ang, self.cst[:, 640:641], None, ALU.mult, None, [Ba, self.cstB], [Ba])
        TWO_PI = float(2 * np.pi)

        def reduce_sin(src, dst_bf, shift):
            self.ts("dve", a2, src, float(shift), None, ALU.add, None, [Ba], [Ba2])
            self.ts("dve", kf, a2, float(1.0 / TWO_PI), None, ALU.mult, None, [Ba2], [Bk])
            self.cp("dve", ki, kf, [Bk], [Bki])
            self.cp("dve", kf, ki, [Bki], [Bk])
            self.stt("dve", a2, kf, -TWO_PI, a2, ALU.mult, ALU.add, [Bk, Ba2], [Ba2])
            self.ts("dve", kf, a2, float(np.pi), -TWO_PI, ALU.is_gt, ALU.mult, [Ba2], [Bk])
            self.tt("dve", a2, a2, kf, ALU.add, [Bk, Ba2], [Ba2])
            self.ts("dve", kf, a2, float(-np.pi), TWO_PI, ALU.is_lt, ALU.mult, [Ba2], [Bk])
            self.tt("dve", a2, a2, kf, ALU.add, [Bk, Ba2], [Ba2])
            self.act(dst_bf, a2, AF.Sin, [Ba2], [self.csB])
        reduce_sin(ang, self.sinT[:], 0.0)
        reduce_sin(ang, self.cosT[:], np.pi / 2)
        vst, o6 = self.ovv(o5, DEPTH * 128 + 128 + 128, F32)
        Bv = Buf()
        for l in range(DEPTH):
            self.dma("sp", vst[0:NVEC, l * 128:(l + 1) * 128], self.vec_d[l], [], [Bv])
        self.dma("sp", vst[0:8, 512:640], self.fin_d, [], [Bv])
        self.dma("sp", vst[0:8, 640:768], self.c_d.rearrange("o (k p) -> (o k) p", p=128), [], [Bv])
        for l in range(DEPTH):
            bk, bb = self.allbanks.next()
            self.tr(bk[:, 0:NVEC], vst[0:NVEC, l * 128:(l + 1) * 128], self.ident_f[0:NVEC, 0:NVEC], [Bv, self.cstB], [bb])
            self.cp("dve", self.vecT[:, l, :], bk[:, 0:NVEC], [bb], [self.vecB])
        bk, bb = self.allbanks.next()
        self.tr(bk[:, 0:8], vst[0:8, 512:640], self.ident_f[0:8, 0:8], [Bv, self.cstB], [bb])
        self.tr(bk[:, 8:16], vst[0:8, 640:768], self.ident_f[0:8, 0:8], [Bv, self.cstB], [bb])
        self.cp("act", self.finT[:], bk[:, 0:8], [bb], [self.vecB])
        self.act(self.cact[:], bk[:, 8:16], AF.Silu, [bb], [self.cactB])
        nblk = 0
        pending = []
        jobs = [(l, cb) for l in range(DEPTH) for cb in range(12)]

        def issue(i):
            l, cb = jobs[i]
            src = self.adaw_d[l].rearrange("(k p) n -> p k n", p=128)[:, :, cb * 512:(cb + 1) * 512]
            return self.wload([i % 4], src, 128, KC, 512)
        for i in range(min(3, len(jobs))):
            pending.append(issue(i))
        for i, (l, cb) in enumerate(jobs):
            W, wb = pending.pop(0)
            if i + 3 < len(jobs):
                pending.append(issue(i + 3))
            if cb % 12 == 0:
                mbk, mbb = self.allbanks.next()
            for j in range(4):
                col = cb * 4 + j
                for kc in range(KC):
                    self.mm(mbk[:, col:col + 1], W[:, kc, j * 128:(j + 1) * 128], self.cact[:, kc:kc + 1], kc == 0, kc == KC - 1,
                            wb + [self.cactB], [mbb], signal=(kc == KC - 1))
            if cb == 11:
                self.tt("dve", self.modT[:, l, :], mbk[:, 0:48], self.vecT[:, l, 0:48], ALU.add, [mbb, self.vecB], [self.modB])
                self.stt("dve", self.gsT[:, l, 0:8], self.modT[:, l, 8:16], 1.0, self.vecT[:, l, 48:56], ALU.add, ALU.mult,
                         [self.modB, self.vecB], [self.gsB])
                self.stt("dve", self.gsT[:, l, 8:16], self.modT[:, l, 32:40], 1.0, self.vecT[:, l, 56:64], ALU.add, ALU.mult,
                         [self.modB, self.vecB], [self.gsB])
        xt0, p = self.ovv(o6, 4 * D, F32)
        xt1, p = self.ovv(p, 4 * D, F32)
        xs0, p = self.ovv(p, KC * 512, F32)
        xs1, p = self.ovv(p, KC * 512, F32)
        xtr = Ring([(xt0.rearrange("p (a d) -> p a d", a=4), Buf()), (xt1.rearrange("p (a d) -> p a d", a=4), Buf())])
        xsr = Ring([(xs0.rearrange("p (k t) -> p k t", k=KC), Buf()), (xs1.rearrange("p (k t) -> p k t", k=KC), Buf())])
        xv = self.x_d.rearrange("(a p) d -> p a d", p=128)
        for tb in range(NTB):
            xt, xtB = xtr.next()
            self.dma("sp", xt, xv[:, 4 * tb:4 * tb + 4, :], [], [xtB])
            xo, xoB = xsr.next()
            for kc in range(KC):
                bk, bb = self.allbanks.next()
                for a in range(4):
                    self.tr(bk[:, a * 128:(a + 1) * 128], xt[:, a, kc * 128:(kc + 1) * 128], self.ident_f, [xtB, self.cstB], [bb])
                self.cp("act" if kc % 2 else "dve", xo[:, kc, :], bk[:, :], [bb], [xoB])
            self.dma("sp", self.xs.rearrange("k p t -> p k t")[:, :, tb * 512:(tb + 1) * 512], xo, [xoB], [self.xsB[tb]])
        S.barrier()

    def norm_phase(self, l, which, tbs, ov0, router=None):
        gs = self.gsT[:, l, 8 * which:8 * which + 8]
        sh = self.modT[:, l, (0 if which == 0 else 24):(0 if which == 0 else 24) + 8]
        p = ov0
        xi = []
        for i in range(2):
            v, p = self.ovv(p, KC * 512, F32)
            xi.append((v.rearrange("p (k t) -> p k t", k=KC), Buf()))
        xir = Ring(xi)
        sq = []
        for i in range(2):
            v, p = self.ovv(p, 512)
            sq.append((v, Buf()))
        sqr = Ring(sq)
        rt = []
        for i in range(2):
            v, p = self.ovv(p, 512, F32)
            rt.append((v, Buf()))
        rtr = Ring(rt)
        tm = []
        for i in range(2):
            v, p = self.ovv(p, 512, F32)
            tm.append((v, Buf()))
        tmr = Ring(tm)
        h32 = []
        if router is not None:
            for i in range(2):
                v, p = self.ovv(p, 512, F32)
                h32.append((v, Buf()))
            h32r = Ring(h32)
        xsv = self.xs.rearrange("k p t -> p k t")
        loaded = {}

        def load(tb):
            x, xB = xir.next()
            self.dma("sp", x, xsv[:, :, tb * 512:(tb + 1) * 512], [self.xsB[tb]], [xB])
            loaded[tb] = (x, xB)
        load(tbs[0])
        for i, tb in enumerate(tbs):
            if i + 1 < len(tbs):
                load(tbs[i + 1])
            x, xB = loaded.pop(tb)
            bk, bb = self.allbanks.next()
            for kc in range(KC):
                s, sB = sqr.next()
                self.act(s, x[:, kc, :], AF.Square, [xB], [sB])
                self.mm(bk[:, :], self.ones_b, s, kc == 0, kc == KC - 1, [sB, self.cbfB], [bb], signal=True)
            r, rB = rtr.next()
            self.act(r, bk[:, :], AF.Sqrt, [bb, self.cstB], [rB], scale=1.0 / D, bias=self.eps_ap)
            self.recip(r, r, [rB], [rB])
            if router is not None:
                lgbk, lgbb = self.allbanks.next()
            for kc in range(KC):
                t, tB = tmr.next()
                self.tt("dve", t, x[:, kc, :], r, ALU.mult, [xB, rB], [tB])
                self.act(self.RA[:, kc, tb * 512:(tb + 1) * 512], t, AF.Identity, [tB, self.gsB, self.modB], [self.RAb[kc][tb]],
                         scale=gs[:, kc:kc + 1], bias=sh[:, kc:kc + 1])
                if router is not None:
                    h, hB = h32r.next()
                    self.act(h, t, AF.Identity, [tB, self.gsB, self.modB], [hB], scale=gs[:, kc:kc + 1], bias=sh[:, kc:kc + 1])
                    wr, wrB = router["w"]
                    for a in range(4):
                        self.mm(lgbk[:, a * 8:(a + 1) * 8], h[:, a * 128:(a + 1) * 128], wr[:, kc, :], (kc == 0 and a == 0), kc == KC - 1,
                                [hB, wrB], [lgbb], signal=(a == 3))
            if router is not None:
                lg, lgB = router["lg"]
                tl = (tb % 2) * 4
                self.cp("dve", lg[:, tl:tl + 4, :], lgbk[:, 0:32].rearrange("p (a e) -> p a e", e=8), [lgbb], [lgB])
        return p

    def proj_fm(self, src, srcB, nk, kp, W, wb, wcol, ncols, handler):
        for tb in range(NTB):
            bk, bb = self.allbanks.next()
            for kc in range(nk):
                self.mm(bk[0:ncols, :], W[0:kp, kc, wcol:wcol + ncols], src[0:kp, kc, tb * 512:(tb + 1) * 512], kc == 0, kc == nk - 1,
                        wb + [srcB[kc][tb]], [bb], signal=(kc == nk - 1))
            self.flush_pe_deferred()
            self._pe_deferred = handler(tb, bk, bb)

    def flush_pe_deferred(self):
        d = getattr(self, "_pe_deferred", None)
        self._pe_deferred = None
        if d is not None:
            d()

    def rope_evac(self, tb, bk, bb, n, tmps, then):
        (cbt, cbB), (ubt, ubB) = tmps.next(), tmps.next()
        cs = slice(tb * 512, (tb + 1) * 512)
        self.tt("dve", cbt[0:n, :], bk[0:n, :], self.cosT[0:n, cs], ALU.mult, [bb, self.csB], [cbB])
        self.tt("dve", ubt[0:n, :], bk[0:n, :], self.sinT[0:n, cs], ALU.mult, [bb, self.csB], [ubB])

        def stage2():
            b2, b2B = self.allbanks.next()
            self.mm(b2[0:n, :], self.ident_b[0:n, 0:n], cbt[0:n, :], True, False, [cbB, self.cbfB], [b2B], signal=False)
            self.mm(b2[0:n, :], self.perm_b[0:n, 0:n], ubt[0:n, :], False, True, [ubB, self.cbfB], [b2B])
            then(b2, b2B)
        return stage2

    def layer(self, l):
        S = self.S
        self._pj_pre = [self.pj_issue(l, i) for i in range(3)]
        self.norm_phase(l, 0, list(range(NTB)), 35864)
        self.proj_phase(l)
        S.barrier()
        self._br_pre = {0: self.br_issue(l, 0, [0, 1])}
        self.mla_attn(l)
        S.barrier()
        self._br_pre[1] = self.br_issue(l, 1, [2, 3])
        self.branch_out(l, 0)
        S.barrier()
        self.dil_attn(l)
        S.barrier()
        self._br_pre[2] = self.br_issue(l, 2, [0, 1])
        self.branch_out(l, 1)
        S.barrier()
        self._wout_pre = self.wload([2, 3], self.wout_d[l].rearrange("(k p) n -> p k n", p=128), 128, KC, 1024)
        self.ret_attn(l)
        S.barrier()
        self.branch_out(l, 2)
        S.barrier()
        self.ffn(l)
        S.barrier()

    def pj_blocks(self):
        blocks = []
        blocks.append((C_CQ, 512, "cq", 0)); blocks.append((C_CQ + 512, 256, "cq", 4))
        blocks.append((C_CKV, 512, "ckv", 0))
        blocks.append((C_KR, 64, "kr", 0))
        for g in range(3):
            blocks.append((C_DQ + g * 512, 512, "dq", g))
        for g in range(3):
            blocks.append((C_DK + g * 512, 512, "dk", g))
        for g in range(3):
            blocks.append((C_DV + g * 512, 512, "dv", g))
        blocks.append((C_RQ, 512, "rq", 0)); blocks.append((C_RK, 512, "rk", 0))
        blocks.append((C_RV, 512, "rv", 0)); blocks.append((C_RV + 512, 512, "rv", 1))
        blocks.append((C_RG, 512, "rg", 0)); blocks.append((C_RG + 512, 512, "rg", 1))
        for b3 in range(3):
            blocks.append((C_GA + b3 * 1024, 512, "gate", (b3, 0))); blocks.append((C_GA + b3 * 1024 + 512, 512, "gate", (b3, 1)))
        return blocks

    def pj_issue(self, l, i):
        c0, n, _, _ = self.pj_blocks()[i]
        winv = self.win_d[l].rearrange("(k p) n -> p k n", p=128)
        return self.wload([i % 4], winv[:, :, c0:c0 + n], 128, KC, n)

    def br_issue(self, l, b, slots):
        kp = 64 if b == 1 else 128
        if b == 0:
            src = self.wbm_d[l].rearrange("(k p) n -> p k n", p=128)
        elif b == 1:
            src = self.wbd_d[l].rearrange("(k p) n -> p k n", p=64)
        else:
            src = self.wbr_d[l].rearrange("(k p) n -> p k n", p=128)
        return self.wload(slots, src, kp, KC, 1024)

    def proj_phase(self, l):
        p = 0
        cq, p = self.ovv(p, 6 * SEQ)
        cq = cq.rearrange("p (k t) -> p k t", k=6)
        ckv, p = self.ovv(p, 4 * SEQ)
        ckv = ckv.rearrange("p (k t) -> p k t", k=4)
        cqB = [[Buf() for _ in range(NTB)] for _ in range(6)]
        ckvB = [[Buf() for _ in range(NTB)] for _ in range(4)]
        stg = []
        for i in range(3):
            v, p = self.ovv(p, SEQ)
            stg.append((v, Buf()))
        stgr = Ring(stg)
        rtm = []
        for i in range(6):
            v, p = self.ovv(p, 512)
            rtm.append((v, Buf()))
        rtmr = Ring(rtm)
        stv = []
        for i in range(3):
            v, p = self.ovv(p, 520)
            stv.append((v, Buf()))
            self.memset("dve", v, 1.0, [stv[-1][1]])
        stvr = Ring(stv)
        stw = []
        for i in range(3):
            v, p = self.ovv(p, 512)
            stw.append((v, Buf()))
        stwr = Ring(stw)
        sq = []
        for i in range(2):
            v, p = self.ovv(p, 512)
            sq.append((v, Buf()))
        sqr = Ring(sq)
        rt = []
        for i in range(2):
            v, p = self.ovv(p, 512, F32)
            rt.append((v, Buf()))
        rtr = Ring(rt)
        RA, RAb = self.RA, self.RAb
        winv = self.win_d[l].rearrange("(k p) n -> p k n", p=128)

        blocks = []
        blocks.append((C_CQ, 512, "cq", 0)); blocks.append((C_CQ + 512, 256, "cq", 4))
        blocks.append((C_CKV, 512, "ckv", 0))
        blocks.append((C_KR, 64, "kr", 0))
        for g in range(3):
            blocks.append((C_DQ + g * 512, 512, "dq", g))
        for g in range(3):
            blocks.append((C_DK + g * 512, 512, "dk", g))
        for g in range(3):
            blocks.append((C_DV + g * 512, 512, "dv", g))
        blocks.append((C_RQ, 512, "rq", 0)); blocks.append((C_RK, 512, "rk", 0))
        blocks.append((C_RV, 512, "rv", 0)); blocks.append((C_RV + 512, 512, "rv", 1))
        blocks.append((C_RG, 512, "rg", 0)); blocks.append((C_RG + 512, 512, "rg", 1))
        for b3 in range(3):
            blocks.append((C_GA + b3 * 1024, 512, "gate", (b3, 0))); blocks.append((C_GA + b3 * 1024 + 512, 512, "gate", (b3, 1)))
        nb = len(blocks)
        pend = []

        def issue(i):
            c0, n, _, _ = blocks[i]
            return self.wload([i % 4], winv[:, :, c0:c0 + n], 128, KC, n)
        pend.extend(self._pj_pre)

        def fm_store_handler(dram_rows_ap, dramB, n, func=AF.Copy, scale=None):
            st_, stB = stgr.next()

            def h(tb, bk, bb):
                self.act(st_[0:n, tb * 512:(tb + 1) * 512], bk[0:n, :], func, [bb], [stB], scale=scale)
                if tb == NTB - 1:
                    self.dma("sp", dram_rows_ap, st_[0:n, :], [stB], [dramB])
            return h

        def rope_store_handler(dram_rows_ap, dramB, n, d, scale=None, sbuf_dest=None, sbufB=None):
            if sbuf_dest is None:
                st_, stB = stgr.next()
            else:
                st_, stB = sbuf_dest, None

            def h(tb, bk, bb):
                def then(b2, b2B):
                    if d == 1:
                        dst = st_[0:n, tb * 512:(tb + 1) * 512]
                        src = b2[0:n, :]
                    else:
                        w = 512 // d
                        dst = st_[0:n, :].rearrange("p (r l) -> p r l", r=d)[:, :, tb * w:(tb + 1) * w]
                        src = b2[0:n, :].rearrange("p (j r) -> p r j", r=d)
                    wB = [stB] if sbuf_dest is None else [sbufB[tb]]
                    self.act(dst, src, AF.Copy, [b2B], wB, scale=scale)
                    if sbuf_dest is None and tb == NTB - 1:
                        self.dma("sp", dram_rows_ap, st_[0:n, :], [stB], [dramB])
                return self.rope_evac(tb, bk, bb, n, rtmr, then)
            return h

        for bi_, (c0, n, kind, info) in enumerate(blocks):
            W, wb = pend.pop(0)
            if bi_ + 3 < nb:
                pend.append(issue(bi_ + 3))
            if kind in ("cq", "ckv"):
                dstt, dB = (cq, cqB) if kind == "cq" else (ckv, ckvB)
                for j in range(n // 128):
                    c = info + j

                    def h(tb, bk, bb, c=c, dstt=dstt, dB=dB):
                        self.cp("act", dstt[:, c, tb * 512:(tb + 1) * 512], bk[:, :], [bb], [dB[c][tb]])
                    self.proj_fm(RA, RAb, KC, 128, W, wb, j * 128, 128, h)
            elif kind == "kr":
                self.proj_fm(RA, RAb, KC, 128, W, wb, 0, 64, rope_store_handler(None, None, 64, 1, sbuf_dest=self.krT, sbufB=self.krB))
            elif kind in ("dq", "dk"):
                g = info
                dr, dB = (self.dqT, self.dqTB) if kind == "dq" else (self.dkT, self.dkTB)
                for j in range(4):
                    self.proj_fm(RA, RAb, KC, 128, W, wb, j * 128, 128,
                                 rope_store_handler(dr[g, j * 128:(j + 1) * 128, :], dB[g][j], 128, DIL_D[g]))
            elif kind in ("rq", "rk"):
                dr, dB = (self.rqT, self.rqTB) if kind == "rq" else (self.rkT, self.rkTB)
                for j in range(4):
                    self.proj_fm(RA, RAb, KC, 128, W, wb, j * 128, 128,
                                 rope_store_handler(dr[j * 128:(j + 1) * 128, :], dB[j], 128, 1, scale=(0.125 if kind == "rk" else None)))
            elif kind == "rg":
                for j in range(4):
                    o = info * 4 + j
                    self.proj_fm(RA, RAb, KC, 128, W, wb, j * 128, 128, fm_store_handler(self.rgT[o * 128:(o + 1) * 128, :], self.rgTB[o], 128, AF.Silu))
            elif kind == "gate":
                b3, hf = info
                for j in range(4):
                    o = hf * 4 + j
                    self.proj_fm(RA, RAb, KC, 128, W, wb, j * 128, 128,
                                 fm_store_handler(self.gT[b3, o * 128:(o + 1) * 128, :], self.gTB[b3][o], 128, AF.Sigmoid))
            elif kind == "dv":
                self.flush_pe_deferred()
                g = info
                d = DIL_D[g]
                Lr = SEQ // d
                for tt_ in range(NTT):
                    r = (128 * tt_) // Lr
                    j0 = (128 * tt_) % Lr
                    t0 = r + d * j0
                    tsl = slice(t0, t0 + d * 127 + 1, d)
                    tbs = sorted(set([t0 // 512, (t0 + d * 127) // 512])) if d < 16 else list(range(NTB))
                    bk, bb = self.allbanks.next()
                    for kc in range(KC):
                        self.mm(bk[:, :], RA[:, kc, tsl], W[:, kc, :], kc == 0, kc == KC - 1, wb + [RAb[kc][t] for t in tbs], [bb],
                                signal=(kc == KC - 1))
                    sv, svB = stvr.next()
                    self.cp("act" if tt_ % 2 else "dve", sv.rearrange("p (h c) -> p h c", c=65)[:, :, 0:64],
                            bk[:, :].rearrange("p (h c) -> p h c", c=64), [bb], [svB])
                    self.dma("sp", self.dv[g, tt_ * 128:(tt_ + 1) * 128, :], sv, [svB], [self.dvB[g]])
            elif kind == "rv":
                self.flush_pe_deferred()
                hf = info
                for tt_ in range(NTT):
                    tb = tt_ // 4
                    bk, bb = self.allbanks.next()
                    for kc in range(KC):
                        self.mm(bk[:, :], RA[:, kc, tt_ * 128:(tt_ + 1) * 128], W[:, kc, :], kc == 0, kc == KC - 1, wb + [RAb[kc][tb]], [bb],
                                signal=(kc == KC - 1))
                    sw, swB = stwr.next()
                    self.cp("act" if tt_ % 2 else "dve", sw, bk[:, :], [bb], [swB])
                    self.dma("sp", self.rv[tt_ * 128:(tt_ + 1) * 128, hf * 512:(hf + 1) * 512], sw, [swB], [self.rvB[hf]])

        self.flush_pe_deferred()

        def rmsn(src, srcB, nk, gcol0, inv_n):
            for tb in range(NTB):
                bk, bb = self.allbanks.next()
                for c in range(nk):
                    s, sB = sqr.next()
                    self.act(s, src[:, c, tb * 512:(tb + 1) * 512], AF.Square, [srcB[c][tb]], [sB])
                    self.mm(bk[:, :], self.ones_b, s, c == 0, c == nk - 1, [sB, self.cbfB], [bb], signal=True)
                r, rB = rtr.next()
                self.act(r, bk[:, :], AF.Sqrt, [bb, self.cstB], [rB], scale=inv_n, bias=self.eps_ap)
                self.recip(r, r, [rB], [rB])
                for c in range(nk):
                    v = src[:, c, tb * 512:(tb + 1) * 512]
                    self.stt("dve", v, v, self.vecT[:, l, gcol0 + c:gcol0 + c + 1], r, ALU.mult, ALU.mult, [srcB[c][tb], rB, self.vecB],
                             [srcB[c][tb]])
        rmsn(cq, cqB, 6, 64, 1.0 / 768)
        rmsn(ckv, ckvB, 4, 70, 1.0 / 512)

        wuqv = self.wuq_d[l].rearrange("(k p) n -> p k n", p=128)
        wukvv = self.wukv_d[l].rearrange("(k p) n -> p k n", p=128)
        jobs = []
        for hp in range(4):
            jobs.append(("q", hp))
            jobs.append(("kv", hp))
        pend = []

        def issue2(i):
            kind, hp = jobs[i]
            if kind == "q":
                return self.wload([i % 4], wuqv[:, :, hp * 384:(hp + 1) * 384], 128, 6, 384)
            return self.wload([i % 4], wukvv[:, :, hp * 512:(hp + 1) * 512], 128, 4, 512)
        for i in range(3):
            pend.append(issue2(i))
        for i, (kind, hp) in enumerate(jobs):
            W, wb = pend.pop(0)
            if i + 3 < len(jobs):
                pend.append(issue2(i + 3))
            for hh in range(2):
                h_ = 2 * hp + hh
                if kind == "q":
                    self.proj_fm(cq, cqB, 6, 128, W, wb, hh * 192, 128, fm_store_handler(self.qT[h_, 0:128, :], self.qTB[h_], 128))
                    self.proj_fm(cq, cqB, 6, 128, W, wb, hh * 192 + 128, 64, rope_store_handler(self.qT[h_, 128:192, :], self.qTB[h_], 64, 1))
                else:
                    self.proj_fm(ckv, ckvB, 4, 128, W, wb, hh * 256, 128, fm_store_handler(self.knT[h_], self.knTB[h_], 128))
            self.flush_pe_deferred()
            if kind == "kv":
                Wv = W.rearrange("p k (h c) -> p k h c", c=256)[:, :, :, 128:256]
                for tt_ in range(NTT):
                    tb = tt_ // 4
                    bk, bb = self.allbanks.next()
                    for c in range(4):
                        self.mm(bk[:, 0:256].rearrange("p (h c) -> p h c", c=128), ckv[:, c, tt_ * 128:(tt_ + 1) * 128], Wv[:, c, :, :],
                                c == 0, c == 3, wb + [ckvB[c][tb]], [bb], signal=(c == 3))
                    sw, swB = stwr.next()
                    self.cp("act" if tt_ % 2 else "dve", sw[:, 0:256], bk[:, 0:256], [bb], [swB])
                    self.dma("sp", self.vm[tt_ * 128:(tt_ + 1) * 128, hp * 256:(hp + 1) * 256], sw[:, 0:256], [swB], [self.vmB[hp // 2]])

    def mla_attn(self, l):
        p = 0
        Ld = []
        for i in range(2):
            d_ = {}
            for nm, n in (("qn", SEQ), ("qr", SEQ), ("kn", SEQ), ("vh", NTT * 128)):
                v, p = self.ovv(p, n)
                d_[nm] = (v, Buf())
            Ld.append(d_)
        Et = []
        for i in range(6):
            v, p = self.ovv(p, 512)
            Et.append((v, Buf()))
        Er = Ring(Et)
        rcs = []
        for i in range(2):
            v, p = self.ovv(p, 512, F32)
            rcs.append((v, Buf()))
        rcr = Ring(rcs)
        ess = []
        for i in range(4):
            v, p = self.ovv(p, 512, F32)
            ess.append((v, Buf()))
        esr = Ring(ess)
        Sr = Ring([(self.pb[i], self.PB[i]) for i in (0, 1, 2)])
        Or = Ring([(self.pb[i], self.PB[i]) for i in (3, 4)])
        Dr = Ring([(self.pb[i], self.PB[i]) for i in (5, 6)])
        vmv = self.vm.rearrange("(t p) f -> p t f", p=128)
        scale = float(192 ** -0.5)

        def loads(h):
            d_ = Ld[h % 2]
            self.dma("sp", d_["qn"][0], self.qT[h, 0:128, :], [self.qTB[h]], [d_["qn"][1]])
            self.dma("sp", d_["qr"][0][0:64, :], self.qT[h, 128:192, :], [self.qTB[h]], [d_["qr"][1]])
            self.dma("sp", d_["kn"][0], self.knT[h], [self.knTB[h]], [d_["kn"][1]])
            self.dma("sp", d_["vh"][0].rearrange("p (t f) -> p t f", f=128), vmv[:, :, h * 128:(h + 1) * 128], [self.vmB[h // 4]], [d_["vh"][1]])
        loads(0)
        for h in range(8):
            if h + 1 < 8:
                loads(h + 1)
            d_ = Ld[h % 2]
            qn, qnB = d_["qn"]; qr, qrB = d_["qr"]; kn, knB = d_["kn"]; vh, vhB = d_["vh"]
            vh3 = vh.rearrange("p (t f) -> p t f", f=128)
            for qb in range(NTB):
                qs = slice(qb * 512, (qb + 1) * 512)
                ob, obB = Or.next()
                db, dbB = Dr.next()

                def smm(kt):
                    sb_, sbB = Sr.next()
                    ks = slice(kt * 128, (kt + 1) * 128)
                    self.mm(sb_[:, :], kn[:, ks], qn[:, qs], True, False, [knB, qnB], [sbB], signal=False)
                    self.mm(sb_[:, :], self.krT[0:64, ks], qr[0:64, qs], False, True, [self.krB[kt // 4], qrB], [sbB])
                    e, eB = Er.next()
                    self.act(e, sb_[:, :], AF.Exp, [sbB], [eB], scale=scale)
                    return e, eB
                esA, esAB = esr.next()
                esBt, esBB = esr.next()
                cur, nxt = smm(0), smm(1)
                for kt in range(NTT):
                    nn = smm(kt + 2) if kt + 2 < NTT else None
                    e, eB = cur
                    self.mm(ob[:, :], vh3[:, kt, :], e, kt == 0, kt == NTT - 1, [vhB, eB], [obB], signal=True)
                    if kt == 0:
                        self.cp("dve", esA, e, [eB], [esAB])
                    elif kt == 1:
                        self.cp("pool", esBt, e, [eB], [esBB])
                    elif kt % 2 == 0:
                        self.tt("dve", esA, esA, e, ALU.add, [eB, esAB], [esAB])
                    else:
                        self.tt("pool", esBt, esBt, e, ALU.add, [eB, esBB], [esBB])
                    cur, nxt = nxt, nn
                self.mm(db[:, :], self.onesf[:], esA, True, False, [esAB, self.onesfB], [dbB], signal=False)
                self.mm(db[:, :], self.onesf[:], esBt, False, True, [esBB, self.onesfB], [dbB])
                r, rB = rcr.next()
                self.recip(r, db[:, :], [dbB], [rB])
                self.tt("dve", self.RA[:, h, qs], ob[:, :], r, ALU.mult, [obB, rB], [self.RAb[h][qb]])
        self.dbg_dump_y(0)

    def dbg_dump_y(self, b, kp=128):
        if self.ydbg is None:
            return
        for k in range(KC):
            self.dma("sp", self.ydbg[b, k * 128:k * 128 + kp, :], self.RA[0:kp, k, :], [self.RAb[k][t] for t in range(NTB)], [self.dbgB])

    def branch_out(self, l, b):
        kp = 64 if b == 1 else 128
        if b == 0:
            src = self.wbm_d[l].rearrange("(k p) n -> p k n", p=128)
        elif b == 1:
            src = self.wbd_d[l].rearrange("(k p) n -> p k n", p=64)
        else:
            src = self.wbr_d[l].rearrange("(k p) n -> p k n", p=128)
        Wb, wbb = self._br_pre[b]
        last = (b == 2)
        if last:
            Wo, wob = self._wout_pre
        p = 0
        xi = []
        for i in range(2):
            v, p = self.ovv(p, KC * 512, F32)
            xi.append((v.rearrange("p (k t) -> p k t", k=KC), Buf()))
        xir = Ring(xi)
        mi = []
        for i in range(2):
            v, p = self.ovv(p, KC * 512, F32)
            mi.append((v.rearrange("p (k t) -> p k t", k=KC), Buf()))
        mir = Ring(mi)
        gts = []
        for i in range(2):
            v, p = self.ovv(p, KC * 512)
            gts.append((v.rearrange("p (k t) -> p k t", k=KC), Buf()))
        gtr = Ring(gts)
        ms = []
        for i in range(2):
            v, p = self.ovv(p, KC * 512)
            ms.append((v.rearrange("p (k t) -> p k t", k=KC), Buf()))
        msr = Ring(ms)
        tps = []
        for i in range(2):
            v, p = self.ovv(p, 512, F32)
            tps.append((v, Buf()))
        tpr = Ring(tps)
        xsv = self.xs.rearrange("k p t -> p k t")
        mgv = self.mg.rearrange("k p t -> p k t")
        gv = self.gT[b].rearrange("(o p) t -> p o t", p=128)
        gate1 = self.modT[:, l, 16:24]
        pre = {}

        def load(tb):
            ts_ = slice(tb * 512, (tb + 1) * 512)
            g, gB = gtr.next()
            self.dma("sp", g, gv[:, :, ts_], self.gTB[b], [gB])
            mgt, mgB = mir.next()
            if b > 0:
                self.dma("sp", mgt, mgv[:, :, ts_], [self.mgB[tb]], [mgB])
            x, xB = (None, None)
            if last:
                x, xB = xir.next()
                self.dma("sp", x, xsv[:, :, ts_], [self.xsB[tb]], [xB])
            pre[tb] = (x, xB, g, gB, mgt, mgB)
        load(0)
        for tb in range(NTB):
            if tb + 1 < NTB:
                load(tb + 1)
            x, xB, g, gB, mgt, mgB = pre.pop(tb)
            ts_ = slice(tb * 512, (tb + 1) * 512)
            if last:
                m, mB = msr.next()
            for o in range(KC):
                bk, bb = self.allbanks.next()
                for kc in range(KC):
                    self.mm(bk[:, :], Wb[0:kp, kc, o * 128:(o + 1) * 128], self.RA[0:kp, kc, ts_], kc == 0, kc == KC - 1,
                            wbb + [self.RAb[kc][tb]], [bb], signal=(kc == KC - 1))
                if b == 0:
                    self.tt("dve", mgt[:, o, :], bk[:, :], g[:, o, :], ALU.mult, [bb, gB], [mgB])
                else:
                    t_, tB = tpr.next()
                    self.tt("dve", t_, bk[:, :], g[:, o, :], ALU.mult, [bb, gB], [tB])
                    if last:
                        self.tt("pool", m[:, o, :], t_, mgt[:, o, :], ALU.add, [tB, mgB], [mB])
                    else:
                        self.tt("pool", mgt[:, o, :], t_, mgt[:, o, :], ALU.add, [tB, mgB], [mgB])
            if not last:
                self.dma("sp", mgv[:, :, ts_], mgt, [mgB], [self.mgB[tb]])
                continue
            for o2 in range(KC):
                bk, bb = self.allbanks.next()
                for o in range(KC):
                    self.mm(bk[:, :], Wo[:, o, o2 * 128:(o2 + 1) * 128], m[:, o, :], o == 0, o == KC - 1, wob + [mB], [bb], signal=(o == KC - 1))
                self.stt("dve", x[:, o2, :], bk[:, :], gate1[:, o2:o2 + 1], x[:, o2, :], ALU.mult, ALU.add, [bb, xB, self.modB], [xB])
            self.dma("sp", xsv[:, :, ts_], x, [xB], [self.xsB[tb]])

    def dil_attn(self, l):
        p = 0
        va = []
        for g in range(3):
            v, p = self.ovv(p, NTT * 520)
            va.append((v.rearrange("p (t f) -> p t f", f=520), Buf()))
            self.dma("sp", va[g][0], self.dv[g].rearrange("(t p) f -> p t f", p=128), [self.dvB[g]], [va[g][1]])
        qk = []
        for g in range(3):
            q_, p = self.ovv(p, SEQ)
            k_, p = self.ovv(p, SEQ)
            qk.append((q_, Buf(), k_, Buf()))
        accs = []
        for i in range(2):
            v, p = self.ovv(p, SEQ, F32)
            accs.append((v, Buf()))
        Et = []
        for i in range(4):
            v, p = self.ovv(p, 384)
            Et.append((v, Buf()))
        Er = Ring(Et)
        rrv, p = self.ovv(p, SEQ, F32)
        rrB = Buf()
        bcs = []
        for i in range(2):
            v, p = self.ovv(p, 512, F32)
            bcs.append((v, Buf()))
        bcr = Ring(bcs)
        Sr = Ring([(self.pb[i], self.PB[i]) for i in (0, 1, 2)])
        obs = [(self.pb[i], self.PB[i]) for i in (3, 4, 5, 6)]
        Br = Ring([(self.pb[i], self.PB[i]) for i in (7,)])

        def loads(hp):
            for g in range(3):
                q_, qB, k_, kB = qk[g]
                self.dma("sp", q_, self.dqT[g, hp * 128:(hp + 1) * 128, :], [self.dqTB[g][hp]], [qB])
                self.dma("sp", k_, self.dkT[g, hp * 128:(hp + 1) * 128, :], [self.dkTB[g][hp]], [kB])
        for hp in range(4):
            loads(hp)
            for hh in range(2):
                h = 2 * hp + hh
                rows = slice(64 * hh, 64 * hh + 64)
                acc, accB = accs[hh]
                for g in range(3):
                    d = DIL_D[g]
                    TPR = (SEQ // d) // 128
                    q_, qB, k_, kB = qk[g]
                    vg, vgB = va[g]

                    def qblocks(n):
                        return [i for i in (n - 1, n, n + 1) if 0 <= i < NTT and i // TPR == n // TPR]

                    def s_stage(n):
                        qb_ = qblocks(n)
                        qlo, qhi = qb_[0] * 128, (qb_[-1] + 1) * 128
                        W = qhi - qlo
                        m0 = (qb_[0] - (n - 1)) * 128
                        sb_, sbB = Sr.next()
                        self.mm(sb_[:, 0:W], k_[rows, n * 128:(n + 1) * 128], q_[rows, qlo:qhi], True, True, [kB, qB], [sbB], signal=True)
                        e, eB = Er.next()
                        self.act(e[:, 0:W], sb_[:, 0:W], AF.Exp, [sbB], [eB], scale=0.125)
                        self.tt("dve", e[:, 0:W], e[:, 0:W], self.maskT_b[:, m0:m0 + W], ALU.mult, [eB, self.cbfB], [eB])
                        return (e, eB, qlo, qhi)
                    last_n = [max(n for n in range(NTT) if any(i // 4 == b4 for i in qblocks(n))) for b4 in range(4)]
                    stages = {0: s_stage(0), 1: s_stage(1)}
                    started = [False] * 4
                    for n in range(NTT):
                        if n + 2 < NTT:
                            stages[n + 2] = s_stage(n + 2)
                        e, eB, qlo, qhi = stages.pop(n)
                        c = qlo
                        while c < qhi:
                            b4 = c // 512
                            ce = min(qhi, (b4 + 1) * 512)
                            ob, obB = obs[b4]
                            self.mm(ob[0:65, c - b4 * 512:ce - b4 * 512], vg[:, n, h * 65:(h + 1) * 65], e[:, c - qlo:ce - qlo],
                                    not started[b4], False, [vgB, eB], [obB], signal=True)
                            started[b4] = True
                            c = ce
                        for b4 in range(4):
                            if last_n[b4] != n:
                                continue
                            ob, obB = obs[b4]
                            if g == 0:
                                self.cp("dve", acc[0:65, b4 * 512:(b4 + 1) * 512], ob[0:65, :], [obB], [accB])
                            elif g == 1:
                                dst = acc[0:65, :].rearrange("p (j r) -> p r j", r=4)[:, b4, :]
                                self.tt("dve", dst, dst, ob[0:65, :], ALU.add, [obB, accB], [accB])
                            else:
                                dst = acc[0:65, :].rearrange("p (j r) -> p r j", r=16)[:, 4 * b4:4 * b4 + 4, :]
                                self.tt("dve", dst, dst, ob[0:65, :].rearrange("p (b j) -> p b j", b=4), ALU.add, [obB, accB], [accB])
                self.act(rrv[64:65, :], acc[64:65, :], AF.Ln, [accB], [rrB])
                self.act(rrv[64:65, :], rrv[64:65, :], AF.Exp, [rrB], [rrB], scale=-1.0)
                for tb in range(NTB):
                    ts_ = slice(tb * 512, (tb + 1) * 512)
                    bk, bb = Br.next()
                    self.mm(bk[0:64, :], self.onesf[64:65, 0:64], rrv[64:65, ts_], True, True, [rrB, self.onesfB], [bb])
                    bc, bcB = bcr.next()
                    self.cp("act", bc[0:64, :], bk[0:64, :], [bb], [bcB])
                    self.tt("dve", self.RA[0:64, h, ts_], acc[0:64, ts_], bc[0:64, :], ALU.mult, [accB, bcB], [self.RAb[h][tb]])
        self.dbg_dump_y(1, 64)

    def ret_attn(self, l):
        p = 0
        dpos, p = self.ovv(p, STRIPW, F32)
        dneg, p = self.ovv(p, STRIPW, F32)
        dB = Buf()
        self.dma("sp", dpos, self.dpn_d[0], [], [dB])
        self.dma("sp", dneg, self.dpn_d[1], [], [dB])
        HW_ = STRIPW // 2
        ef, p = self.ovv(p, HW_)
        eb, p = self.ovv(p, HW_)
        efB, ebB = Buf(), Buf()
        strips = []
        for i in range(2):
            v, p = self.ovv(p, STRIPW)
            strips.append((v, Buf()))
        qk = []
        for i in range(2):
            q_, p = self.ovv(p, SEQ)
            k_, p = self.ovv(p, SEQ)
            qk.append((q_, Buf(), k_, Buf()))
        hv = []
        for i in range(2):
            v_, p = self.ovv(p, NTT * 128)
            g_, p = self.ovv(p, SEQ)
            hv.append((v_.rearrange("p (t f) -> p t f", f=128), Buf(), g_, Buf()))
        SD = []
        for i in range(6):
            v, p = self.ovv(p, 512)
            SD.append((v, Buf()))
        SDr = Ring(SD)
        SC = []
        for i in range(3):
            v, p = self.ovv(p, 512)
            SC.append((v, Buf()))
        SCr = Ring(SC)
        ysbs, sqs = [], []
        for i in range(2):
            v, p = self.ovv(p, 512, F32)
            ysbs.append((v, Buf()))
            v, p = self.ovv(p, 512, F32)
            sqs.append((v, Buf()))
        ysbr, sqr_ = Ring(ysbs), Ring(sqs)
        tmp = {}
        for nm in ("msq", "var", "rstd"):
            v, p = self.ovv(p, 512, F32)
            tmp[nm] = (v, Buf())
        Sr = Ring([(self.pb[i], self.PB[i]) for i in (0, 1, 2)])
        Yr = Ring([(self.pb[i], self.PB[i]) for i in (3, 4)])
        Mr = Ring([(self.pb[i], self.PB[i]) for i in (5,)])
        Vb, VbB = self.pb[6], self.PB[6]
        dmy, dmyB = self.pb[7], self.PB[7]
        rvv = self.rv.rearrange("(t p) f -> p t f", p=128)

        def loads_pair(hp):
            q_, qB, k_, kB = qk[hp % 2]
            self.dma("sp", q_, self.rqT[hp * 128:(hp + 1) * 128, :], [self.rqTB[hp]], [qB])
            self.dma("sp", k_, self.rkT[hp * 128:(hp + 1) * 128, :], [self.rkTB[hp]], [kB])

        def loads_head(h):
            v_, vB, g_, gB = hv[h % 2]
            self.dma("sp", v_, rvv[:, :, h * 128:(h + 1) * 128], [self.rvB[h // 4]], [vB])
            self.dma("sp", g_, self.rgT[h * 128:(h + 1) * 128, :], [self.rgTB[h]], [gB])

        def strip_steps(h):
            strip, stripB = strips[h % 2]
            lgf = self.rldT[:, l * 16 + h:l * 16 + h + 1]
            nlgb = self.rldT[:, l * 16 + 8 + h:l * 16 + 8 + h + 1]
            steps = []
            for half in range(2):
                cs = slice(half * HW_, (half + 1) * HW_)
                steps.append(lambda cs=cs: self.act(ef, dpos[:, cs], AF.Exp, [dB, self.rldB], [efB], scale=lgf))
                steps.append(lambda cs=cs: self.act(eb, dneg[:, cs], AF.Exp, [dB, self.rldB], [ebB], scale=nlgb))
                steps.append(lambda cs=cs: self.tt("dve", strip[:, cs], ef, eb, ALU.mult, [efB, ebB], [stripB]))
            return steps

        def stats_steps(h, qb, yb, ybB, g_, gB):
            qs = slice(qb * 512, (qb + 1) * 512)
            ysb, ysbB = ysbr.next()
            sq, sqB = sqr_.next()
            msq, msqB = tmp["msq"]; var, varB = tmp["var"]; rstd, rstdB = tmp["rstd"]
            st = {}

            def s3():
                st["mb"], st["mbB"] = Mr.next()
                self.mm(st["mb"][:, :], self.avgf[:], ysb, True, True, [ysbB, self.onesfB], [st["mbB"]])
                self.mm(Vb[:, :], self.avgf[:], sq, True, True, [sqB, self.onesfB], [VbB])
            return [
                lambda: self.cp("act", ysb, yb[:, :], [ybB], [ysbB]),
                lambda: self.act(sq, yb[:, :], AF.Square, [ybB], [sqB]),
                s3,
                lambda: self.act(msq, st["mb"][:, :], AF.Square, [st["mbB"]], [msqB]),
                lambda: self.tt("dve", var, Vb[:, :], msq, ALU.subtract, [VbB, msqB], [varB]),
                lambda: self.ts("dve", var, var, 0.0, EPS, ALU.max, ALU.add, [varB], [varB]),
                lambda: self.act(rstd, var, AF.Ln, [varB], [rstdB]),
                lambda: self.act(rstd, rstd, AF.Exp, [rstdB], [rstdB], scale=-0.5),
                lambda: self.tt("dve", ysb, ysb, st["mb"][:, :], ALU.subtract, [ysbB, st["mbB"]], [ysbB]),
                lambda: self.tt("pool", ysb, ysb, rstd, ALU.mult, [ysbB, rstdB], [ysbB]),
                lambda: self.stt("dve", self.RA[:, h, qs], ysb, self.vecT[:, l, 74 + h:75 + h], g_[:, qs], ALU.mult, ALU.mult,
                                 [ysbB, self.vecB, gB], [self.RAb[h][qb]]),
            ]
        loads_pair(0)
        loads_head(0)
        for s in strip_steps(0):
            s()
        pending = []
        for h in range(8):
            hp, hh = h // 2, h % 2
            if hh == 0 and hp + 1 < 4:
                loads_pair(hp + 1)
            rows = slice(64 * hh, 64 * hh + 64)
            q_, qB, k_, kB = qk[hp % 2]
            v_, vB, g_, gB = hv[h % 2]
            strip, stripB = strips[h % 2]
            for qb in range(NTB):
                qs = slice(qb * 512, (qb + 1) * 512)
                yb, ybB = Yr.next()
                if qb == 1 and h + 1 < 8:
                    loads_head(h + 1)
                    pending.extend(strip_steps(h + 1))

                def smm(kt):
                    sb_, sbB = Sr.next()
                    self.mm(sb_[:, :], k_[rows, kt * 128:(kt + 1) * 128], q_[rows, qs], True, True, [kB, qB], [sbB])
                    sd, sdB = SDr.next()
                    off = 512 * qb - 128 * kt + 1920
                    if kt % 2 == 0:
                        self.tt("dve", sd, sb_[:, :], strip[:, off:off + 512], ALU.mult, [sbB, stripB], [sdB])
                    else:
                        sc, scB = SCr.next()
                        self.cp("act", sc, sb_[:, :], [sbB], [scB])
                        self.tt("dve", sd, sc, strip[:, off:off + 512], ALU.mult, [scB, stripB], [sdB])
                    return sd, sdB
                cur, nxt = smm(0), smm(1)
                for kt in range(NTT):
                    nn = smm(kt + 2) if kt + 2 < NTT else None
                    sd, sdB = cur
                    self.mm(yb[:, :], v_[:, kt, :], sd, kt == 0, kt == NTT - 1, [vB, sdB], [ybB], signal=True)
                    cur, nxt = nxt, nn
                    if pending:
                        pending.pop(0)()
                while pending:
                    pending.pop(0)()
                pending.extend(stats_steps(h, qb, yb, ybB, g_, gB))
        while pending:
            pending.pop(0)()
        self.dbg_dump_y(2)

    def ffn(self, l):
        moe = (l % 2 == 1)
        li = l // 2
        NF = (D_FFE if moe else D_FF) // 128
        nexp = NEXP if moe else 1
        p0 = 0
        gTt, p0 = self.ovv(p0, NF * 1024)
        gTt = gTt.rearrange("p (f t) -> p f t", t=1024)
        gB = [[Buf() for _ in range(2)] for _ in range(NF)]
        facc, p0 = self.ovv(p0, KC * 1024, F32)
        facc = facc.rearrange("p (k t) -> p k t", t=1024)
        faccB = [[Buf() for _ in range(2)] for _ in range(KC)]
        gate2 = self.modT[:, l, 40:48]
        router = None
        if moe:
            wr, p0 = self.ovv(p0, KC * 8, F32)
            wr = wr.rearrange("p (k e) -> p k e", e=8)
            wrB = Buf()
            self.dma("sp", wr, self.mr_d[li].rearrange("(k p) e -> p k e", p=128), [], [wrB])
            lg, p0 = self.ovv(p0, 64, F32)
            lg = lg.rearrange("p (a e) -> p a e", e=8)
            lgB = Buf()
            router = {"w": (wr, wrB), "lg": (lg, lgB)}
            small = {}
            for nm, n in (("m1", 8), ("m2", 8), ("eq1", 64), ("eq2", 64), ("lg2", 64), ("dm", 8), ("w1", 8), ("w2", 8), ("wts", 64)):
                v, p0 = self.ovv(p0, n, F32)
                small[nm] = v
            smB = Buf()
            dg, p0 = self.ovv(p0, 128, F32)
            dgs = [(dg, Buf())]
            v, p0 = self.ovv(p0, 128, F32)
            dgs.append((v, Buf()))
            dgr = Ring(dgs)
            wbt, p0 = self.ovv(p0, NEXP * 1024)
            wbt = wbt.rearrange("p (e t) -> p e t", t=1024)
            wbB = [Buf() for _ in range(NEXP)]
        tms = []
        for i in range(2):
            s_, p0 = self.ovv(p0, 512, F32)
            g0, p0 = self.ovv(p0, 512)
            tms.append((s_, Buf(), g0, Buf()))
        tmr = Ring(tms)
        pn = 0

        steps = [(f0, min(4, NF - f0)) for f0 in range(0, NF, 4)]

        def wviews(ex):
            if moe:
                return (self.m1_d[li, ex].rearrange("(k p) n -> p k n", p=128), self.m3_d[li, ex].rearrange("(k p) n -> p k n", p=128),
                        self.m2_d[li, ex].rearrange("(f p) n -> p f n", p=128))
            return (self.f1_d[li].rearrange("(k p) n -> p k n", p=128), self.f3_d[li].rearrange("(k p) n -> p k n", p=128),
                    self.f2_d[li].rearrange("(f p) n -> p f n", p=128))
        jobs = []
        jidx = {}
        for ex in range(nexp):
            w1v, w3v, w2v = wviews(ex)
            for i, (f0, nf) in enumerate(steps):
                jidx[("f1", ex, i)] = len(jobs)
                jobs.append([(w1v[:, :, f0 * 128:(f0 + nf) * 128], KC, nf * 128), (w3v[:, :, f0 * 128:(f0 + nf) * 128], KC, nf * 128)])
            for o in range(KC):
                jidx[("f2", ex, o)] = len(jobs)
                jobs.append([(w2v[:, :, o * 128:(o + 1) * 128], NF, 128)])

        for hf in range(2):
            tbs = [2 * hf, 2 * hf + 1]
            ws = WStream(self, jobs)
            ws._try_issue()
            self.norm_phase(l, 1, tbs, pn, router=router)
            if moe:
                lg3 = lg
                m1, m2, eq1, eq2, lg2, dm, w1, w2, wts = (small[k] for k in ("m1", "m2", "eq1", "eq2", "lg2", "dm", "w1", "w2", "wts"))
                e3 = lambda v: v.rearrange("p (a e) -> p a e", e=8)
                b3 = lambda v: v.unsqueeze(2).to_broadcast([128, 8, 8])
                self.S.op("dve", lambda e: e.tensor_reduce(out=m1, in_=lg3, axis=mybir.AxisListType.X, op=ALU.max), [lgB], [smB])
                self.tt("dve", e3(eq1), lg3, b3(m1), ALU.is_ge, [lgB, smB], [smB])
                self.stt("dve", e3(lg2), e3(eq1), -1e30, lg3, ALU.mult, ALU.add, [smB, lgB], [smB])
                self.S.op("dve", lambda e: e.tensor_reduce(out=m2, in_=e3(lg2), axis=mybir.AxisListType.X, op=ALU.max), [smB], [smB])
                self.tt("dve", e3(eq2), e3(lg2), b3(m2), ALU.is_ge, [smB], [smB])
                self.tt("dve", dm, m1, m2, ALU.subtract, [smB], [smB])
                self.act(w1, dm, AF.Sigmoid, [smB], [smB])
                self.act(w2, dm, AF.Sigmoid, [smB], [smB], scale=-1.0)
                self.tt("dve", e3(eq1), e3(eq1), b3(w1), ALU.mult, [smB], [smB])
                self.tt("dve", e3(eq2), e3(eq2), b3(w2), ALU.mult, [smB], [smB])
                self.tt("dve", e3(wts), e3(eq1), e3(eq2), ALU.add, [smB], [smB])
                for ex in range(NEXP):
                    for a4 in range(2):
                        bk, bb = self.allbanks.next()
                        for a in range(4):
                            ta = a4 * 4 + a
                            dgt, dgB = dgr.next()
                            self.ts("dve", dgt, self.ident_f, e3(wts)[:, ta, ex:ex + 1], None, ALU.mult, None, [smB, self.cstB], [dgB])
                            self.mm(bk[:, a * 128:(a + 1) * 128], self.onesf[:], dgt, True, True, [dgB, self.onesfB], [bb])
                        self.cp("act", wbt[:, ex, a4 * 512:(a4 + 1) * 512], bk[:, :], [bb], [wbB[ex]])
            self.S.barrier()
            for ex in range(nexp):
                if moe:
                    w1v = self.m1_d[li, ex].rearrange("(k p) n -> p k n", p=128)
                    w3v = self.m3_d[li, ex].rearrange("(k p) n -> p k n", p=128)
                    w2v = self.m2_d[li, ex].rearrange("(f p) n -> p f n", p=128)
                else:
                    w1v = self.f1_d[li].rearrange("(k p) n -> p k n", p=128)
                    w3v = self.f3_d[li].rearrange("(k p) n -> p k n", p=128)
                    w2v = self.f2_d[li].rearrange("(f p) n -> p f n", p=128)
                for i, (f0, nf) in enumerate(steps):
                    jn = jidx[("f1", ex, i)]
                    (W1, w1b), (W3, w3b) = ws.get(jn)
                    for j in range(nf):
                        f = f0 + j
                        for t2 in range(2):
                            tb = tbs[t2]
                            ts_ = slice(tb * 512, (tb + 1) * 512)
                            b1, b1B = self.allbanks.next()
                            b3_, b3B = self.allbanks.next()
                            for kc in range(KC):
                                self.mm(b1[:, :], W1[:, kc, j * 128:(j + 1) * 128], self.RA[:, kc, ts_], kc == 0, kc == KC - 1,
                                        w1b + [self.RAb[kc][tb]], [b1B], signal=(kc == KC - 1))
                            for kc in range(KC):
                                self.mm(b3_[:, :], W3[:, kc, j * 128:(j + 1) * 128], self.RA[:, kc, ts_], kc == 0, kc == KC - 1,
                                        w3b + [self.RAb[kc][tb]], [b3B], signal=(kc == KC - 1))
                            s_, sB, g0, g0B = tmr.next()
                            self.act(s_, b1[:, :], AF.Silu, [b1B], [sB])
                            gd = gTt[:, f, t2 * 512:(t2 + 1) * 512]
                            if moe:
                                self.tt("dve", g0, b3_[:, :], s_, ALU.mult, [b3B, sB], [g0B])
                                self.tt("pool", gd, g0, wbt[:, ex, t2 * 512:(t2 + 1) * 512], ALU.mult, [g0B, wbB[ex]], [gB[f][t2]])
                            else:
                                self.tt("dve", gd, b3_[:, :], s_, ALU.mult, [b3B, sB], [gB[f][t2]])
                    ws.finish(jn)
                for o in range(KC):
                    jn = jidx[("f2", ex, o)]
                    ((W2, w2b),) = ws.get(jn)
                    for t2 in range(2):
                        bk, bb = self.allbanks.next()
                        for f in range(NF):
                            self.mm(bk[:, :], W2[:, f, :], gTt[:, f, t2 * 512:(t2 + 1) * 512], f == 0, f == NF - 1, w2b + [gB[f][t2]], [bb],
                                    signal=(f == NF - 1))
                        fa = facc[:, o, t2 * 512:(t2 + 1) * 512]
                        if ex == 0:
                            self.cp("act", fa, bk[:, :], [bb], [faccB[o][t2]])
                        else:
                            self.tt("dve", fa, fa, bk[:, :], ALU.add, [bb, faccB[o][t2]], [faccB[o][t2]])
                    ws.finish(jn)
            xsv = self.xs.rearrange("k p t -> p k t")
            pp = pn
            xi = []
            for i in range(2):
                v, pp = self.ovv(pp, KC * 512, F32)
                xi.append((v.rearrange("p (k t) -> p k t", k=KC), Buf()))
            self.S.barrier()
            for t2 in range(2):
                tb = tbs[t2]
                x, xB = xi[t2]
                self.dma("sp", x, xsv[:, :, tb * 512:(tb + 1) * 512], [self.xsB[tb]], [xB])
                for o in range(KC):
                    self.stt("dve", x[:, o, :], facc[:, o, t2 * 512:(t2 + 1) * 512], gate2[:, o:o + 1], x[:, o, :], ALU.mult, ALU.add,
                             [faccB[o][t2], xB, self.modB], [xB])
                self.dma("sp", xsv[:, :, tb * 512:(tb + 1) * 512], x, [xB], [self.xsB[tb]])
            self.S.barrier()

    def final(self):
        p = 0
        xi = []
        for i in range(2):
            v, p = self.ovv(p, KC * 512, F32)
            xi.append((v.rearrange("p (k t) -> p k t", k=KC), Buf()))
        xir = Ring(xi)
        sq = []
        for i in range(2):
            v, p = self.ovv(p, 512)
            sq.append((v, Buf()))
        sqr = Ring(sq)
        rt = []
        for i in range(2):
            v, p = self.ovv(p, 512, F32)
            rt.append((v, Buf()))
        rtr = Ring(rt)
        tm = []
        for i in range(3):
            v, p = self.ovv(p, 512, F32)
            tm.append((v, Buf()))
        tmr = Ring(tm)
        ob = []
        for i in range(2):
            v, p = self.ovv(p, 4 * D, F32)
            ob.append((v.rearrange("p (a k m) -> p a k m", a=4, k=KC), Buf()))
        obr = Ring(ob)
        xsv = self.xs.rearrange("k p t -> p k t")
        ov = self.out_d.rearrange("(a p) d -> p a d", p=128)
        for tb in range(NTB):
            x, xB = xir.next()
            self.dma("sp", x, xsv[:, :, tb * 512:(tb + 1) * 512], [self.xsB[tb]], [xB])
            bk, bb = self.allbanks.next()
            for kc in range(KC):
                s, sB = sqr.next()
                self.act(s, x[:, kc, :], AF.Square, [xB], [sB])
                self.mm(bk[:, :], self.ones_b, s, kc == 0, kc == KC - 1, [sB, self.cbfB], [bb], signal=True)
            r, rB = rtr.next()
            self.act(r, bk[:, :], AF.Sqrt, [bb, self.cstB], [rB], scale=1.0 / D, bias=self.eps_ap)
            self.recip(r, r, [rB], [rB])
            o_, oB = obr.next()
            for kc in range(KC):
                t, tB = tmr.next()
                self.stt("dve", t, x[:, kc, :], self.finT[:, kc:kc + 1], r, ALU.mult, ALU.mult, [xB, rB, self.vecB], [tB])
                b2, b2B = self.allbanks.next()
                for a in range(4):
                    self.tr(b2[:, a * 128:(a + 1) * 128], t[:, a * 128:(a + 1) * 128], self.ident_f, [tB, self.cstB], [b2B])
                self.cp("act", o_[:, :, kc, :], b2[:, :].rearrange("p (a m) -> p a m", a=4), [b2B], [oB])
            self.dma("sp", ov[:, 4 * tb:4 * tb + 4, :], o_.rearrange("p a k m -> p a (k m)"), [oB], [self.outB[tb]])


_CACHE = {}


def _vecpack(inputs):
    vp = np.zeros((DEPTH, NVEC, 128), np.float32)
    for l in range(DEPTH):
        vp[l, 0:48] = np.asarray(inputs["ada_b"][l], np.float32).reshape(48, 128)
        vp[l, 48:56] = np.asarray(inputs["norm_mix"][l], np.float32).reshape(8, 128)
        vp[l, 56:64] = np.asarray(inputs["norm_ffn"][l], np.float32).reshape(8, 128)
        vp[l, 64:70] = np.asarray(inputs["mla_q_norm"][l], np.float32).reshape(6, 128)
        vp[l, 70:74] = np.asarray(inputs["mla_kv_norm"][l], np.float32).reshape(4, 128)
        vp[l, 74:82] = np.asarray(inputs["ret_norm"][l], np.float32).reshape(8, 128)
    return vp


def make_in_maps(inputs, ncores=NCORES):
    cst, dpn = make_consts()
    f = lambda k: np.ascontiguousarray(np.asarray(inputs[k], np.float32))
    shared = {
        "ada_w": f("ada_w"), "vecpack": _vecpack(inputs), "final_norm": f("final_norm").reshape(8, 128),
        "w_in": f("w_in"), "mla_w_uq": f("mla_w_uq"), "mla_w_ukv": f("mla_w_ukv"),
        "ret_log_decay": f("ret_log_decay").reshape(1, DEPTH * 16),
        "w_br_mla": f("w_br_mla"), "w_br_dil": f("w_br_dil"), "w_br_ret": f("w_br_ret"), "w_out": f("w_out"),
        "ffn_w1": f("ffn_w1"), "ffn_w3": f("ffn_w3"), "ffn_w2": f("ffn_w2"),
        "moe_router": f("moe_router"), "moe_w1": f("moe_w1"), "moe_w3": f("moe_w3"), "moe_w2": f("moe_w2"),
        "consts": cst, "dposneg": dpn,
    }
    x = f("x")
    c = f("c")
    pos = np.ascontiguousarray(np.asarray(inputs["positions"], np.int32))
    maps = []
    for b in range(ncores):
        m = dict(shared)
        m["x"] = x[b]
        m["c"] = c[b:b + 1]
        m["positions"] = pos[b:b + 1]
        maps.append(m)
    return maps


def kernel(**inputs):
    if "nc" not in _CACHE:
        _CACHE["nc"] = Builder().build()
    nc = _CACHE["nc"]
    maps = make_in_maps(inputs)
    res = run_bass_kernel_spmd(nc, maps, core_ids=list(range(NCORES)))
    return np.stack([np.asarray(r["out"], np.float32) for r in res.results], axis=0)
```
